# Optimizing a Trainium2 kernel written in Bass

```python
import math
import jax, jax.numpy as jnp
from jax import lax
import numpy as np

D_MODEL = 1024
BATCH = 8
SEQ = 4096
DEPTH = 1

N_META = 16
GDN_HEADS = 8
GDN_DK = 128
GDN_DV = 128
CONV_WIDTH = 4
CHUNK = 64
ATT_HEADS = 8
ATT_KV_HEADS = 2
ATT_HD = 128
ATT_GROUP = ATT_HEADS // ATT_KV_HEADS
IDX_HEADS = 8
IDX_HD = 64
TOPK_MAX = 256
Q_BLOCK = 128
ROPE_THETA = 10000.0
D_FF = 4 * D_MODEL
EPS = 1e-6

GDN_QK_W = GDN_HEADS * GDN_DK
GDN_V_W = GDN_HEADS * GDN_DV
ATT_Q_W = ATT_HEADS * ATT_HD
ATT_KV_W = ATT_KV_HEADS * ATT_HD
SPLITS = (GDN_QK_W, GDN_QK_W, GDN_V_W, GDN_V_W, GDN_HEADS, GDN_HEADS,
          ATT_Q_W, ATT_KV_W, ATT_KV_W, IDX_HEADS * IDX_HD, IDX_HD, IDX_HEADS,
          D_MODEL, D_MODEL)
N_IN = sum(SPLITS)

kernel_name = "hybrid_gdn_dsa_gated_block"


def _rmsnorm(x, g):
    xf = x.astype(jnp.float32)
    y = xf * lax.rsqrt(jnp.mean(xf * xf, axis=-1, keepdims=True) + EPS)
    return (y * g.astype(jnp.float32)).astype(x.dtype)


def _l2norm(x):
    return x * lax.rsqrt(jnp.sum(x * x, axis=-1, keepdims=True) + EPS)


def _rope_tables(pos, dim):
    inv = ROPE_THETA ** (-jnp.arange(0, dim, 2, dtype=jnp.float32) / dim)
    ang = pos.astype(jnp.float32)[:, None] * inv[None, :]
    return jnp.cos(ang), jnp.sin(ang)


def _apply_rope(x, cos, sin):
    half = x.shape[-1] // 2
    xf = x.astype(jnp.float32)
    x1, x2 = xf[..., :half], xf[..., half:]
    c = cos[None, :, None, :]
    s = sin[None, :, None, :]
    return jnp.concatenate([x1 * c - x2 * s, x2 * c + x1 * s], axis=-1).astype(x.dtype)


def _causal_conv(x, w):
    K = w.shape[0]
    T = x.shape[1]
    xp = jnp.pad(x, ((0, 0), (K - 1, 0), (0, 0)))
    y = xp[:, 0:T] * w[0]
    for j in range(1, K):
        y = y + xp[:, j:j + T] * w[j]
    return y


def _gated_delta_rule(q, k, v, beta, g):
    B_, T_, H, dk = q.shape
    dv = v.shape[-1]
    C = CHUNK
    pad = (-N_META) % C
    tail = (-(T_ + pad)) % C

    def padf(a):
        return jnp.pad(a, [(0, 0), (pad, tail)] + [(0, 0)] * (a.ndim - 2))

    q, k, v, beta, g = padf(q), padf(k), padf(v), padf(beta), padf(g)
    N = (T_ + pad + tail) // C

    def vec_chunks(a):
        return a.reshape(B_, N, C, H, a.shape[-1]).transpose(0, 3, 1, 2, 4)

    def sc_chunks(a):
        return a.reshape(B_, N, C, H).transpose(0, 3, 1, 2)

    q, k, v = vec_chunks(q), vec_chunks(k), vec_chunks(v)
    beta, g = sc_chunks(beta), sc_chunks(g)
    gc = jnp.cumsum(g, axis=-1)

    tri_incl = jnp.tril(jnp.ones((C, C), dtype=bool))
    tri_strict = jnp.tril(jnp.ones((C, C), dtype=bool), -1)
    decay = jnp.exp(jnp.where(tri_incl, gc[..., :, None] - gc[..., None, :], -jnp.inf))

    kb = k * beta[..., None]
    A = jnp.where(tri_strict, jnp.einsum('bhncd,bhnsd->bhncs', kb, k) * decay, 0.0)
    eye = jnp.eye(C, dtype=jnp.float32)
    Tm = lax.linalg.triangular_solve(A + eye, jnp.broadcast_to(eye, A.shape),
                                     left_side=True, lower=True, unit_diagonal=True)
    u = jnp.einsum('bhncs,bhnse->bhnce', Tm, v * beta[..., None])
    w = jnp.einsum('bhncs,bhnsd->bhncd', Tm, kb * jnp.exp(gc)[..., None])
    attn = jnp.einsum('bhncd,bhnsd->bhncs', q, k) * decay
    q_dec = q * jnp.exp(gc)[..., None]
    k_tail = k * jnp.exp(gc[..., -1:] - gc)[..., None]
    g_last = jnp.exp(gc[..., -1])

    def to_scan(a):
        return jnp.moveaxis(a, 2, 0)

    xs = (to_scan(q_dec), to_scan(attn), to_scan(u), to_scan(w), to_scan(k_tail), to_scan(g_last))

    def step(S, inp):
        qe, at, u_i, w_i, kt, gl = inp
        v_new = u_i - jnp.einsum('bhcd,bhde->bhce', w_i, S)
        o = jnp.einsum('bhcd,bhde->bhce', qe, S) + jnp.einsum('bhcs,bhse->bhce', at, v_new)
        S = S * gl[..., None, None] + jnp.einsum('bhcd,bhce->bhde', kt, v_new)
        return S, o

    S0 = jnp.zeros((B_, H, dk, dv), jnp.float32)
    _, o = lax.scan(step, S0, xs)
    o = o.transpose(1, 0, 3, 2, 4).reshape(B_, N * C, H, dv)
    return o[:, pad:pad + T_]


def _gdn_branch(q, k, v, z, b_raw, a_raw, conv_w, a_log, dt_bias, norm_w):
    B_, T_, _ = q.shape
    dt = q.dtype
    qkv = jax.nn.silu(_causal_conv(jnp.concatenate([q, k, v], axis=-1), conv_w))
    q, k, v = jnp.split(qkv, [GDN_QK_W, 2 * GDN_QK_W], axis=-1)
    q = q.reshape(B_, T_, GDN_HEADS, GDN_DK).astype(jnp.float32)
    k = k.reshape(B_, T_, GDN_HEADS, GDN_DK).astype(jnp.float32)
    v = v.reshape(B_, T_, GDN_HEADS, GDN_DV).astype(jnp.float32)
    q = _l2norm(q) * (GDN_DK ** -0.5)
    k = _l2norm(k)
    beta = jax.nn.sigmoid(b_raw.astype(jnp.float32))
    g = -jnp.exp(a_log.astype(jnp.float32)) * jax.nn.softplus(a_raw.astype(jnp.float32) + dt_bias.astype(jnp.float32))
    o = _gated_delta_rule(q, k, v, beta, g)
    zf = z.reshape(B_, T_, GDN_HEADS, GDN_DV).astype(jnp.float32)
    o = _rmsnorm(o, norm_w) * jax.nn.silu(zf)
    return o.reshape(B_, T_, GDN_V_W).astype(dt)


def _dsa_branch(q, k, v, qi, ki, wi, rope_att, rope_idx, n_visible):
    B_, T_, _ = q.shape
    cos_a, sin_a = rope_att
    cos_i, sin_i = rope_idx
    q = _apply_rope(q.reshape(B_, T_, ATT_HEADS, ATT_HD), cos_a, sin_a)
    k = _apply_rope(k.reshape(B_, T_, ATT_KV_HEADS, ATT_HD), cos_a, sin_a)
    v = v.reshape(B_, T_, ATT_KV_HEADS, ATT_HD)
    qi = _apply_rope(qi.reshape(B_, T_, IDX_HEADS, IDX_HD), cos_i, sin_i)
    ki = _apply_rope(ki.reshape(B_, T_, 1, IDX_HD), cos_i, sin_i)[:, :, 0]
    wi = wi * ((IDX_HEADS * IDX_HD) ** -0.5)
    kv = jnp.concatenate([k, v], axis=-1)
    topk = min(TOPK_MAX, n_visible // 4)
    nb = -(-T_ // Q_BLOCK)
    Tq = nb * Q_BLOCK

    def to_blocks(a):
        a = jnp.pad(a, [(0, 0), (0, Tq - T_)] + [(0, 0)] * (a.ndim - 2))
        return jnp.moveaxis(a.reshape((B_, nb, Q_BLOCK) + a.shape[2:]), 1, 0)

    key_pos = jnp.arange(T_, dtype=jnp.int32)

    def block(args):
        qb, qib, wib, posb = args
        s = jnp.einsum('bqhd,bsd->bqhs', qib, ki)
        s = jnp.einsum('bqhs,bqh->bqs', jax.nn.relu(s), wib).astype(jnp.float32)
        s = jnp.where(key_pos[None, None, :] <= posb[None, :, None], s, -jnp.inf)
        _, idx = lax.top_k(s, topk)
        kv_sel = jax.vmap(lambda a, i: a[i])(kv, idx)
        k_sel, v_sel = jnp.split(kv_sel, 2, axis=-1)
        qg = qb.reshape(B_, Q_BLOCK, ATT_KV_HEADS, ATT_GROUP, ATT_HD)
        logits = jnp.einsum('bqgrd,bqkgd->bqgrk', qg, k_sel).astype(jnp.float32) * (ATT_HD ** -0.5)
        valid = (idx <= posb[None, :, None])[:, :, None, None, :]
        p = jax.nn.softmax(jnp.where(valid, logits, -jnp.inf), axis=-1).astype(v.dtype)
        o = jnp.einsum('bqgrk,bqkgd->bqgrd', p, v_sel)
        return o.reshape(B_, Q_BLOCK, ATT_Q_W)

    pos_blocks = jnp.arange(Tq, dtype=jnp.int32).reshape(nb, Q_BLOCK)
    out = lax.map(block, (to_blocks(q), to_blocks(qi), to_blocks(wi), pos_blocks))
    return jnp.moveaxis(out, 0, 1).reshape(B_, Tq, ATT_Q_W)[:, :T_]


def setup_inputs(seed: int = 0) -> dict:
    key = jax.random.key(seed)
    ks = jax.random.split(key, 20)
    f32 = jnp.float32

    def nrm(k, shape, scale):
        return jax.random.normal(k, shape, f32) * scale

    def gain(k, shape):
        return 1.0 + 0.02 * jax.random.normal(k, shape, f32)

    dt = jnp.exp(jax.random.uniform(ks[5], (DEPTH, GDN_HEADS), f32, math.log(1e-3), math.log(1e-1)))
    return {
        "x": jax.random.normal(ks[0], (BATCH, SEQ, D_MODEL), f32),
        "meta_tokens": nrm(ks[1], (N_META, D_MODEL), 1.0),
        "pre_mix_norm": gain(ks[2], (DEPTH, D_MODEL)),
        "w_in": nrm(ks[3], (DEPTH, D_MODEL, N_IN), D_MODEL ** -0.5),
        "conv_w": nrm(ks[4], (DEPTH, CONV_WIDTH, 2 * GDN_QK_W + GDN_V_W), CONV_WIDTH ** -0.5),
        "a_log": jnp.log(jax.random.uniform(ks[6], (DEPTH, GDN_HEADS), f32, 1.0, 16.0)),
        "dt_bias": jnp.log(jnp.expm1(dt)),
        "gdn_norm": gain(ks[7], (DEPTH, GDN_DV)),
        "w_branch_gdn": nrm(ks[8], (DEPTH, GDN_V_W, D_MODEL), GDN_V_W ** -0.5),
        "w_branch_dsa": nrm(ks[9], (DEPTH, ATT_Q_W, D_MODEL), ATT_Q_W ** -0.5),
        "w_out": nrm(ks[10], (DEPTH, D_MODEL, D_MODEL), D_MODEL ** -0.5),
        "post_mix_norm": gain(ks[11], (DEPTH, D_MODEL)),
        "pre_mlp_norm": gain(ks[12], (DEPTH, D_MODEL)),
        "w_up": nrm(ks[13], (DEPTH, D_MODEL, D_FF), D_MODEL ** -0.5),
        "w_down": nrm(ks[14], (DEPTH, D_FF, D_MODEL), D_FF ** -0.5),
        "post_mlp_norm": gain(ks[15], (DEPTH, D_MODEL)),
    }


def reference(x, meta_tokens, pre_mix_norm, w_in, conv_w, a_log, dt_bias, gdn_norm,
              w_branch_gdn, w_branch_dsa, w_out, post_mix_norm, pre_mlp_norm, w_up, w_down,
              post_mlp_norm):
    B_, L, _ = x.shape
    meta = jnp.broadcast_to(meta_tokens.astype(x.dtype)[None], (B_, N_META, D_MODEL))
    h = jnp.concatenate([meta, x], axis=1)
    T_ = h.shape[1]
    pos = jnp.arange(T_, dtype=jnp.int32)
    rope_att = _rope_tables(pos, ATT_HD)
    rope_idx = _rope_tables(pos, IDX_HD)
    split_at = [int(i) for i in np.cumsum(SPLITS)[:-1]]

    for l in range(DEPTH):
        n = _rmsnorm(h, pre_mix_norm[l])
        proj = jnp.einsum('btd,dn->btn', n, w_in[l])
        (gq, gk, gv, gz, gb, ga, aq, ak, av, iq, ik, iw, gate_a, gate_b) = jnp.split(proj, split_at, axis=-1)
        y_gdn = _gdn_branch(gq, gk, gv, gz, gb, ga, conv_w[l], a_log[l], dt_bias[l], gdn_norm[l])
        y_dsa = _dsa_branch(aq, ak, av, iq, ik, iw, rope_att, rope_idx, L)
        merged = (jax.nn.sigmoid(gate_a) * jnp.einsum('btv,vd->btd', y_gdn, w_branch_gdn[l])
                  + jax.nn.sigmoid(gate_b) * jnp.einsum('btv,vd->btd', y_dsa, w_branch_dsa[l]))
        mix = jnp.einsum('btd,de->bte', merged, w_out[l])
        h = h + _rmsnorm(mix, post_mix_norm[l])
        n2 = _rmsnorm(h, pre_mlp_norm[l])
        u = jnp.square(jax.nn.relu(jnp.einsum('btd,df->btf', n2, w_up[l])))
        h = h + _rmsnorm(jnp.einsum('btf,fd->btd', u, w_down[l]), post_mlp_norm[l])

    return h[:, N_META:]
```

```python
import numpy as np
import concourse.bass as bass
import concourse.mybir as mybir
from concourse.bass_utils import run_bass_kernel_spmd
from contextlib import ExitStack

F32 = mybir.dt.float32
BF16 = mybir.dt.bfloat16
ALU = mybir.AluOpType
AF = mybir.ActivationFunctionType
AX = mybir.AxisListType

T = 4224
NT = 33
PAD = 112
D = 1024
EPS = 1e-6
C_GQ, C_GK, C_GV, C_GZ, C_GB, C_GA = 0, 1024, 2048, 3072, 4096, 4104
C_AQ, C_AK, C_AV, C_IQ, C_IK, C_IW, C_GTA, C_GTB = 4112, 5136, 5392, 5648, 6160, 6224, 6232, 7256
N_IN = 8280


class Buf:
    __slots__ = ("name", "w", "r")

    def __init__(self, name):
        self.name = name
        self.w = None
        self.r = {}


class Tok:
    __slots__ = ("key", "val", "hist")

    def __init__(self, key, val, hist):
        self.key = key
        self.val = val
        self.hist = hist


class Eng:
    def __init__(self, name, handle, sem, key):
        self.name = name
        self.h = handle
        self.sem = sem
        self.key = key
        self.count = 0
        self.seen = {}
        self.snap = {}
        self.nwaits = 0
        self.ninstr = 0


class Sched:
    NDMA = 24

    def __init__(self, nc, es):
        self.nc = nc
        self.es = es
        self.sems = []
        self.E = {}
        for name, h in [("pe", nc.tensor), ("dve", nc.vector), ("act", nc.scalar),
                        ("pool", nc.gpsimd), ("sp", nc.sync)]:
            sem = es.enter_context(nc.semaphore("s_" + name))
            e = Eng(name, h, sem, len(self.sems))
            self.sems.append(sem)
            self.E[name] = e
        self.dma = []
        for i in range(self.NDMA):
            sem = es.enter_context(nc.semaphore("d%d" % i))
            self.dma.append([len(self.sems), 0])
            self.sems.append(sem)
        self.dma_rr = 0

    def _wait(self, e, tok):
        if tok is None:
            return
        if e.seen.get(tok.key, 0) >= tok.val:
            return
        if tok.key == e.key and e.name == "pe":
            return
        e.h.wait_ge(self.sems[tok.key], tok.val)
        e.nwaits += 1
        e.seen[tok.key] = tok.val
        if tok.hist:
            for k, v in tok.hist.items():
                if e.seen.get(k, 0) < v:
                    e.seen[k] = v
        e.snap = None

    def _deps(self, e, reads, writes):
        for b in reads:
            self._wait(e, b.w)
        for b in writes:
            self._wait(e, b.w)
            for t in b.r.values():
                self._wait(e, t)

    def _commit(self, tok, reads, writes):
        for b in reads:
            o = b.r.get(tok.key)
            if o is None or o.val < tok.val:
                b.r[tok.key] = tok
        for b in writes:
            b.w = tok
            b.r = {}

    def op(self, eng, fn, reads=(), writes=()):
        e = self.E[eng]
        self._deps(e, reads, writes)
        ins = fn()
        e.count += 1
        e.ninstr += 1
        ins.then_inc(e.sem, 1)
        if e.snap is None:
            e.snap = dict(e.seen)
        tok = Tok(e.key, e.count, e.snap)
        self._commit(tok, reads, writes)
        return tok

    def group(self, eng, fns, reads=(), writes=()):
        e = self.E[eng]
        self._deps(e, reads, writes)
        ins = None
        for fn in fns:
            ins = fn()
            e.ninstr += 1
        e.count += 1
        ins.then_inc(e.sem, 1)
        if e.snap is None:
            e.snap = dict(e.seen)
        tok = Tok(e.key, e.count, e.snap)
        self._commit(tok, reads, writes)
        return tok

    def dma_op(self, eng, out, in_, reads=(), writes=(), **kw):
        e = self.E[eng]
        if eng == "pool":
            sem = self.es.enter_context(self.nc.semaphore("q%d" % len(self.sems)))
            slot = [len(self.sems), 0]
            self.sems.append(sem)
            self.dma.append(slot)
        else:
            slot = self.dma[self.dma_rr]
            self.dma_rr = (self.dma_rr + 1) % self.NDMA
        key = slot[0]
        if slot[1] > 0:
            self._wait(e, Tok(key, slot[1], None))
        self._deps(e, reads, writes)
        slot[1] += 16
        ins = e.h.dma_start(out=out, in_=in_, **kw)
        ins.then_inc(self.sems[key], 16)
        e.ninstr += 1
        tok = Tok(key, slot[1], None)
        self._commit(tok, reads, writes)
        return tok

    def barrier(self):
        for e in self.E.values():
            for o in self.E.values():
                if o is not e and o.count > 0:
                    self._wait(e, Tok(o.key, o.count, None))
            for key, val in self.dma:
                if val > 0:
                    self._wait(e, Tok(key, val, None))

    def finish(self):
        self.barrier()
        for e in self.E.values():
            if e.count > 0 and e.name != "pe":
                e.h.wait_ge(e.sem, e.count)

    def stats(self):
        return {n: (e.ninstr, e.nwaits) for n, e in self.E.items()}


class K:
    pass


def build(debug=False, phases="ABCDE"):
    nc = bass.Bass("TRN2", target_bir_lowering=False)
    k = K()
    k.nc = nc
    k.debug = debug

    def din(name, shape, dt=F32):
        return nc.dram_tensor(name, list(shape), dt, kind="ExternalInput").ap()

    def dscr(name, shape, dt):
        kind = "ExternalOutput" if debug else "Internal"
        return nc.dram_tensor(name, list(shape), dt, kind=kind).ap()

    k.x = din("x", [4096, D])
    k.meta = din("meta", [16, D])
    k.w_in = din("w_in", [D, N_IN])
    k.w_sw = din("w_sw", [D, 15 * 256])
    k.conv_w = din("conv_w", [4, 3072])
    k.a_log = din("a_log", [1, 8])
    k.dt_bias = din("dt_bias", [1, 8])
    k.gdn_norm = din("gdn_norm", [1, 128])
    k.wg = din("wg", [D, D])
    k.wd = din("wd", [D, D])
    k.wo = din("wo", [D, D])
    k.w_up = din("w_up", [D, 4096])
    k.w_down = din("w_down", [4096, D])
    k.norms = din("norms", [4, D])
    k.ident_d = din("ident", [128, 128])
    k.cosA = din("cosA", [128, T])
    k.sinA = din("sinA", [128, T])
    k.cosI = din("cosI", [128, T])
    k.sinI = din("sinI", [128, T])
    k.out = nc.dram_tensor("out", [4096, D], F32, kind="ExternalOutput").ap()

    k.QG = dscr("QG", [T, 1024], BF16)
    k.KG = dscr("KG", [T, 1024], BF16)
    k.VG = dscr("VG", [T, 1024], BF16)
    k.ZS = dscr("ZS", [T, 1024], F32)
    k.BG = dscr("BG", [T, 16], F32)
    k.QT = dscr("QT", [8, 128, T], BF16)
    k.KT = dscr("KT", [2, 128, T], BF16)
    k.Vd = dscr("V", [T, 256], BF16)
    k.QIT = dscr("QIT", [4, 128, T], BF16)
    k.KIT = dscr("KIT", [128, T], BF16)
    k.IW = dscr("IW", [T, 8], F32)
    k.SGA = dscr("SGA", [T, 1024], F32)
    k.SGB = dscr("SGB", [T, 1024], F32)
    k.MDSA = dscr("MDSA", [T, 1024], F32)
    k.H1 = dscr("H1", [T, 1024], F32)
    if debug:
        k.YGDN = dscr("YGDN", [T, 1024], BF16)
    k.gmasks = din("gmasks", [4, 128, 128])
    k.cmask = din("cmask", [128, 2])
    k.masks = din("masks", [3, 128, 128])
    k.pow2 = din("pow2", [1, 15])

    with ExitStack() as es:
        S = Sched(nc, es)
        k.S = S
        k.es = es

        def sb(name, shape, dt, stack=es):
            return stack.enter_context(nc.sbuf_tensor(name, list(shape), dt))
        k.sb = sb

        k.PSB = [es.enter_context(nc.psum_tensor("psb%d" % i, [128, 1024], F32)) for i in range(4)]
        k.PS = [k.PSB[i // 2][:, (i % 2) * 512:(i % 2 + 1) * 512] for i in range(8)]
        k.PB = [Buf("ps%d" % i) for i in range(8)]
        k.ps_rr = 0
        k.ps_n = 8

        k.ps_base = 0

        def nextps():
            i = k.ps_rr % k.ps_n
            k.ps_rr = (i + 1) % k.ps_n
            return k.PS[k.ps_base + i], k.PB[k.ps_base + i]
        k.nextps = nextps

        k.identf = sb("identf", [128, 128], F32)
        k.identb = sb("identb", [128, 128], BF16)
        k.b_ident = Buf("ident")
        S.dma_op("sp", k.identf[:], k.ident_d, writes=[k.b_ident])
        S.op("dve", lambda: nc.vector.tensor_copy(k.identb[:], k.identf[:]), reads=[k.b_ident], writes=[k.b_ident])

        if "A" in phases:
            phase_A(k)
            S.barrier()
        if "C" in phases:
            phase_C(k)
        if "B" in phases:
            phase_B(k)
        if "E" in phases:
            phase_E(k)
        S.finish()
        print("instr stats", S.stats())
    return nc


def phase_A(k):
    nc, S = k.nc, k.S
    with ExitStack() as es:
        cur = [es]

        def sb(name, shape, dt):
            return cur[0].enter_context(nc.sbuf_tensor(name, list(shape), dt))

        def substack():
            st = ExitStack()
            cur[0] = st
            return st

        def endsub(st):
            S.barrier()
            st.close()
            cur[0] = es

        nT = sb("nT", [128, 8, 3 + T], BF16)
        b_nT = [Buf("nT%d" % i) for i in range(NT)]
        b_nTpad = Buf("nTpad")
        S.op("pool", lambda: nc.gpsimd.memset(nT[:, :, 0:3], 0.0), writes=[b_nTpad])

        ysb = [sb("ysb%d" % i, [128, 512], F32) for i in range(2)]
        b_ysb = [Buf("ysb%d" % i) for i in range(2)]
        ob = [sb("ob%d" % i, [128, 512], BF16) for i in range(3)]
        b_ob = [Buf("ob%d" % i) for i in range(3)]
        of = [sb("of%d" % i, [128, 512], F32) for i in range(3)]
        b_of = [Buf("of%d" % i) for i in range(3)]
        sq = [sb("sq%d" % i, [128, 8], F32) for i in range(2)]
        b_sq = [Buf("sq%d" % i) for i in range(2)]
        nhalf = sb("nhalf", [128, 4], F32)
        b_nhalf = Buf("nhalf")
        S.op("pool", lambda: nc.gpsimd.memset(nhalf[:], -0.5), writes=[b_nhalf])
        junkf = sb("junkf", [128, 128], F32)
        b_junkf = Buf("junkf")

        st = substack()
        g1 = sb("g1", [128, D], F32)
        b_g1 = Buf("g1")
        S.dma_op("sp", g1[:], k.norms[0:1, :].partition_broadcast(128), writes=[b_g1])

        ht = [sb("ht%d" % i, [128, D], F32) for i in range(2)]
        b_ht = [Buf("ht%d" % i) for i in range(2)]
        junk = sb("junkA", [128, D], BF16)
        b_junk = Buf("junkA")
        nb = [sb("nb%d" % i, [128, D], BF16) for i in range(2)]
        b_nb = [Buf("nb%d" % i) for i in range(2)]
        ss = [sb("ssA%d" % i, [128, 4], F32) for i in range(2)]
        b_ss = [Buf("ssA%d" % i) for i in range(2)]

        for i in range(NT):
            p = i % 2
            h_, bh = ht[p], b_ht[p]
            if i == 0:
                S.op("pool", lambda: nc.gpsimd.memset(h_[:], 0.0), writes=[bh])
                S.dma_op("sp", h_[PAD:128, :], k.meta, writes=[bh])
            else:
                S.dma_op("sp", h_[:], k.x[(i - 1) * 128:i * 128, :], writes=[bh])
            s_, bs = ss[p], b_ss[p]
            S.op("act", lambda: nc.scalar.activation(out=junk[:], in_=h_[:], func=AF.Square,
                                                     accum_out=s_[:, 0:1]),
                 reads=[bh], writes=[b_junk, bs])
            S.op("act", lambda: nc.scalar.activation(out=s_[:, 1:2], in_=s_[:, 0:1], func=AF.Sqrt,
                                                     scale=1.0 / D, bias=EPS),
                 reads=[bs], writes=[bs])
            S.op("dve", lambda: nc.vector.reciprocal(s_[:, 2:3], s_[:, 1:2]), reads=[bs], writes=[bs])
            n_, bn = nb[p], b_nb[p]
            S.op("dve", lambda: nc.vector.scalar_tensor_tensor(out=n_[:], in0=h_[:], scalar=s_[:, 2:3],
                                                               in1=g1[:], op0=ALU.mult, op1=ALU.mult),
                 reads=[bh, bs, b_g1], writes=[bn])
            ps, bp = k.nextps()
            psb = ps[:].bitcast(BF16).rearrange("p (k t) -> p k t", k=8)
            fns = []
            for kk in range(8):
                fns.append(lambda kk=kk: nc.tensor.transpose(psb[:, kk, :], n_[:, kk * 128:(kk + 1) * 128],
                                                             k.identb[:]))
            S.group("pe", fns, reads=[bn, k.b_ident], writes=[bp])
            S.op("dve", lambda: nc.vector.tensor_copy(nT[:, :, 3 + i * 128:3 + (i + 1) * 128], psb),
                 reads=[bp], writes=[b_nT[i]])

        endsub(st)
        st = substack()
        wst = [sb("wst%d" % i, [128, 8, 512], F32) for i in range(1)]
        b_wst = [Buf("wst%d" % i) for i in range(1)]
        w4 = [sb("w4_%d" % i, [128, 8, 4, 512], BF16) for i in range(2)]
        b_w4 = [Buf("w4_%d" % i) for i in range(2)]
        cw = [sb("cw%d" % i, [128, 4, 512], F32) for i in range(1)]
        b_cw = [Buf("cw%d" % i) for i in range(1)]
        cnt = {"g": 0, "t": 0}

        def conv_group(cc0, kind):
            gp = cnt["g"] % 2
            cnt["g"] += 1
            S.dma_op("sp", wst[0][:], k.w_in[:, cc0:cc0 + 512].rearrange("(k p) n -> p k n", p=128),
                     writes=[b_wst[0]])
            S.dma_op("sp", cw[0][:], k.conv_w[:, cc0:cc0 + 512].partition_broadcast(128),
                     writes=[b_cw[0]])
            for j in range(4):
                S.op("dve", lambda: nc.vector.tensor_tensor(
                    out=w4[gp][:, :, j, :], in0=wst[0][:],
                    in1=cw[0][:, j:j + 1, :].broadcast_to([128, 8, 512]), op=ALU.mult),
                    reads=[b_wst[0], b_cw[0]], writes=[b_w4[gp]])
            dst = {"q": k.QG, "k": k.KG, "v": k.VG}[kind]
            dcol = cc0 % 1024
            for i in range(NT):
                ps, bp = k.nextps()
                fns = []
                for j in range(4):
                    for kk in range(8):
                        fns.append(lambda j=j, kk=kk: nc.tensor.matmul(
                            ps[:], lhsT=nT[:, kk, i * 128 + j:i * 128 + j + 128], rhs=w4[gp][:, kk, j, :],
                            start=(j == 0 and kk == 0), stop=(j == 3 and kk == 7)))
                rd = [b_w4[gp], b_nT[i], b_nTpad] + ([b_nT[i - 1]] if i > 0 else [])
                S.group("pe", fns, reads=rd, writes=[bp])
                tp = cnt["t"] % 2
                t3 = cnt["t"] % 3
                cnt["t"] += 1
                o_, bo = ob[t3], b_ob[t3]
                if kind == "v":
                    S.op("act", lambda: nc.scalar.activation(out=o_[:], in_=ps[:], func=AF.Silu),
                         reads=[bp], writes=[bo])
                else:
                    y_, by = ysb[tp], b_ysb[tp]
                    q_, bq = sq[tp], b_sq[tp]
                    S.op("act", lambda: nc.scalar.activation(out=y_[:], in_=ps[:], func=AF.Silu),
                         reads=[bp], writes=[by])
                    for hh in range(4):
                        S.op("dve", lambda: nc.vector.scalar_tensor_tensor(
                            out=junkf[:], in0=y_[:, hh * 128:(hh + 1) * 128], scalar=1.0,
                            in1=y_[:, hh * 128:(hh + 1) * 128], op0=ALU.mult, op1=ALU.mult,
                            accum_out=q_[:, hh:hh + 1]),
                            reads=[by], writes=[b_junkf, bq])
                    sc = 128.0 if kind == "q" else 1.0
                    S.op("pool", lambda: nc.gpsimd.tensor_scalar(out=q_[:, 0:4], in0=q_[:, 0:4], scalar1=sc,
                                                                 scalar2=sc * EPS, op0=ALU.mult, op1=ALU.add),
                         reads=[bq], writes=[bq])
                    S.op("pool", lambda: nc.gpsimd.tensor_tensor(out=q_[:, 4:8], in0=q_[:, 0:4], in1=nhalf[:],
                                                                 op=ALU.pow),
                         reads=[bq, b_nhalf], writes=[bq])
                    for hh in range(4):
                        S.op("dve", lambda: nc.vector.tensor_scalar(
                            out=o_[:, hh * 128:(hh + 1) * 128], in0=y_[:, hh * 128:(hh + 1) * 128],
                            scalar1=q_[:, 4 + hh:5 + hh], scalar2=None, op0=ALU.mult),
                            reads=[by, bq], writes=[bo])
                S.dma_op("sp", dst[i * 128:(i + 1) * 128, dcol:dcol + 512], o_[:], reads=[bo])

        for cb in range(6):
            conv_group(cb * 512, "qqkkvv"[cb])

        endsub(st)
        st = substack()
        wt = [sb("wt%d" % i, [128, 8, 512], BF16) for i in range(2)]
        b_wt = [Buf("wt%d" % i) for i in range(2)]
        albc = sb("albc", [128, 16], F32)
        b_albc = Buf("albc")
        S.dma_op("sp", albc[:, 0:8], k.a_log.partition_broadcast(128), writes=[b_albc])
        S.dma_op("sp", albc[:, 8:16], k.dt_bias.partition_broadcast(128), writes=[b_albc])
        S.op("act", lambda: nc.scalar.activation(out=albc[:, 0:8], in_=albc[:, 0:8], func=AF.Exp),
             reads=[b_albc], writes=[b_albc])

        def plain_group(c0, ncols, post):
            gp = cnt["g"] % 2
            cnt["g"] += 1
            S.dma_op("pool", wt[gp][:, :, 0:ncols], k.w_in[:, c0:c0 + ncols].rearrange("(k p) n -> p k n", p=128),
                     writes=[b_wt[gp]])
            for i in range(NT):
                ps, bp = k.nextps()
                fns = []
                for kk in range(8):
                    fns.append(lambda kk=kk: nc.tensor.matmul(
                        ps[:, 0:ncols], lhsT=nT[:, kk, 3 + i * 128:3 + (i + 1) * 128], rhs=wt[gp][:, kk, 0:ncols],
                        start=(kk == 0), stop=(kk == 7)))
                S.group("pe", fns, reads=[b_wt[gp], b_nT[i]], writes=[bp])
                post(i, ps, bp)

        def post_act(func, dst, dcol, ncols, bf, scale=1.0):
            def post(i, ps, bp):
                t3 = cnt["t"] % 3
                cnt["t"] += 1
                o_, bo = (ob[t3], b_ob[t3]) if bf else (of[t3], b_of[t3])
                S.op("act", lambda: nc.scalar.activation(out=o_[:, 0:ncols], in_=ps[:, 0:ncols], func=func,
                                                         scale=scale),
                     reads=[bp], writes=[bo])
                S.dma_op("sp", dst[i * 128:(i + 1) * 128, dcol:dcol + ncols], o_[:, 0:ncols], reads=[bo])
            return post

        for cb in range(2):
            plain_group(C_GZ + cb * 512, 512, post_act(AF.Silu, k.ZS, cb * 512, 512, False))
        for cb in range(2):
            plain_group(C_GTA + cb * 512, 512, post_act(AF.Sigmoid, k.SGA, cb * 512, 512, False))
        for cb in range(2):
            plain_group(C_GTB + cb * 512, 512, post_act(AF.Sigmoid, k.SGB, cb * 512, 512, False))
        plain_group(C_AV, 256, post_act(AF.Copy, k.Vd, 0, 256, True))
        plain_group(C_IW, 8, post_act(AF.Copy, k.IW, 0, 8, False, scale=512.0 ** -0.5))

        def post_bg(i, ps, bp):
            t3 = cnt["t"] % 3
            cnt["t"] += 1
            o_, bo = of[t3], b_of[t3]
            S.op("act", lambda: nc.scalar.activation(out=o_[:, 0:8], in_=ps[:, 0:8], func=AF.Sigmoid),
                 reads=[bp], writes=[bo])
            S.op("dve", lambda: nc.vector.tensor_tensor(out=o_[:, 16:24], in0=ps[:, 8:16], in1=albc[:, 8:16],
                                                        op=ALU.add),
                 reads=[bp, b_albc], writes=[bo])
            S.op("act", lambda: nc.scalar.activation(out=o_[:, 16:24], in_=o_[:, 16:24], func=AF.Exp),
                 reads=[bo], writes=[bo])
            S.op("act", lambda: nc.scalar.activation(out=o_[:, 16:24], in_=o_[:, 16:24], func=AF.Ln, bias=1.0),
                 reads=[bo], writes=[bo])
            S.op("dve", lambda: nc.vector.scalar_tensor_tensor(out=o_[:, 8:16], in0=o_[:, 16:24], scalar=-1.0,
                                                               in1=albc[:, 0:8], op0=ALU.mult, op1=ALU.mult),
                 reads=[bo, b_albc], writes=[bo])
            S.dma_op("sp", k.BG[i * 128:(i + 1) * 128, :], o_[:, 0:16], reads=[bo])
        plain_group(C_GB, 16, post_bg)

        endsub(st)
        st = substack()
        cosT = sb("cosT", [128, T], F32)
        sinT = sb("sinT", [128, T], F32)
        b_tab = Buf("ropetab")
        wf = [sb("wf%d" % i, [128, 8, 256], BF16) for i in range(2)]
        b_wf = [Buf("wf%d" % i) for i in range(2)]
        t1 = [sb("t1_%d" % i, [128, 512], F32) for i in range(2)]
        b_t1 = [Buf("t1_%d" % i) for i in range(2)]
        t2 = [sb("t2_%d" % i, [128, 512], F32) for i in range(2)]
        b_t2 = [Buf("t2_%d" % i) for i in range(2)]

        def load_tab(c, s):
            S.dma_op("sp", cosT[:], c, writes=[b_tab])
            S.dma_op("sp", sinT[:], s, writes=[b_tab])

        def rope_group(w_ap, dst):
            gp = cnt["g"] % 2
            cnt["g"] += 1
            S.dma_op("pool", wf[gp][:], w_ap.rearrange("(k p) n -> p k n", p=128), writes=[b_wf[gp]])
            for tb in range(9):
                c0 = tb * 512
                n = min(512, T - c0)
                tiles = list(range(c0 // 128, (c0 + n) // 128))
                psA, bpA = k.nextps()
                psB, bpB = k.nextps()
                for (ps, bp, off) in ((psA, bpA, 0), (psB, bpB, 128)):
                    fns = []
                    for kk in range(8):
                        fns.append(lambda kk=kk, ps=ps, off=off: nc.tensor.matmul(
                            ps[:, 0:n], lhsT=wf[gp][:, kk, off:off + 128], rhs=nT[:, kk, 3 + c0:3 + c0 + n],
                            start=(kk == 0), stop=(kk == 7)))
                    S.group("pe", fns, reads=[b_wf[gp]] + [b_nT[i] for i in tiles], writes=[bp])
                tp = cnt["t"] % 2
                t3 = cnt["t"] % 3
                cnt["t"] += 1
                S.op("dve", lambda: nc.vector.tensor_tensor(out=t1[tp][:, 0:n], in0=psA[:, 0:n],
                                                            in1=cosT[:, c0:c0 + n], op=ALU.mult),
                     reads=[bpA, b_tab], writes=[b_t1[tp]])
                S.op("dve", lambda: nc.vector.tensor_tensor(out=t2[tp][:, 0:n], in0=psB[:, 0:n],
                                                            in1=sinT[:, c0:c0 + n], op=ALU.mult),
                     reads=[bpB, b_tab], writes=[b_t2[tp]])
                o_, bo = ob[t3], b_ob[t3]
                S.op("pool", lambda: nc.gpsimd.tensor_tensor(out=o_[:, 0:n], in0=t1[tp][:, 0:n],
                                                             in1=t2[tp][:, 0:n], op=ALU.add),
                     reads=[b_t1[tp], b_t2[tp]], writes=[bo])
                S.dma_op("sp", dst[:, c0:c0 + n], o_[:, 0:n], reads=[bo])

        load_tab(k.cosA, k.sinA)
        for h in range(8):
            rope_group(k.w_sw[:, h * 256:(h + 1) * 256], k.QT[h])
        for h in range(2):
            rope_group(k.w_sw[:, (8 + h) * 256:(9 + h) * 256], k.KT[h])
        load_tab(k.cosI, k.sinI)
        for h in range(4):
            rope_group(k.w_sw[:, (10 + h) * 256:(11 + h) * 256], k.QIT[h])
        rope_group(k.w_sw[:, 14 * 256:15 * 256], k.KIT)
        endsub(st)


def phase_C(k):
    nc, S = k.nc, k.S
    NIT = 14
    with ExitStack() as es:
        def sb(name, shape, dt):
            return es.enter_context(nc.sbuf_tensor(name, list(shape), dt))
        kit = sb("kit", [128, T], BF16)
        kts = sb("kts", [128, 2, T], BF16)
        vs = sb("vs", [128, NT, 256], BF16)
        wd = sb("wd_sb", [128, 8, D], BF16)
        b_res = Buf("resC")
        S.dma_op("sp", kit[:], k.KIT, writes=[b_res])
        S.dma_op("sp", kts[:], k.KT.rearrange("g p t -> p g t"), writes=[b_res])
        S.dma_op("sp", vs[:], k.Vd.rearrange("(j p) c -> p j c", p=128), writes=[b_res])
        S.dma_op("pool", wd[:], k.wd.rearrange("(h p) n -> p h n", p=128), writes=[b_res])
        msk = sb("msk", [128, 3, 128], F32)
        S.dma_op("sp", msk[:], k.masks.rearrange("m p c -> p m c"), writes=[b_res])
        p2 = sb("p2", [128, NIT + 1], F32)
        S.dma_op("sp", p2[:], k.pow2.partition_broadcast(128), writes=[b_res])
        ones = sb("onesC", [128, 128], BF16)
        S.op("pool", lambda: nc.gpsimd.memset(ones[:], 1.0), writes=[b_res])
        S.barrier()

        I = sb("I", [128, T], F32); b_I = Buf("I")
        M = sb("M", [128, T], BF16); b_M = Buf("M")
        MT2 = [sb("MT%d" % i, [128, NT, 128], BF16) for i in range(2)]; b_MT2 = [Buf("MT%d" % i) for i in range(2)]
        jk = sb("jkC", [128, T], BF16); b_jk = Buf("jkC")
        qit = [sb("qit%d" % i, [128, 4, 128], BF16) for i in range(2)]; b_qit = [Buf("qit%d" % i) for i in range(2)]
        qt = [sb("qt%d" % i, [128, 8, 128], BF16) for i in range(2)]; b_qt = [Buf("qt%d" % i) for i in range(2)]
        iw = [sb("iw%d" % i, [128, 8], F32) for i in range(2)]; b_iw = [Buf("iw%d" % i) for i in range(2)]
        sgb = [sb("sgb%d" % i, [128, D], F32) for i in range(2)]; b_sgb = [Buf("sgb%d" % i) for i in range(2)]
        r_ = [sb("r_%d" % i, [128, 512], F32) for i in range(2)]; b_r = [Buf("r_%d" % i) for i in range(2)]
        e_ = [sb("e_%d" % i, [128, 512], BF16) for i in range(2)]; b_e = [Buf("e_%d" % i) for i in range(2)]
        p_ = [sb("p_%d" % i, [128, 512], BF16) for i in range(3)]; b_p = [Buf("p_%d" % i) for i in range(3)]
        rden = sb("rden", [128, 512], F32); b_rden = Buf("rden")
        ot = [sb("ot%d" % i, [128, 4, 128], BF16) for i in range(2)]; b_ot = [Buf("ot%d" % i) for i in range(2)]
        md = sb("md", [128, D], F32); b_md = Buf("md")
        st = sb("stC", [128, 8], F32); b_st = Buf("stC")
        w2 = sb("w2C", [128, NIT + 1], F32); b_w2 = Buf("w2C")
        tmpd = sb("tmpd", [128, 128], F32); b_tmpd = Buf("tmpd")
        midt = sb("midt", [128, 1], F32); b_mid = Buf("midt")
        cn = sb("cnC", [128, 1], F32); b_cn = Buf("cnC")
        sa = sb("saC", [128, 1], F32); b_sa = Buf("saC")
        gt = sb("gtC", [128, 2], F32); b_gt = Buf("gtC")
        jk2 = sb("jk2C", [128, T], BF16); b_jk2 = Buf("jk2C")
        k.ps_base = 5
        k.ps_n = 3
        k.ps_rr = 0
        psOg = [k.PS[3], k.PS[3]]
        bOg = [k.PB[3], k.PB[3]]
        psDg = [k.PS[4], k.PS[4]]
        bDg = [k.PB[4], k.PB[4]]
        att_rr = [0]

        def attps():
            i = att_rr[0] % 3
            att_rr[0] += 1
            return k.PS[i], k.PB[i]
        cnt = {"r": 0, "e": 0}
        SC = 128.0 ** -0.5

        def stage1a(i):
            nk = i + 1
            NK = nk * 128
            par = i % 2
            yield
            S.dma_op("sp", qit[par][:], k.QIT[:, :, i * 128:(i + 1) * 128].rearrange("g p t -> p g t"),
                     writes=[b_qit[par]])
            yield
            S.dma_op("sp", qt[par][:], k.QT[:, :, i * 128:(i + 1) * 128].rearrange("g p t -> p g t"),
                     writes=[b_qt[par]])
            yield
            S.dma_op("sp", iw[par][:], k.IW[i * 128:(i + 1) * 128, :], writes=[b_iw[par]])
            yield
            S.dma_op("sp", sgb[par][:], k.SGB[i * 128:(i + 1) * 128, :], writes=[b_sgb[par]])
            for c0 in range(0, NK, 512):
                n = min(512, NK - c0)
                for h in range(8):
                    g, hf = h // 2, h % 2
                    ps, bp = k.nextps()
                    yield
                    S.op("pe", lambda: nc.tensor.matmul(ps[:, 0:n], lhsT=qit[par][64 * hf:64 * hf + 64, g, :],
                                                        rhs=kit[64 * hf:64 * hf + 64, c0:c0 + n],
                                                        start=True, stop=True),
                         reads=[b_qit[par]], writes=[bp])
                    rp = cnt["r"] % 2
                    cnt["r"] += 1
                    yield
                    S.op("act", lambda: nc.scalar.activation(out=r_[rp][:, 0:n], in_=ps[:, 0:n], func=AF.Relu),
                         reads=[bp], writes=[b_r[rp]])
                    if h == 0:
                        yield
                        S.op("dve", lambda: nc.vector.tensor_scalar(out=I[:, c0:c0 + n], in0=r_[rp][:, 0:n],
                                                                    scalar1=iw[par][:, 0:1], scalar2=None,
                                                                    op0=ALU.mult),
                             reads=[b_r[rp], b_iw[par]], writes=[b_I])
                    else:
                        yield
                        S.op("dve", lambda: nc.vector.scalar_tensor_tensor(
                            out=I[:, c0:c0 + n], in0=r_[rp][:, 0:n], scalar=iw[par][:, h:h + 1],
                            in1=I[:, c0:c0 + n], op0=ALU.mult, op1=ALU.add),
                            reads=[b_r[rp], b_iw[par]], writes=[b_I])
            m0 = 1 if i == 0 else 2
            yield
            S.op("dve", lambda: nc.vector.tensor_tensor(out=I[:, 0:128], in0=I[:, 0:128], in1=msk[:, m0, :],
                                                        op=ALU.add), reads=[b_I], writes=[b_I])
            if i >= 1:
                yield
                S.op("dve", lambda: nc.vector.tensor_tensor(out=I[:, i * 128:NK], in0=I[:, i * 128:NK],
                                                            in1=msk[:, 0, :], op=ALU.add),
                     reads=[b_I], writes=[b_I])
            if i < 2:
                yield
                S.op("dve", lambda: nc.vector.memset(st[:, 6:7], -1e29), writes=[b_st])
            else:
                yield
                S.op("dve", lambda: nc.vector.tensor_reduce(out=st[:, 0:1], in_=I[:, 0:NK], axis=AX.X, op=ALU.max),
                     reads=[b_I], writes=[b_st])
                yield
                S.op("dve", lambda: nc.vector.tensor_reduce(out=st[:, 1:2], in_=I[:, PAD:i * 128], axis=AX.X,
                                                            op=ALU.min), reads=[b_I], writes=[b_st])
                if i == 2:
                    yield
                    S.op("dve", lambda: nc.vector.scalar_tensor_tensor(out=tmpd[:], in0=msk[:, 0, :], scalar=-2.0,
                                                                       in1=I[:, i * 128:NK], op0=ALU.mult,
                                                                       op1=ALU.add),
                         reads=[b_I], writes=[b_tmpd])
                    yield
                    S.op("dve", lambda: nc.vector.tensor_reduce(out=st[:, 7:8], in_=tmpd[:], axis=AX.X, op=ALU.min),
                         reads=[b_tmpd], writes=[b_st])
                    yield
                    S.op("dve", lambda: nc.vector.tensor_tensor(out=st[:, 1:2], in0=st[:, 1:2], in1=st[:, 7:8],
                                                                op=ALU.min), reads=[b_st], writes=[b_st])
                yield
                S.op("dve", lambda: nc.vector.tensor_tensor(out=st[:, 2:3], in0=st[:, 0:1], in1=st[:, 1:2],
                                                            op=ALU.subtract), reads=[b_st], writes=[b_st])
                yield
                S.op("dve", lambda: nc.vector.tensor_scalar(out=w2[:], in0=p2[:], scalar1=st[:, 2:3], scalar2=None,
                                                            op0=ALU.mult), reads=[b_st], writes=[b_w2])
                yield
                S.op("dve", lambda: nc.vector.tensor_tensor(out=st[:, 3:4], in0=st[:, 1:2], in1=w2[:, 0:1],
                                                            op=ALU.add), reads=[b_st, b_w2], writes=[b_st])
                hc = NK if nk <= 3 else 128 * max(1, int(round(0.45 * nk)))
                na = NK - hc
                yield
                S.op("dve", lambda: nc.vector.tensor_copy(midt[:], st[:, 3:4]), reads=[b_st], writes=[b_mid])
                for it in range(NIT):
                    if na > 0:
                        yield
                        S.op("act", lambda: nc.scalar.activation(out=jk2[:, 0:na], in_=I[:, hc:NK], func=AF.Sign,
                                                                 scale=-1.0, bias=midt[:, 0:1], accum_out=sa[:, 0:1]),
                             reads=[b_I, b_mid], writes=[b_jk2, b_sa])
                    yield
                    S.op("dve", lambda: nc.vector.tensor_scalar(out=jk[:, 0:hc], in0=I[:, 0:hc], scalar1=midt[:, 0:1],
                                                                scalar2=None, op0=ALU.is_ge, op1=ALU.add,
                                                                accum_out=cn[:, 0:1]),
                         reads=[b_I, b_mid], writes=[b_jk, b_cn])
                    if na > 0:
                        yield
                        S.op("dve", lambda: nc.vector.scalar_tensor_tensor(out=gt[:, 0:1], in0=sa[:, 0:1], scalar=-0.5,
                                                                           in1=cn[:, 0:1], op0=ALU.mult, op1=ALU.add),
                             reads=[b_sa, b_cn], writes=[b_gt])
                        src, bsrc = gt[:, 0:1], b_gt
                    else:
                        src, bsrc = cn[:, 0:1], b_cn
                    yield
                    S.op("dve", lambda: nc.vector.tensor_scalar(out=gt[:, 1:2], in0=src, scalar1=255.5 - 0.5 * na,
                                                                scalar2=-0.5, op0=ALU.is_ge, op1=ALU.add),
                         reads=[bsrc], writes=[b_gt])
                    yield
                    S.op("dve", lambda: nc.vector.scalar_tensor_tensor(out=midt[:, 0:1], in0=gt[:, 1:2],
                                                                       scalar=w2[:, it:it + 1], in1=midt[:, 0:1],
                                                                       op0=ALU.mult, op1=ALU.add),
                         reads=[b_gt, b_w2, b_mid], writes=[b_mid])
                yield
                S.op("dve", lambda: nc.vector.tensor_copy(st[:, 3:4], midt[:]), reads=[b_mid], writes=[b_st])
                yield
                S.op("dve", lambda: nc.vector.tensor_scalar(out=st[:, 6:7], in0=st[:, 3:4], scalar1=w2[:, NIT:NIT + 1],
                                                            scalar2=-1e29, op0=ALU.subtract, op1=ALU.max),
                     reads=[b_st, b_w2], writes=[b_st])
            yield
            S.op("dve", lambda: nc.vector.tensor_scalar(out=M[:, 0:NK], in0=I[:, 0:NK], scalar1=st[:, 6:7],
                                                        scalar2=None, op0=ALU.is_ge),
                 reads=[b_I, b_st], writes=[b_M])
        def stage1b(i):
            nk = i + 1
            MT, b_MT = MT2[i % 2], b_MT2[i % 2]
            for j0 in range(0, nk, 8):
                nj = min(8, nk - j0)
                ps, bp = k.nextps()
                psb = ps[:].bitcast(BF16).rearrange("p (k t) -> p k t", k=8)
                fns = [(lambda jj=jj: nc.tensor.transpose(psb[:, jj, :], M[:, (j0 + jj) * 128:(j0 + jj + 1) * 128],
                                                          k.identb[:])) for jj in range(nj)]
                yield
                S.group("pe", fns, reads=[b_M, k.b_ident], writes=[bp])
                yield
                S.op("act", lambda: nc.scalar.activation(out=MT[:, j0:j0 + nj, :], in_=psb[:, 0:nj, :], func=AF.Copy,
                                                         scale=30000.0, bias=-30000.0),
                     reads=[bp], writes=[b_MT])
        def stage2(i):
            nk = i + 1
            par = i % 2
            MT, b_MT = MT2[i % 2], b_MT2[i % 2]
            steps = [(g, j) for g in range(2) for j in range(nk)]

            def emitS(g, j):
                ps, bp = attps()
                S.group("pe", [
                    lambda: nc.tensor.matmul(ps[:], lhsT=kts[:, g, j * 128:(j + 1) * 128],
                                             rhs=qt[par][:, 4 * g:4 * g + 4, :], start=True, stop=False),
                    lambda: nc.tensor.matmul(ps[:], lhsT=k.identb[:],
                                             rhs=MT[:, j:j + 1, :].broadcast_to([128, 4, 128]),
                                             start=False, stop=True)],
                    reads=[b_qt[par], b_MT, k.b_ident], writes=[bp])
                return ps, bp
            pend = []
            for sidx in range(min(2, len(steps))):
                yield
                pend.append(emitS(*steps[sidx]))
            for sidx, (g, j) in enumerate(steps):
                ps, bp = pend.pop(0)
                ep = cnt["e"] % 3
                cnt["e"] += 1
                yield
                S.op("act", lambda: nc.scalar.activation(out=p_[ep][:], in_=ps[:], func=AF.Exp, scale=SC),
                     reads=[bp], writes=[b_p[ep]])
                if sidx + 2 < len(steps):
                    yield
                    pend.append(emitS(*steps[sidx + 2]))
                yield
                S.op("pe", lambda: nc.tensor.matmul(psOg[g][:], lhsT=vs[:, j, g * 128:(g + 1) * 128], rhs=p_[ep][:],
                                                    start=(j == 0), stop=(j == nk - 1)),
                     reads=[b_p[ep]], writes=[bOg[g]])
                yield
                S.op("pe", lambda: nc.tensor.matmul(psDg[g][:], lhsT=ones[:], rhs=p_[ep][:],
                                                    start=(j == 0), stop=(j == nk - 1)),
                     reads=[b_p[ep]], writes=[bDg[g]])
                if j == nk - 1:
                    yield
                    S.op("dve", lambda: nc.vector.tensor_scalar(out=rden[:], in0=psDg[g][:], scalar1=1e-20,
                                                                scalar2=None, op0=ALU.max),
                         reads=[bDg[g]], writes=[b_rden])
                    yield
                    S.op("dve", lambda: nc.vector.reciprocal(rden[:], rden[:]), reads=[b_rden], writes=[b_rden])
                    yield
                    S.op("dve", lambda: nc.vector.tensor_tensor(out=ot[g][:].rearrange("p h t -> p (h t)"),
                                                                in0=psOg[g][:], in1=rden[:], op=ALU.mult),
                         reads=[bOg[g], b_rden], writes=[b_ot[g]])
            for cb in range(2):
                ps, bp = attps()
                fns = [(lambda h=h: nc.tensor.matmul(ps[:], lhsT=ot[h // 4][:, h % 4, :],
                                                     rhs=wd[:, h, cb * 512:(cb + 1) * 512],
                                                     start=(h == 0), stop=(h == 7))) for h in range(8)]
                yield
                S.group("pe", fns, reads=[b_ot[0], b_ot[1]], writes=[bp])
                yield
                S.op("dve", lambda: nc.vector.tensor_tensor(out=md[:, cb * 512:(cb + 1) * 512], in0=ps[:],
                                                            in1=sgb[par][:, cb * 512:(cb + 1) * 512], op=ALU.mult),
                     reads=[bp, b_sgb[par]], writes=[b_md])
            yield
            S.dma_op("sp", k.MDSA[i * 128:(i + 1) * 128, :], md[:], reads=[b_md])

        def drain(g):
            for _ in g:
                pass

        def chain(*gs):
            for g in gs:
                yield from g

        def lockstep(gA, nA, gB, nB):
            aA = aB = True
            pA = pB = 0.0
            while aA or aB:
                if aA and (not aB or pA <= pB):
                    try:
                        next(gA)
                        pA += 1.0 / nA
                    except StopIteration:
                        aA = False
                else:
                    try:
                        next(gB)
                        pB += 1.0 / nB
                    except StopIteration:
                        aB = False

        def est1(i):
            nk = i + 1
            return 4 + ((nk + 3) // 4) * 24 + (NIT * 5 + 12 if i >= 2 else 3) + 2 * ((nk + 7) // 8)

        def est2(i):
            return 8 * (i + 1) + 12

        drain(chain(stage1a(0), stage1b(0)))
        for i in range(NT):
            if i + 1 < NT:
                lockstep(stage2(i), est2(i), chain(stage1a(i + 1), stage1b(i + 1)), est1(i + 1))
            else:
                drain(stage2(i))
        k.ps_base = 0
        k.ps_n = 8
        S.barrier()


def phase_B(k):
    nc, S = k.nc, k.S
    with ExitStack() as es:
        def sb(name, shape, dt):
            return es.enter_context(nc.sbuf_tensor(name, list(shape), dt))
        b_c = Buf("constB")
        gm = sb("gm", [128, 4, 128], F32)
        S.dma_op("sp", gm[:], k.gmasks.rearrange("m p c -> p m c"), writes=[b_c])
        cmask = sb("cmask_sb", [128, 2], F32)
        S.dma_op("sp", cmask[:], k.cmask, writes=[b_c])
        onesf = sb("onesf", [128, 128], F32)
        S.op("pool", lambda: nc.gpsimd.memset(onesf[:], 1.0), writes=[b_c])
        nhalf = sb("nhalfB", [128, 8], F32)
        S.op("pool", lambda: nc.gpsimd.memset(nhalf[:], -0.5), writes=[b_c])
        wg = sb("wg_sb", [128, 8, D], BF16)
        wo = sb("wo_sb", [128, 8, D], BF16)
        S.dma_op("pool", wg[:], k.wg.rearrange("(h p) n -> p h n", p=128), writes=[b_c])
        S.dma_op("pool", wo[:], k.wo.rearrange("(h p) n -> p h n", p=128), writes=[b_c])
        gn = sb("gn", [128, 128], F32)
        S.dma_op("sp", gn[:], k.gdn_norm.partition_broadcast(128), writes=[b_c])
        g2 = sb("g2", [128, D], F32)
        S.dma_op("sp", g2[:], k.norms[1:2, :].partition_broadcast(128), writes=[b_c])
        Sst = sb("Sst", [128, 8, 128], F32); b_S = Buf("Sst")
        Sbf = sb("Sbf", [128, 8, 128], BF16); b_Sbf = Buf("Sbf")
        S.op("pool", lambda: nc.gpsimd.memset(Sst[:], 0.0), writes=[b_S])
        S.op("pool", lambda: nc.gpsimd.memset(Sbf[:], 0.0), writes=[b_Sbf])
        S.barrier()

        def mk(name, shape, dt, n=1):
            ts = [sb("%s%d" % (name, i), shape, dt) for i in range(n)]
            bs = [Buf("%s%d" % (name, i)) for i in range(n)]
            return (ts, bs) if n > 1 else (ts[0], bs[0])
        qg, b_qg = mk("qgB", [128, 8, 128], BF16)
        kg, b_kg = mk("kgB", [128, 8, 128], BF16)
        vg, b_vg = mk("vgB", [128, 8, 128], BF16)
        zs2, b_zs2 = mk("zsB", [128, 8, 128], F32, 2)
        bg, b_bg = mk("bgB", [128, 16], F32)
        sga2, b_sga2 = mk("sgaB", [128, D], F32, 2)
        mdsa2, b_mdsa2 = mk("mdsaB", [128, D], F32, 2)
        hx2, b_hx2 = mk("hxB", [128, D], F32, 2)
        sc, b_sc = mk("scB", [128, 8, 8], F32)
        glb2, b_glb2 = mk("glb", [128, 2, 8], F32, 2)
        Bu, b_Bu = mk("Bu", [128, 8, 128], F32)
        B2, b_B2 = mk("B2", [128, 2, 8], F32)
        kb, b_kb = mk("kbB", [128, 8, 128], BF16)
        qd, b_qd = mk("qdB", [128, 8, 128], BF16)
        kbg, b_kbg = mk("kbgB", [128, 8, 128], BF16)
        ktl2, b_ktl2 = mk("ktlB", [128, 8, 128], BF16, 2)
        vb, b_vb = mk("vbB", [128, 8, 128], BF16)
        kT, b_kT = mk("kTB", [128, 8, 128], BF16)
        kbT, b_kbT = mk("kbTB", [128, 8, 128], BF16)
        qT, b_qT = mk("qTB", [128, 8, 128], BF16)
        qdT2, b_qdT2 = mk("qdTB", [128, 8, 128], BF16, 2)
        D1, b_D1 = mk("D1", [128, 8, 128], F32)
        Em, b_Em = mk("Em", [128, 8, 128], F32)
        ETm, b_ETm = mk("ETm", [128, 8, 128], F32)
        tA, b_tA = mk("tA", [128, 8, 128], F32)
        Bm, b_Bm = mk("Bm", [128, 8, 128], BF16, 2)
        Cm, b_Cm = mk("Cm", [128, 8, 128], BF16, 2)
        Ym, b_Ym = mk("Ym", [128, 8, 128], BF16, 2)
        Yf, b_Yf = mk("Yf", [128, 8, 128], F32, 2)
        aT2, b_aT2 = mk("aT", [128, 8, 128], BF16, 2)
        uS2, b_uS2 = mk("uS", [128, 8, 128], F32, 2)
        wT2, b_wT2 = mk("wT", [128, 8, 128], BF16, 2)
        vn, b_vn = mk("vn", [128, 8, 128], BF16)
        osb, b_osb = mk("osb", [128, 8, 128], F32)
        st8, b_st8 = mk("st8", [128, 24], F32)
        sqt, b_sqt = mk("sqtB", [128, 8, 128], F32)
        yb, b_yb = mk("ybB", [128, 8, 128], BF16)
        yT, b_yT = mk("yTB", [128, 8, 128], BF16)
        mg, b_mg = mk("mgB", [128, D], F32)
        mgb, b_mgb = mk("mgbB", [128, D], BF16)
        mT, b_mT = mk("mTB", [128, 8, 128], BF16)
        mx, b_mx = mk("mxB", [128, D], F32)
        jb, b_jb = mk("jbB", [128, D], BF16)

        def bc_h(ap2):
            return ap2.unsqueeze(2).broadcast_to([128, 8, 128])

        def bc_m(ap2):
            return ap2.unsqueeze(1).broadcast_to([128, 8, 128])

        pool_rr = {"par": 0, "seq": 0}
        cur_pool = ["par"]

        class PT(tuple):
            pass

        def two_banks():
            if cur_pool[0] == "par":
                m = pool_rr["par"] % 2
                pool_rr["par"] += 1
            else:
                m = 2 + pool_rr["seq"] % 2
                pool_rr["seq"] += 1
            pss = PT((k.PS[2 * m], k.PS[2 * m + 1]))
            pss.big = k.PSB[m]
            return pss, (k.PB[2 * m], k.PB[2 * m + 1])

        def nextps():
            pss, bps = two_banks()
            return pss[0], bps[0]

        def pv(ps, hh):
            return ps[hh // 4][:, (hh % 4) * 128:(hh % 4 + 1) * 128]

        def mm8(lhs_fn, rhs_fn, reads):
            pss, bps = two_banks()
            fns = [(lambda hh=hh: nc.tensor.matmul(pv(pss, hh), lhsT=lhs_fn(hh), rhs=rhs_fn(hh),
                                                   start=True, stop=True)) for hh in range(8)]
            S.group("pe", fns, reads=reads, writes=[bps[0], bps[1]])
            return pss, bps

        def ev8(eng, fn, pss, bps, reads, writes):
            S.op(eng, (lambda: fn(pss.big[:].rearrange("p (h t) -> p h t", h=8), slice(0, 8))),
                 reads=[bps[0], bps[1]] + reads, writes=writes)

        def tr8(src, b_src, dst, b_dst):
            ps, bp = nextps()
            psb = ps[:].bitcast(BF16).rearrange("p (k t) -> p k t", k=8)
            fns = [(lambda hh=hh: nc.tensor.transpose(psb[:, hh, :], src[:, hh, :], k.identb[:])) for hh in range(8)]
            S.group("pe", fns, reads=[b_src, k.b_ident], writes=[bp])
            S.op("act", lambda: nc.scalar.copy(out=dst[:], in_=psb), reads=[bp], writes=[b_dst])

        def tile_par(i):
            qdT, b_qdT = qdT2[i % 2], b_qdT2[i % 2]
            aT, b_aT = aT2[i % 2], b_aT2[i % 2]
            uS, b_uS = uS2[i % 2], b_uS2[i % 2]
            wT, b_wT = wT2[i % 2], b_wT2[i % 2]
            ktl, b_ktl = ktl2[i % 2], b_ktl2[i % 2]
            glb, b_glb = glb2[i % 2], b_glb2[i % 2]
            zs, b_zs = zs2[i % 2], b_zs2[i % 2]
            sga, b_sga = sga2[i % 2], b_sga2[i % 2]
            mdsa, b_mdsa = mdsa2[i % 2], b_mdsa2[i % 2]
            hx, b_hx = hx2[i % 2], b_hx2[i % 2]
            r0, r1 = i * 128, (i + 1) * 128
            yield
            S.dma_op("sp", qg[:], k.QG[r0:r1, :].rearrange("p (h d) -> p h d", h=8), writes=[b_qg])
            yield
            S.dma_op("sp", kg[:], k.KG[r0:r1, :].rearrange("p (h d) -> p h d", h=8), writes=[b_kg])
            yield
            S.dma_op("sp", vg[:], k.VG[r0:r1, :].rearrange("p (h d) -> p h d", h=8), writes=[b_vg])
            yield
            S.dma_op("sp", zs[:], k.ZS[r0:r1, :].rearrange("p (h d) -> p h d", h=8), writes=[b_zs])
            yield
            S.dma_op("sp", bg[:], k.BG[r0:r1, :], writes=[b_bg])
            yield
            S.dma_op("sp", sga[:], k.SGA[r0:r1, :], writes=[b_sga])
            yield
            S.dma_op("sp", mdsa[:], k.MDSA[r0:r1, :], writes=[b_mdsa])
            if i == 0:
                yield
                S.op("pool", lambda: nc.gpsimd.memset(hx[:], 0.0), writes=[b_hx])
                yield
                S.dma_op("sp", hx[PAD:128, :], k.meta, writes=[b_hx])
            else:
                yield
                S.dma_op("sp", hx[:], k.x[r0 - 128:r1 - 128, :], writes=[b_hx])
            beta = bg[:, 0:8]
            gg = bg[:, 8:16]
            yield
            ps, bp = nextps()
            yield
            S.op("pe", lambda: nc.tensor.matmul(ps[:, 0:8], lhsT=gm[:, 0, :], rhs=gg, start=True, stop=True),
                 reads=[b_bg, b_c], writes=[bp])
            yield
            S.op("pe", lambda: nc.tensor.matmul(ps[:, 8:16], lhsT=gm[:, 1, :], rhs=gg, start=True, stop=True),
                 reads=[b_bg, b_c], writes=[bp])
            yield
            S.op("dve", lambda: nc.vector.tensor_copy(sc[:, 0:2, :], ps[:, 0:16].rearrange("p (a h) -> p a h", a=2)),
                 reads=[bp], writes=[b_sc])
            yield
            S.op("act", lambda: nc.scalar.activation(out=sc[:, 2, :], in_=sc[:, 0, :], func=AF.Exp),
                 reads=[b_sc], writes=[b_sc])
            yield
            S.op("dve", lambda: nc.vector.tensor_tensor(out=sc[:, 3, :], in0=sc[:, 2, :], in1=beta, op=ALU.mult),
                 reads=[b_sc, b_bg], writes=[b_sc])
            yield
            S.op("dve", lambda: nc.vector.tensor_tensor(out=sc[:, 5, :], in0=sc[:, 1, :], in1=sc[:, 0, :],
                                                        op=ALU.subtract), reads=[b_sc], writes=[b_sc])
            yield
            S.op("act", lambda: nc.scalar.activation(out=sc[:, 4, :], in_=sc[:, 5, :], func=AF.Exp),
                 reads=[b_sc], writes=[b_sc])
            yield
            S.op("dve", lambda: nc.vector.tensor_tensor(out=B2[:], in0=gg.unsqueeze(1).broadcast_to([128, 2, 8]),
                                                        in1=cmask[:].unsqueeze(2).broadcast_to([128, 2, 8]),
                                                        op=ALU.mult), reads=[b_bg, b_c], writes=[b_B2])
            yield
            ps, bp = nextps()
            yield
            S.op("pe", lambda: nc.tensor.matmul(ps[:, 0:16], lhsT=onesf[:], rhs=B2[:].rearrange("p c h -> p (c h)"),
                                                start=True, stop=True), reads=[b_B2, b_c], writes=[bp])
            yield
            S.op("act", lambda: nc.scalar.activation(out=glb[:].rearrange("p c h -> p (c h)"), in_=ps[:, 0:16],
                                                     func=AF.Exp), reads=[bp], writes=[b_glb])
            yield
            S.op("dve", lambda: nc.vector.tensor_tensor(out=Bu[:], in0=bc_m(gm[:, 0, :]), in1=bc_h(gg), op=ALU.mult),
                 reads=[b_bg, b_c], writes=[b_Bu])
            pss, bps = two_banks()
            yield
            for half in range(2):
                yield
                S.op("pe", (lambda half=half: nc.tensor.matmul(
                    pss[half][:], lhsT=onesf[:], rhs=Bu[:, 4 * half:4 * half + 4, :].rearrange("p h t -> p (h t)"),
                    start=True, stop=True)), reads=[b_Bu, b_c], writes=[bps[half]])
            yield
            ev8("dve", lambda psv, sl: nc.vector.tensor_tensor(
                out=D1[:, sl, :], in0=psv,
                in1=sc[:, 0, :].unsqueeze(2).broadcast_to([128, 8, 128]), op=ALU.subtract),
                pss, bps, [b_sc], [b_D1])
            yield
            S.op("dve", lambda: nc.vector.tensor_scalar(out=Em[:], in0=D1[:], scalar1=0.0, scalar2=None, op0=ALU.max),
                 reads=[b_D1], writes=[b_Em])
            yield
            S.op("act", lambda: nc.scalar.activation(out=Em[:], in_=Em[:], func=AF.Exp, scale=-1.0),
                 reads=[b_Em], writes=[b_Em])
            yield
            S.op("dve", lambda: nc.vector.tensor_scalar(out=ETm[:], in0=D1[:], scalar1=0.0, scalar2=None, op0=ALU.min),
                 reads=[b_D1], writes=[b_ETm])
            yield
            S.op("act", lambda: nc.scalar.activation(out=ETm[:], in_=ETm[:], func=AF.Exp),
                 reads=[b_ETm], writes=[b_ETm])
            yield
            S.op("pool", lambda: nc.gpsimd.tensor_tensor(out=kb[:], in0=kg[:], in1=bc_h(beta), op=ALU.mult),
                 reads=[b_kg, b_bg], writes=[b_kb])
            yield
            S.op("pool", lambda: nc.gpsimd.tensor_tensor(out=qd[:], in0=qg[:], in1=bc_h(sc[:, 2, :]), op=ALU.mult),
                 reads=[b_qg, b_sc], writes=[b_qd])
            yield
            S.op("pool", lambda: nc.gpsimd.tensor_tensor(out=kbg[:], in0=kg[:], in1=bc_h(sc[:, 3, :]), op=ALU.mult),
                 reads=[b_kg, b_sc], writes=[b_kbg])
            yield
            S.op("pool", lambda: nc.gpsimd.tensor_tensor(out=ktl[:], in0=kg[:], in1=bc_h(sc[:, 4, :]), op=ALU.mult),
                 reads=[b_kg, b_sc], writes=[b_ktl])
            yield
            S.op("pool", lambda: nc.gpsimd.tensor_tensor(out=vb[:], in0=vg[:], in1=bc_h(beta), op=ALU.mult),
                 reads=[b_vg, b_bg], writes=[b_vb])
            yield
            tr8(kg, b_kg, kT, b_kT)
            yield
            tr8(kb, b_kb, kbT, b_kbT)
            yield
            tr8(qg, b_qg, qT, b_qT)
            yield
            tr8(qd, b_qd, qdT, b_qdT)
            yield
            S.op("pool", lambda: nc.gpsimd.tensor_tensor(out=tA[:], in0=Em[:], in1=bc_m(gm[:, 2, :]), op=ALU.mult),
                 reads=[b_Em, b_c], writes=[b_tA])
            yield
            pss, bps = mm8(lambda hh: kbT[:, hh, :], lambda hh: kT[:, hh, :], [b_kbT, b_kT])
            yield
            ev8("dve", lambda psv, sl: nc.vector.tensor_tensor(out=Bm[0][:, sl, :], in0=psv,
                                                                  in1=tA[:, sl, :], op=ALU.mult),
                pss, bps, [b_tA], [b_Bm[0]])
            yield
            S.op("pool", lambda: nc.gpsimd.tensor_tensor(out=tA[:], in0=ETm[:], in1=bc_m(gm[:, 3, :]), op=ALU.mult),
                 reads=[b_ETm, b_c], writes=[b_tA])
            yield
            pss, bps = mm8(lambda hh: kT[:, hh, :], lambda hh: kbT[:, hh, :], [b_kbT, b_kT])
            yield
            ev8("dve", lambda psv, sl: nc.vector.tensor_tensor(out=Cm[0][:, sl, :], in0=psv,
                                                                  in1=tA[:, sl, :], op=ALU.mult),
                pss, bps, [b_tA], [b_Cm[0]])
            yield
            S.op("pool", lambda: nc.gpsimd.tensor_tensor(out=tA[:], in0=ETm[:], in1=bc_m(gm[:, 0, :]), op=ALU.mult),
                 reads=[b_ETm, b_c], writes=[b_tA])
            yield
            pss, bps = mm8(lambda hh: kT[:, hh, :], lambda hh: qT[:, hh, :], [b_qT, b_kT])
            yield
            ev8("dve", lambda psv, sl: nc.vector.tensor_tensor(out=aT[:, sl, :], in0=psv,
                                                                  in1=tA[:, sl, :], op=ALU.mult),
                pss, bps, [b_tA], [b_aT])
            yield
            S.op("dve", lambda: nc.vector.tensor_tensor(out=Yf[0][:], in0=Cm[0][:], in1=bc_m(k.identf[:]), op=ALU.add),
                 reads=[b_Cm[0], k.b_ident], writes=[b_Yf[0]])
            yield
            S.op("act", lambda: nc.scalar.copy(out=Ym[0][:], in_=Yf[0][:]), reads=[b_Yf[0]], writes=[b_Ym[0]])
            cur = 0
            yield
            for lev in range(1, 6):
                nx = 1 - cur
                yield
                pss, bps = mm8(lambda hh: Cm[cur][:, hh, :], lambda hh: Bm[cur][:, hh, :], [b_Cm[cur], b_Bm[cur]])
                yield
                ev8("act", lambda psv, sl: nc.scalar.copy(out=Bm[nx][:, sl, :], in_=psv),
                    pss, bps, [], [b_Bm[nx]])
                if lev < 5:
                    yield
                    pss, bps = mm8(lambda hh: Bm[cur][:, hh, :], lambda hh: Cm[cur][:, hh, :], [b_Cm[cur], b_Bm[cur]])
                    yield
                    ev8("act", lambda psv, sl: nc.scalar.copy(out=Cm[nx][:, sl, :], in_=psv),
                        pss, bps, [], [b_Cm[nx]])
                yield
                pss, bps = mm8(lambda hh: Bm[nx][:, hh, :], lambda hh: Ym[cur][:, hh, :], [b_Bm[nx], b_Ym[cur]])
                yield
                ev8("dve", lambda psv, sl: nc.vector.tensor_tensor(out=Yf[nx][:, sl, :], in0=psv,
                                                                      in1=Yf[cur][:, sl, :],
                                                                      op=ALU.add),
                    pss, bps, [b_Yf[cur]], [b_Yf[nx]])
                yield
                S.op("act", lambda: nc.scalar.copy(out=Ym[nx][:], in_=Yf[nx][:]), reads=[b_Yf[nx]], writes=[b_Ym[nx]])
                cur = nx
            Y = Ym[cur]
            bY = b_Ym[cur]
            yield
            pss, bps = mm8(lambda hh: Y[:, hh, :], lambda hh: vb[:, hh, :], [bY, b_vb])
            yield
            ev8("act", lambda psv, sl: nc.scalar.copy(out=uS[:, sl, :], in_=psv),
                pss, bps, [], [b_uS])
            yield
            pss, bps = mm8(lambda hh: kbg[:, hh, :], lambda hh: Y[:, hh, :], [bY, b_kbg])
            yield
            ev8("act", lambda psv, sl: nc.scalar.copy(out=wT[:, sl, :], in_=psv),
                pss, bps, [], [b_wT])

        def tile_seq(i):
            r0, r1 = i * 128, (i + 1) * 128
            qdT, b_qdT = qdT2[i % 2], b_qdT2[i % 2]
            aT, b_aT = aT2[i % 2], b_aT2[i % 2]
            uS, b_uS = uS2[i % 2], b_uS2[i % 2]
            wT, b_wT = wT2[i % 2], b_wT2[i % 2]
            ktl, b_ktl = ktl2[i % 2], b_ktl2[i % 2]
            glb, b_glb = glb2[i % 2], b_glb2[i % 2]
            zs, b_zs = zs2[i % 2], b_zs2[i % 2]
            sga, b_sga = sga2[i % 2], b_sga2[i % 2]
            mdsa, b_mdsa = mdsa2[i % 2], b_mdsa2[i % 2]
            hx, b_hx = hx2[i % 2], b_hx2[i % 2]
            yield
            for c in range(2):
                c0, c1 = 64 * c, 64 * c + 64
                pss, bps = two_banks()
                fns = [(lambda hh=hh: nc.tensor.matmul(pv(pss, hh)[c0:c1, :], lhsT=wT[:, hh, c0:c1],
                                                       rhs=Sbf[:, hh, :], start=True, stop=True))
                       for hh in range(8)]
                yield
                S.group("pe", fns, reads=[b_wT, b_Sbf], writes=[bps[0], bps[1]])
                yield
                S.op("dve", lambda: nc.vector.tensor_tensor(
                    out=vn[c0:c1, :, :], in0=uS[c0:c1, :, :],
                    in1=pss.big[c0:c1, :].rearrange("p (h t) -> p h t", h=8), op=ALU.subtract),
                    reads=[bps[0], bps[1], b_uS], writes=[b_vn])
                pso, bpo = two_banks()
                fns = []
                for hh in range(8):
                    fns.append(lambda hh=hh: nc.tensor.matmul(pv(pso, hh)[c0:c1, :], lhsT=qdT[:, hh, c0:c1],
                                                              rhs=Sbf[:, hh, :], start=True, stop=False))
                    fns.append(lambda hh=hh: nc.tensor.matmul(pv(pso, hh)[c0:c1, :], lhsT=aT[c0:c1, hh, c0:c1],
                                                              rhs=vn[c0:c1, hh, :], start=False, stop=True))
                yield
                S.group("pe", fns, reads=[b_qdT, b_Sbf, b_aT, b_vn], writes=[bpo[0], bpo[1]])
                yield
                S.op("act", lambda: nc.scalar.copy(
                    out=osb[c0:c1, :, :], in_=pso.big[c0:c1, :].rearrange("p (h t) -> p h t", h=8)),
                    reads=[bpo[0], bpo[1]], writes=[b_osb])
                pss2, bps2 = two_banks()
                fns = [(lambda hh=hh: nc.tensor.matmul(pv(pss2, hh), lhsT=ktl[c0:c1, hh, :], rhs=vn[c0:c1, hh, :],
                                                       start=True, stop=True)) for hh in range(8)]
                yield
                S.group("pe", fns, reads=[b_ktl, b_vn], writes=[bps2[0], bps2[1]])
                yield
                S.op("pool", lambda: nc.gpsimd.tensor_tensor(out=Sst[:], in0=Sst[:], in1=bc_h(glb[:, c, :]),
                                                             op=ALU.mult), reads=[b_S, b_glb], writes=[b_S])
                yield
                S.op("dve", lambda: nc.vector.tensor_tensor(
                    out=Sst[:], in0=Sst[:], in1=pss2.big[:].rearrange("p (h t) -> p h t", h=8), op=ALU.add),
                    reads=[bps2[0], bps2[1], b_S], writes=[b_S])
                yield
                S.op("act", lambda: nc.scalar.copy(out=Sbf[:], in_=Sst[:]), reads=[b_S], writes=[b_Sbf])
            yield
            S.op("dve", lambda: nc.vector.tensor_tensor(out=sqt[:], in0=osb[:], in1=osb[:], op=ALU.mult),
                 reads=[b_osb], writes=[b_sqt])
            yield
            S.op("dve", lambda: nc.vector.tensor_reduce(out=st8[:, 0:8], in_=sqt[:], axis=AX.X, op=ALU.add),
                 reads=[b_sqt], writes=[b_st8])
            yield
            S.op("pool", lambda: nc.gpsimd.tensor_scalar(out=st8[:, 8:16], in0=st8[:, 0:8], scalar1=1.0 / 128,
                                                         scalar2=EPS, op0=ALU.mult, op1=ALU.add),
                 reads=[b_st8], writes=[b_st8])
            yield
            S.op("pool", lambda: nc.gpsimd.tensor_tensor(out=st8[:, 16:24], in0=st8[:, 8:16], in1=nhalf[:], op=ALU.pow),
                 reads=[b_st8, b_c], writes=[b_st8])
            yield
            S.op("pool", lambda: nc.gpsimd.tensor_tensor(out=zs[:], in0=zs[:], in1=bc_m(gn[:]), op=ALU.mult),
                 reads=[b_zs, b_c], writes=[b_zs])
            yield
            S.op("dve", lambda: nc.vector.tensor_tensor(out=osb[:], in0=osb[:], in1=bc_h(st8[:, 16:24]), op=ALU.mult),
                 reads=[b_osb, b_st8], writes=[b_osb])
            yield
            S.op("dve", lambda: nc.vector.tensor_tensor(out=yb[:], in0=osb[:], in1=zs[:], op=ALU.mult),
                 reads=[b_osb, b_zs], writes=[b_yb])
            if k.debug:
                yield
                S.dma_op("sp", k.YGDN[r0:r1, :].rearrange("p (h d) -> p h d", h=8), yb[:], reads=[b_yb])
            yield
            tr8(yb, b_yb, yT, b_yT)
            yield
            for cb in range(2):
                ps, bp = nextps()
                fns = [(lambda h=h: nc.tensor.matmul(ps[:], lhsT=yT[:, h, :], rhs=wg[:, h, cb * 512:(cb + 1) * 512],
                                                     start=(h == 0), stop=(h == 7))) for h in range(8)]
                yield
                S.group("pe", fns, reads=[b_yT, b_c], writes=[bp])
                yield
                S.op("dve", lambda: nc.vector.tensor_tensor(out=mg[:, cb * 512:(cb + 1) * 512], in0=ps[:],
                                                            in1=sga[:, cb * 512:(cb + 1) * 512], op=ALU.mult),
                     reads=[bp, b_sga], writes=[b_mg])
            yield
            S.op("pool", lambda: nc.gpsimd.tensor_tensor(out=mgb[:], in0=mg[:], in1=mdsa[:], op=ALU.add),
                 reads=[b_mg, b_mdsa], writes=[b_mgb])
            yield
            tr8(mgb[:].rearrange("p (h d) -> p h d", h=8), b_mgb, mT, b_mT)
            yield
            for cb in range(2):
                ps, bp = nextps()
                fns = [(lambda h=h: nc.tensor.matmul(ps[:], lhsT=mT[:, h, :], rhs=wo[:, h, cb * 512:(cb + 1) * 512],
                                                     start=(h == 0), stop=(h == 7))) for h in range(8)]
                yield
                S.group("pe", fns, reads=[b_mT, b_c], writes=[bp])
                yield
                S.op("act", lambda: nc.scalar.copy(out=mx[:, cb * 512:(cb + 1) * 512], in_=ps[:]),
                     reads=[bp], writes=[b_mx])
            yield
            S.op("act", lambda: nc.scalar.activation(out=jb[:], in_=mx[:], func=AF.Square, accum_out=st8[:, 0:1]),
                 reads=[b_mx], writes=[b_jb, b_st8])
            yield
            S.op("pool", lambda: nc.gpsimd.tensor_scalar(out=st8[:, 8:9], in0=st8[:, 0:1], scalar1=1.0 / D,
                                                         scalar2=EPS, op0=ALU.mult, op1=ALU.add),
                 reads=[b_st8], writes=[b_st8])
            yield
            S.op("pool", lambda: nc.gpsimd.tensor_tensor(out=st8[:, 16:17], in0=st8[:, 8:9], in1=nhalf[:, 0:1],
                                                         op=ALU.pow), reads=[b_st8, b_c], writes=[b_st8])
            yield
            S.op("dve", lambda: nc.vector.scalar_tensor_tensor(out=mx[:], in0=mx[:], scalar=st8[:, 16:17], in1=g2[:],
                                                               op0=ALU.mult, op1=ALU.mult),
                 reads=[b_mx, b_st8, b_c], writes=[b_mx])
            yield
            S.op("pool", lambda: nc.gpsimd.tensor_tensor(out=mx[:], in0=mx[:], in1=hx[:], op=ALU.add),
                 reads=[b_mx, b_hx], writes=[b_mx])
            yield
            S.dma_op("sp", k.H1[r0:r1, :], mx[:], reads=[b_mx])

        def drain(g, pool):
            cur_pool[0] = pool
            n = 0
            for _ in g:
                n += 1
            return n

        def lockstep(gS, nS, gP, nP):
            aS = aP = True
            pS = pP = 0.0
            cS = cP = 0
            while aS or aP:
                if aS and (not aP or pS <= pP):
                    cur_pool[0] = "seq"
                    try:
                        next(gS)
                        cS += 1
                        pS += 1.0 / nS
                    except StopIteration:
                        aS = False
                else:
                    cur_pool[0] = "par"
                    try:
                        next(gP)
                        cP += 1
                        pP += 1.0 / nP
                    except StopIteration:
                        aP = False
            return cS, cP

        nP = drain(tile_par(0), "par")
        nS = nP
        for i in range(NT):
            if i + 1 < NT:
                cS, cP = lockstep(tile_seq(i), nS, tile_par(i + 1), nP)
                nS, nP = max(cS, 1), max(cP, 1)
            else:
                drain(tile_seq(i), "seq")
        S.barrier()


def phase_E(k):
    nc, S = k.nc, k.S
    with ExitStack() as es:
        def sb(name, shape, dt):
            return es.enter_context(nc.sbuf_tensor(name, list(shape), dt))
        b_c = Buf("constE")
        wu = sb("wu_sb", [128, 8, 4096], BF16)
        wdn = sb("wdn_sb", [128, 32, D], BF16)
        for kk in range(8):
            S.dma_op("pool", wu[:, kk, :], k.w_up[kk * 128:(kk + 1) * 128, :], writes=[b_c])
        for kk in range(0, 32, 4):
            S.dma_op("pool", wdn[:, kk:kk + 4, :],
                     k.w_down[kk * 128:(kk + 4) * 128, :].rearrange("(f p) n -> p f n", p=128), writes=[b_c])
        g3 = sb("g3", [128, D], F32)
        g4 = sb("g4", [128, D], F32)
        S.dma_op("sp", g3[:], k.norms[2:3, :].partition_broadcast(128), writes=[b_c])
        S.dma_op("sp", g4[:], k.norms[3:4, :].partition_broadcast(128), writes=[b_c])
        nhalf = sb("nhalfE", [128, 1], F32)
        S.op("pool", lambda: nc.gpsimd.memset(nhalf[:], -0.5), writes=[b_c])
        S.barrier()
        h1 = [sb("h1_%d" % i, [128, 2, D], F32) for i in range(2)]; b_h1 = [Buf("h1_%d" % i) for i in range(2)]
        jb = sb("jbE", [128, D], BF16); b_jb = Buf("jbE")
        st = [sb("stE%d" % i, [128, 2, 8], F32) for i in range(2)]; b_st = [Buf("stE%d" % i) for i in range(2)]
        n2 = [sb("n2_%d" % i, [128, D], BF16) for i in range(2)]; b_n2 = [Buf("n2_%d" % i) for i in range(2)]
        n2T = [sb("n2T%d" % i, [128, 8, 256], BF16) for i in range(2)]; b_n2T = [Buf("n2T%d" % i) for i in range(2)]
        uT1 = sb("uT0", [128, 32, 256], BF16); b_uT1 = Buf("uT0")
        uT = [uT1, uT1]; b_uT = [b_uT1, b_uT1]
        rl = [sb("rl%d" % i, [128, 512], F32) for i in range(2)]; b_rl = [Buf("rl%d" % i) for i in range(2)]
        mo = [sb("mo%d" % i, [128, D], F32) for i in range(2)]; b_mo = [Buf("mo%d" % i) for i in range(2)]
        cnt = {"r": 0, "n": 0, "m": 0}
        NG = (NT - 1) // 2

        def head(gi):
            p = gi % 2
            for t in range(2):
                i = 1 + 2 * gi + t
                r0, r1 = i * 128, (i + 1) * 128
                S.dma_op("sp", h1[p][:, t, :], k.H1[r0:r1, :], writes=[b_h1[p]])
            for t in range(2):
                S.op("act", lambda: nc.scalar.activation(out=jb[:], in_=h1[p][:, t, :], func=AF.Square,
                                                         accum_out=st[p][:, t, 0:1]),
                     reads=[b_h1[p]], writes=[b_jb, b_st[p]])
                S.op("pool", lambda: nc.gpsimd.tensor_scalar(out=st[p][:, t, 1:2], in0=st[p][:, t, 0:1],
                                                             scalar1=1.0 / D, scalar2=EPS, op0=ALU.mult, op1=ALU.add),
                     reads=[b_st[p]], writes=[b_st[p]])
                S.op("pool", lambda: nc.gpsimd.tensor_tensor(out=st[p][:, t, 2:3], in0=st[p][:, t, 1:2], in1=nhalf[:],
                                                             op=ALU.pow), reads=[b_st[p], b_c], writes=[b_st[p]])
                np_ = cnt["n"] % 2
                cnt["n"] += 1
                S.op("dve", lambda: nc.vector.scalar_tensor_tensor(out=n2[np_][:], in0=h1[p][:, t, :],
                                                                   scalar=st[p][:, t, 2:3], in1=g3[:],
                                                                   op0=ALU.mult, op1=ALU.mult),
                     reads=[b_h1[p], b_st[p], b_c], writes=[b_n2[np_]])
                ps, bp = k.nextps()
                psb = ps[:].bitcast(BF16).rearrange("p (k t) -> p k t", k=8)
                fns = [(lambda kk=kk: nc.tensor.transpose(psb[:, kk, :], n2[np_][:, kk * 128:(kk + 1) * 128],
                                                          k.identb[:])) for kk in range(8)]
                S.group("pe", fns, reads=[b_n2[np_], k.b_ident], writes=[bp])
                S.op("act", lambda: nc.scalar.copy(out=n2T[p][:, :, t * 128:(t + 1) * 128], in_=psb),
                     reads=[bp], writes=[b_n2T[p]])

        def up(gi):
            p = gi % 2
            for fb in range(16):
                ps, bp = k.nextps()
                fns = []
                for f2 in range(2):
                    f = fb * 2 + f2
                    for kk in range(8):
                        fns.append(lambda f=f, f2=f2, kk=kk: nc.tensor.matmul(
                            ps[:, f2 * 256:(f2 + 1) * 256], lhsT=wu[:, kk, f * 128:(f + 1) * 128], rhs=n2T[p][:, kk, :],
                            start=(kk == 0), stop=(kk == 7)))
                S.group("pe", fns, reads=[b_n2T[p], b_c], writes=[bp])
                rp = cnt["r"] % 2
                cnt["r"] += 1
                S.op("act", lambda: nc.scalar.activation(out=rl[rp][:], in_=ps[:], func=AF.Relu),
                     reads=[bp], writes=[b_rl[rp]])
                S.op("dve", lambda: nc.vector.tensor_tensor(
                    out=uT[p][:, fb * 2:fb * 2 + 2, :].rearrange("p f t -> p (f t)"), in0=rl[rp][:], in1=rl[rp][:],
                    op=ALU.mult), reads=[b_rl[rp]], writes=[b_uT[p]])

        def down(gi):
            p = gi % 2
            for t in range(2):
                i = 1 + 2 * gi + t
                r0, r1 = i * 128, (i + 1) * 128
                mp = cnt["m"] % 2
                cnt["m"] += 1
                for cb in range(2):
                    ps, bp = k.nextps()
                    fns = [(lambda f=f: nc.tensor.matmul(ps[:], lhsT=uT[p][:, f, t * 128:(t + 1) * 128],
                                                         rhs=wdn[:, f, cb * 512:(cb + 1) * 512],
                                                         start=(f == 0), stop=(f == 31))) for f in range(32)]
                    S.group("pe", fns, reads=[b_uT[p], b_c], writes=[bp])
                    S.op("act", lambda: nc.scalar.copy(out=mo[mp][:, cb * 512:(cb + 1) * 512], in_=ps[:]),
                         reads=[bp], writes=[b_mo[mp]])
                S.op("act", lambda: nc.scalar.activation(out=jb[:], in_=mo[mp][:], func=AF.Square,
                                                         accum_out=st[p][:, t, 4:5]),
                     reads=[b_mo[mp]], writes=[b_jb, b_st[p]])
                S.op("pool", lambda: nc.gpsimd.tensor_scalar(out=st[p][:, t, 5:6], in0=st[p][:, t, 4:5],
                                                             scalar1=1.0 / D, scalar2=EPS, op0=ALU.mult, op1=ALU.add),
                     reads=[b_st[p]], writes=[b_st[p]])
                S.op("pool", lambda: nc.gpsimd.tensor_tensor(out=st[p][:, t, 6:7], in0=st[p][:, t, 5:6], in1=nhalf[:],
                                                             op=ALU.pow), reads=[b_st[p], b_c], writes=[b_st[p]])
                S.op("dve", lambda: nc.vector.scalar_tensor_tensor(out=mo[mp][:], in0=mo[mp][:],
                                                                   scalar=st[p][:, t, 6:7], in1=g4[:],
                                                                   op0=ALU.mult, op1=ALU.mult),
                     reads=[b_mo[mp], b_st[p], b_c], writes=[b_mo[mp]])
                S.op("pool", lambda: nc.gpsimd.tensor_tensor(out=mo[mp][:], in0=mo[mp][:], in1=h1[p][:, t, :],
                                                             op=ALU.add),
                     reads=[b_mo[mp], b_h1[p]], writes=[b_mo[mp]])
                S.dma_op("sp", k.out[r0 - 128:r1 - 128, :], mo[mp][:], reads=[b_mo[mp]])

        head(0)
        for gi in range(NG):
            up(gi)
            if gi + 1 < NG:
                head(gi + 1)
            down(gi)
        S.barrier()

def host_consts():
    pos = np.concatenate([np.zeros(PAD, np.float32), np.arange(T - PAD, dtype=np.float32)])

    def tabs(dim, reps):
        inv = (10000.0 ** (-np.arange(0, dim, 2, dtype=np.float32) / dim)).astype(np.float32)
        ang = pos[:, None] * inv[None, :]
        c = np.cos(ang).astype(np.float32)
        s = np.sin(ang).astype(np.float32)
        cT = np.concatenate([c, c], 1).T
        sT = np.concatenate([-s, s], 1).T
        return (np.ascontiguousarray(np.tile(cT, (reps, 1))), np.ascontiguousarray(np.tile(sT, (reps, 1))))
    cosA, sinA = tabs(128, 1)
    cosI, sinI = tabs(64, 2)
    r = np.arange(128)
    NEG = np.float32(-1e30)
    mdiag = np.where(r[None, :] <= r[:, None], 0.0, NEG).astype(np.float32)
    m0 = np.where((r[None, :] <= r[:, None]) & (r[None, :] >= PAD), 0.0, NEG).astype(np.float32)
    mpad = np.where(r[None, :] >= PAD, 0.0, NEG).astype(np.float32) * np.ones((128, 1), np.float32)
    masks = np.ascontiguousarray(np.stack([mdiag, m0, mpad], 0))
    pow2 = (2.0 ** -(np.arange(15, dtype=np.float32) + 1))[None, :].astype(np.float32)
    same = (r[:, None] // 64 == r[None, :] // 64)
    UT = (same & (r[:, None] <= r[None, :])).astype(np.float32)
    SAME = same.astype(np.float32)
    nSL = -(same & (r[:, None] > r[None, :])).astype(np.float32)
    nSU = -(same & (r[:, None] < r[None, :])).astype(np.float32)
    gmasks = np.ascontiguousarray(np.stack([UT, SAME, nSL, nSU], 0))
    cmask = np.stack([(r < 64), (r >= 64)], 1).astype(np.float32)
    return dict(cosA=cosA, sinA=sinA, cosI=cosI, sinI=sinI, ident=np.eye(128, dtype=np.float32),
                masks=masks, pow2=pow2, gmasks=gmasks, cmask=np.ascontiguousarray(cmask))


def swap_halves(w, hd):
    d, n = w.shape
    w = w.reshape(d, n // hd, 2, hd // 2)
    return np.ascontiguousarray(w[:, :, ::-1, :]).reshape(d, n)


def host_inputs(inputs):
    w_in = np.ascontiguousarray(inputs["w_in"][0])
    ik = w_in[:, C_IK:C_IK + 64]
    ikd = np.concatenate([ik, ik], 1)
    groups = []
    aq = w_in[:, C_AQ:C_AQ + 1024]
    aqs = swap_halves(aq, 128)
    for h in range(8):
        groups += [aq[:, h * 128:(h + 1) * 128], aqs[:, h * 128:(h + 1) * 128]]
    ak = w_in[:, C_AK:C_AK + 256]
    aks = swap_halves(ak, 128)
    for h in range(2):
        groups += [ak[:, h * 128:(h + 1) * 128], aks[:, h * 128:(h + 1) * 128]]
    iq = w_in[:, C_IQ:C_IQ + 512]
    iqs = swap_halves(iq, 64)
    for h in range(4):
        groups += [iq[:, h * 128:(h + 1) * 128], iqs[:, h * 128:(h + 1) * 128]]
    groups += [ikd, swap_halves(ikd, 64)]
    w_sw = np.concatenate(groups, 1)
    common = dict(
        meta=np.ascontiguousarray(inputs["meta_tokens"]),
        w_in=w_in, w_sw=np.ascontiguousarray(w_sw),
        conv_w=np.ascontiguousarray(inputs["conv_w"][0]),
        a_log=np.ascontiguousarray(inputs["a_log"]), dt_bias=np.ascontiguousarray(inputs["dt_bias"]),
        gdn_norm=np.ascontiguousarray(inputs["gdn_norm"]),
        wg=np.ascontiguousarray(inputs["w_branch_gdn"][0]), wd=np.ascontiguousarray(inputs["w_branch_dsa"][0]),
        wo=np.ascontiguousarray(inputs["w_out"][0]),
        w_up=np.ascontiguousarray(inputs["w_up"][0]), w_down=np.ascontiguousarray(inputs["w_down"][0]),
        norms=np.ascontiguousarray(np.concatenate([inputs["pre_mix_norm"], inputs["post_mix_norm"],
                                                   inputs["pre_mlp_norm"], inputs["post_mlp_norm"]], 0)),
    )
    common.update(host_consts())
    return common


def kernel(**inputs):
    inputs = {k_: np.asarray(v) for k_, v in inputs.items()}
    nc = build()
    common = host_inputs(inputs)
    in_maps = []
    for b in range(8):
        m = dict(common)
        m["x"] = np.ascontiguousarray(inputs["x"][b])
        in_maps.append(m)
    res = run_bass_kernel_spmd(nc, in_maps, core_ids=list(range(8)))
    return np.stack([r["out"] for r in res.results], 0).astype(np.float32)
```

```python
import numpy as np
import concourse.bass as bass
import concourse.mybir as mybir
from concourse.bass_utils import run_bass_kernel_spmd
from contextlib import ExitStack

F32 = mybir.dt.float32
BF16 = mybir.dt.bfloat16
ALU = mybir.AluOpType
AF = mybir.ActivationFunctionType
AX = mybir.AxisListType

T = 4224
NT = 33
PAD = 112
D = 1024
EPS = 1e-6
C_GQ, C_GK, C_GV, C_GZ, C_GB, C_GA = 0, 1024, 2048, 3072, 4096, 4104
C_AQ, C_AK, C_AV, C_IQ, C_IK, C_IW, C_GTA, C_GTB = 4112, 5136, 5392, 5648, 6160, 6224, 6232, 7256
N_IN = 8280


class Buf:
    __slots__ = ("name", "w", "r")

    def __init__(self, name):
        self.name = name
        self.w = None
        self.r = {}


class Tok:
    __slots__ = ("key", "val", "hist")

    def __init__(self, key, val, hist):
        self.key = key
        self.val = val
        self.hist = hist


class Eng:
    def __init__(self, name, handle, sem, key):
        self.name = name
        self.h = handle
        self.sem = sem
        self.key = key
        self.count = 0
        self.seen = {}
        self.snap = {}
        self.nwaits = 0
        self.ninstr = 0


class Sched:
    NDMA = 24

    def __init__(self, nc, es):
        self.nc = nc
        self.es = es
        self.sems = []
        self.E = {}
        for name, h in [("pe", nc.tensor), ("dve", nc.vector), ("act", nc.scalar),
                        ("pool", nc.gpsimd), ("sp", nc.sync)]:
            sem = es.enter_context(nc.semaphore("s_" + name))
            e = Eng(name, h, sem, len(self.sems))
            self.sems.append(sem)
            self.E[name] = e
        self.dma = []
        for i in range(self.NDMA):
            sem = es.enter_context(nc.semaphore("d%d" % i))
            self.dma.append([len(self.sems), 0])
            self.sems.append(sem)
        self.dma_rr = 0

    def _wait(self, e, tok):
        if tok is None:
            return
        if e.seen.get(tok.key, 0) >= tok.val:
            return
        if tok.key == e.key and e.name == "pe":
            return
        e.h.wait_ge(self.sems[tok.key], tok.val)
        e.nwaits += 1
        e.seen[tok.key] = tok.val
        if tok.hist:
            for k, v in tok.hist.items():
                if e.seen.get(k, 0) < v:
                    e.seen[k] = v
        e.snap = None

    def _deps(self, e, reads, writes):
        for b in reads:
            self._wait(e, b.w)
        for b in writes:
            self._wait(e, b.w)
            for t in b.r.values():
                self._wait(e, t)

    def _commit(self, tok, reads, writes):
        for b in reads:
            o = b.r.get(tok.key)
            if o is None or o.val < tok.val:
                b.r[tok.key] = tok
        for b in writes:
            b.w = tok
            b.r = {}

    def op(self, eng, fn, reads=(), writes=()):
        e = self.E[eng]
        self._deps(e, reads, writes)
        ins = fn()
        e.count += 1
        e.ninstr += 1
        ins.then_inc(e.sem, 1)
        if e.snap is None:
            e.snap = dict(e.seen)
        tok = Tok(e.key, e.count, e.snap)
        self._commit(tok, reads, writes)
        return tok

    def group(self, eng, fns, reads=(), writes=()):
        e = self.E[eng]
        self._deps(e, reads, writes)
        ins = None
        for fn in fns:
            ins = fn()
            e.ninstr += 1
        e.count += 1
        ins.then_inc(e.sem, 1)
        if e.snap is None:
            e.snap = dict(e.seen)
        tok = Tok(e.key, e.count, e.snap)
        self._commit(tok, reads, writes)
        return tok

    def dma_op(self, eng, out, in_, reads=(), writes=(), **kw):
        e = self.E[eng]
        if eng == "pool":
            sem = self.es.enter_context(self.nc.semaphore("q%d" % len(self.sems)))
            slot = [len(self.sems), 0]
            self.sems.append(sem)
            self.dma.append(slot)
        else:
            slot = self.dma[self.dma_rr]
            self.dma_rr = (self.dma_rr + 1) % self.NDMA
        key = slot[0]
        if slot[1] > 0:
            self._wait(e, Tok(key, slot[1], None))
        self._deps(e, reads, writes)
        slot[1] += 16
        ins = e.h.dma_start(out=out, in_=in_, **kw)
        ins.then_inc(self.sems[key], 16)
        e.ninstr += 1
        tok = Tok(key, slot[1], None)
        self._commit(tok, reads, writes)
        return tok

    def barrier(self):
        for e in self.E.values():
            for o in self.E.values():
                if o is not e and o.count > 0:
                    self._wait(e, Tok(o.key, o.count, None))
            for key, val in self.dma:
                if val > 0:
                    self._wait(e, Tok(key, val, None))

    def finish(self):
        self.barrier()
        for e in self.E.values():
            if e.count > 0 and e.name != "pe":
                e.h.wait_ge(e.sem, e.count)

    def stats(self):
        return {n: (e.ninstr, e.nwaits) for n, e in self.E.items()}


class K:
    pass


def build(debug=False, phases="ABCDE"):
    nc = bass.Bass("TRN2", target_bir_lowering=False)
    k = K()
    k.nc = nc
    k.debug = debug

    def din(name, shape, dt=F32):
        return nc.dram_tensor(name, list(shape), dt, kind="ExternalInput").ap()

    def dscr(name, shape, dt):
        kind = "ExternalOutput" if debug else "Internal"
        return nc.dram_tensor(name, list(shape), dt, kind=kind).ap()

    k.x = din("x", [4096, D])
    k.meta = din("meta", [16, D])
    k.w_in = din("w_in", [D, N_IN])
    k.w_sw = din("w_sw", [D, 15 * 256])
    k.conv_w = din("conv_w", [4, 3072])
    k.conv_wT = din("conv_wT", [3072, 4])
    k.a_log = din("a_log", [1, 8])
    k.dt_bias = din("dt_bias", [1, 8])
    k.gdn_norm = din("gdn_norm", [1, 128])
    k.wg = din("wg", [D, D])
    k.wd = din("wd", [D, D])
    k.wo = din("wo", [D, D])
    k.w_up = din("w_up", [D, 4096])
    k.w_down = din("w_down", [4096, D])
    k.norms = din("norms", [4, D])
    k.ident_d = din("ident", [128, 128])
    k.cosA = din("cosA", [128, T])
    k.sinA = din("sinA", [128, T])
    k.cosI = din("cosI", [128, T])
    k.sinI = din("sinI", [128, T])
    k.out = nc.dram_tensor("out", [4096, D], F32, kind="ExternalOutput").ap()

    k.QG = dscr("QG", [T, 1024], BF16)
    k.KG = dscr("KG", [T, 1024], BF16)
    k.VG = dscr("VG", [T, 1024], BF16)
    k.ZS = dscr("ZS", [T, 1024], F32)
    k.BG = dscr("BG", [T, 16], F32)
    k.QT = dscr("QT", [8, 128, T], BF16)
    k.KT = dscr("KT", [2, 128, T], BF16)
    k.Vd = dscr("V", [T, 256], BF16)
    k.QIT = dscr("QIT", [4, 128, T], BF16)
    k.KIT = dscr("KIT", [128, T], BF16)
    k.IW = dscr("IW", [T, 8], F32)
    k.SGA = dscr("SGA", [T, 1024], F32)
    k.SGB = dscr("SGB", [T, 1024], F32)
    k.MDSA = dscr("MDSA", [T, 1024], F32)
    k.H1 = dscr("H1", [T, 1024], F32)
    if debug:
        k.YGDN = dscr("YGDN", [T, 1024], BF16)
    k.gmasks = din("gmasks", [4, 128, 128])
    k.cmask = din("cmask", [128, 2])
    k.masks = din("masks", [3, 128, 128])
    k.pow2 = din("pow2", [1, 15])

    with ExitStack() as es:
        S = Sched(nc, es)
        k.S = S
        k.es = es

        def sb(name, shape, dt, stack=es):
            return stack.enter_context(nc.sbuf_tensor(name, list(shape), dt))
        k.sb = sb

        k.PSB = [es.enter_context(nc.psum_tensor("psb%d" % i, [128, 1024], F32)) for i in range(4)]
        k.PS = [k.PSB[i // 2][:, (i % 2) * 512:(i % 2 + 1) * 512] for i in range(8)]
        k.PB = [Buf("ps%d" % i) for i in range(8)]
        k.ps_rr = 0
        k.ps_n = 8

        k.ps_base = 0

        def nextps():
            i = k.ps_rr % k.ps_n
            k.ps_rr = (i + 1) % k.ps_n
            return k.PS[k.ps_base + i], k.PB[k.ps_base + i]
        k.nextps = nextps

        k.identf = sb("identf", [128, 128], F32)
        k.identb = sb("identb", [128, 128], BF16)
        k.b_ident = Buf("ident")
        S.dma_op("sp", k.identf[:], k.ident_d, writes=[k.b_ident])
        S.op("dve", lambda: nc.vector.tensor_copy(k.identb[:], k.identf[:]), reads=[k.b_ident], writes=[k.b_ident])

        if "A" in phases:
            phase_A(k)
            S.barrier()
        if "C" in phases:
            phase_C(k)
        if "B" in phases:
            phase_B(k)
        if "E" in phases:
            phase_E(k)
        S.finish()
        print("instr stats", S.stats())
    return nc


def phase_A(k):
    nc, S = k.nc, k.S
    with ExitStack() as es:
        cur = [es]

        def sb(name, shape, dt):
            return cur[0].enter_context(nc.sbuf_tensor(name, list(shape), dt))

        def substack():
            st = ExitStack()
            cur[0] = st
            return st

        def endsub(st):
            S.barrier()
            st.close()
            cur[0] = es

        nT = sb("nT", [128, 8, 3 + T], BF16)
        b_nT = [Buf("nT%d" % i) for i in range(NT)]
        b_nTpad = Buf("nTpad")
        S.op("pool", lambda: nc.gpsimd.memset(nT[:, :, 0:3], 0.0), writes=[b_nTpad])

        ysb = [sb("ysb%d" % i, [128, 512], F32) for i in range(2)]
        b_ysb = [Buf("ysb%d" % i) for i in range(2)]
        ob = [sb("ob%d" % i, [128, 512], BF16) for i in range(3)]
        b_ob = [Buf("ob%d" % i) for i in range(3)]
        of = [sb("of%d" % i, [128, 512], F32) for i in range(3)]
        b_of = [Buf("of%d" % i) for i in range(3)]
        sq = [sb("sq%d" % i, [128, 8], F32) for i in range(2)]
        b_sq = [Buf("sq%d" % i) for i in range(2)]
        nhalf = sb("nhalf", [128, 4], F32)
        b_nhalf = Buf("nhalf")
        S.op("pool", lambda: nc.gpsimd.memset(nhalf[:], -0.5), writes=[b_nhalf])
        junkf = sb("junkf", [128, 128], F32)
        b_junkf = Buf("junkf")

        st = substack()
        g1 = sb("g1", [128, D], F32)
        b_g1 = Buf("g1")
        S.dma_op("sp", g1[:], k.norms[0:1, :].partition_broadcast(128), writes=[b_g1])

        ht = [sb("ht%d" % i, [128, D], F32) for i in range(2)]
        b_ht = [Buf("ht%d" % i) for i in range(2)]
        junk = sb("junkA", [128, D], BF16)
        b_junk = Buf("junkA")
        nb = [sb("nb%d" % i, [128, D], BF16) for i in range(2)]
        b_nb = [Buf("nb%d" % i) for i in range(2)]
        ss = [sb("ssA%d" % i, [128, 4], F32) for i in range(2)]
        b_ss = [Buf("ssA%d" % i) for i in range(2)]

        for i in range(NT):
            p = i % 2
            h_, bh = ht[p], b_ht[p]
            if i == 0:
                S.op("pool", lambda: nc.gpsimd.memset(h_[:], 0.0), writes=[bh])
                S.dma_op("sp", h_[PAD:128, :], k.meta, writes=[bh])
            else:
                S.dma_op("sp", h_[:], k.x[(i - 1) * 128:i * 128, :], writes=[bh])
            s_, bs = ss[p], b_ss[p]
            S.op("act", lambda: nc.scalar.activation(out=junk[:], in_=h_[:], func=AF.Square,
                                                     accum_out=s_[:, 0:1]),
                 reads=[bh], writes=[b_junk, bs])
            S.op("act", lambda: nc.scalar.activation(out=s_[:, 1:2], in_=s_[:, 0:1], func=AF.Sqrt,
                                                     scale=1.0 / D, bias=EPS),
                 reads=[bs], writes=[bs])
            S.op("dve", lambda: nc.vector.reciprocal(s_[:, 2:3], s_[:, 1:2]), reads=[bs], writes=[bs])
            n_, bn = nb[p], b_nb[p]
            S.op("dve", lambda: nc.vector.scalar_tensor_tensor(out=n_[:], in0=h_[:], scalar=s_[:, 2:3],
                                                               in1=g1[:], op0=ALU.mult, op1=ALU.mult),
                 reads=[bh, bs, b_g1], writes=[bn])
            ps, bp = k.nextps()
            psb = ps[:].bitcast(BF16).rearrange("p (k t) -> p k t", k=8)
            fns = []
            for kk in range(8):
                fns.append(lambda kk=kk: nc.tensor.transpose(psb[:, kk, :], n_[:, kk * 128:(kk + 1) * 128],
                                                             k.identb[:]))
            S.group("pe", fns, reads=[bn, k.b_ident], writes=[bp])
            S.op("dve", lambda: nc.vector.tensor_copy(nT[:, :, 3 + i * 128:3 + (i + 1) * 128], psb),
                 reads=[bp], writes=[b_nT[i]])

        endsub(st)
        st = substack()
        cnt = {"g": 0, "t": 0}
        wstg = sb("wstg", [128, 8, 128], F32); b_wstg = Buf("wstg")
        wc = [sb("wc%d" % i, [128, 8, 128], BF16) for i in range(2)]; b_wc = [Buf("wc%d" % i) for i in range(2)]
        cwT = [sb("cwT%d" % i, [128, 4], F32) for i in range(2)]; b_cwT = [Buf("cwT%d" % i) for i in range(2)]
        pT = [sb("pT%d" % i, [128, 3 + T], F32) for i in range(2)]
        b_pT = [[Buf("pT%d_%d" % (i, tb)) for tb in range(9)] for i in range(2)]
        b_pTpad = [Buf("pTpad%d" % i) for i in range(2)]
        for i in range(2):
            S.op("pool", (lambda i=i: nc.gpsimd.memset(pT[i][:, 0:3], 0.0)), writes=[b_pTpad[i]])
        accs = [sb("accs%d" % i, [128, 512], F32) for i in range(2)]; b_accs = [Buf("accs%d" % i) for i in range(2)]
        yTb = [sb("yTb%d" % i, [128, 512], BF16) for i in range(2)]; b_yTb = [Buf("yTb%d" % i) for i in range(2)]
        tmf = [sb("tmf%d" % i, [128, 4, 128], F32) for i in range(2)]; b_tmf = [Buf("tmf%d" % i) for i in range(2)]
        sqf = sb("sqf", [128, 4, 128], F32); b_sqf = Buf("sqf")
        obt = [sb("obt%d" % i, [128, 4, 128], BF16) for i in range(3)]; b_obt = [Buf("obt%d" % i) for i in range(3)]

        blocks = [(g, tb) for g in range(24) for tb in range(9)]
        gbuf = {}

        def conv_X(bi):
            g, tb = blocks[bi]
            cc0 = g * 128
            if tb == 0:
                gp = cnt["g"] % 2
                cnt["g"] += 1
                gbuf[g] = gp
                S.dma_op("sp", wstg[:], k.w_in[:, cc0:cc0 + 128].rearrange("(k p) n -> p k n", p=128),
                         writes=[b_wstg])
                S.op("pool", lambda: nc.gpsimd.tensor_copy(wc[gp][:], wstg[:]), reads=[b_wstg], writes=[b_wc[gp]])
                S.dma_op("sp", cwT[gp][:], k.conv_wT[cc0:cc0 + 128, :], writes=[b_cwT[gp]])
            gp = gbuf[g]
            c0 = tb * 512
            n = min(512, T - c0)
            tiles = list(range(c0 // 128, (c0 + n) // 128))
            ps, bp = k.nextps()
            fns = [(lambda kk=kk: nc.tensor.matmul(ps[:, 0:n], lhsT=wc[gp][:, kk, :],
                                                   rhs=nT[:, kk, 3 + c0:3 + c0 + n],
                                                   start=(kk == 0), stop=(kk == 7))) for kk in range(8)]
            S.group("pe", fns, reads=[b_wc[gp]] + [b_nT[i] for i in tiles], writes=[bp])
            S.op("act", lambda: nc.scalar.copy(out=pT[gp][:, 3 + c0:3 + c0 + n], in_=ps[:, 0:n]),
                 reads=[bp], writes=[b_pT[gp][tb]])
            rdp = [b_pT[gp][tb], b_pT[gp][tb - 1] if tb > 0 else b_pTpad[gp]]
            ap = bi % 2
            S.op("act", lambda: nc.scalar.activation(out=accs[ap][:, 0:n], in_=pT[gp][:, c0:c0 + n],
                                                     func=AF.Copy, scale=cwT[gp][:, 0:1]),
                 reads=rdp + [b_cwT[gp]], writes=[b_accs[ap]])
            for j in range(1, 4):
                eng = "dve"
                h = nc.vector
                S.op(eng, (lambda j=j, h=h: h.scalar_tensor_tensor(
                    out=accs[ap][:, 0:n], in0=pT[gp][:, c0 + j:c0 + j + n], scalar=cwT[gp][:, j:j + 1],
                    in1=accs[ap][:, 0:n], op0=ALU.mult, op1=ALU.add)),
                    reads=rdp + [b_cwT[gp], b_accs[ap]], writes=[b_accs[ap]])

        def conv_Y(bi):
            g, tb = blocks[bi]
            kind = "qkv"[g // 8]
            hd = g % 8
            dst = {"q": k.QG, "k": k.KG, "v": k.VG}[kind]
            c0 = tb * 512
            n = min(512, T - c0)
            nt = n // 128
            ap = bi % 2
            t3 = bi % 3
            S.op("act", lambda: nc.scalar.activation(out=yTb[ap][:, 0:n], in_=accs[ap][:, 0:n], func=AF.Silu),
                 reads=[b_accs[ap]], writes=[b_yTb[ap]])
            ps2, bp2 = k.nextps()
            psb = ps2[:].bitcast(BF16).rearrange("p (k t) -> p k t", k=8)
            fns = [(lambda t=t: nc.tensor.transpose(psb[:, t, :], yTb[ap][:, t * 128:(t + 1) * 128],
                                                    k.identb[:])) for t in range(nt)]
            S.group("pe", fns, reads=[b_yTb[ap], k.b_ident], writes=[bp2])
            o_, bo = obt[t3], b_obt[t3]
            if kind == "v":
                S.op("act", lambda: nc.scalar.copy(out=o_[:, 0:nt, :], in_=psb[:, 0:nt, :]),
                     reads=[bp2], writes=[bo])
            else:
                tm, btm = tmf[ap], b_tmf[ap]
                q_, bq = sq[ap], b_sq[ap]
                S.op("dve", lambda: nc.vector.tensor_copy(tm[:, 0:nt, :], psb[:, 0:nt, :]),
                     reads=[bp2], writes=[btm])
                S.op("dve", lambda: nc.vector.tensor_tensor(out=sqf[:, 0:nt, :], in0=tm[:, 0:nt, :],
                                                            in1=tm[:, 0:nt, :], op=ALU.mult),
                     reads=[btm], writes=[b_sqf])
                S.op("dve", lambda: nc.vector.tensor_reduce(out=q_[:, 0:nt], in_=sqf[:, 0:nt, :], axis=AX.X,
                                                            op=ALU.add), reads=[b_sqf], writes=[bq])
                sc = 128.0 if kind == "q" else 1.0
                S.op("pool", lambda: nc.gpsimd.tensor_scalar(out=q_[:, 0:nt], in0=q_[:, 0:nt], scalar1=sc,
                                                             scalar2=sc * EPS, op0=ALU.mult, op1=ALU.add),
                     reads=[bq], writes=[bq])
                S.op("pool", lambda: nc.gpsimd.tensor_tensor(out=q_[:, 4:4 + nt], in0=q_[:, 0:nt],
                                                             in1=nhalf[:, 0:nt], op=ALU.pow),
                     reads=[bq, b_nhalf], writes=[bq])


            def tail():
                if kind != "v":
                    S.op("dve", lambda: nc.vector.tensor_tensor(
                        out=o_[:, 0:nt, :], in0=tm[:, 0:nt, :],
                        in1=q_[:, 4:4 + nt].unsqueeze(2).broadcast_to([128, nt, 128]), op=ALU.mult),
                        reads=[btm, bq], writes=[bo])
                S.dma_op("sp", dst[c0:c0 + n, hd * 128:(hd + 1) * 128].rearrange("(t p) c -> p t c", p=128),
                         o_[:, 0:nt, :], reads=[bo])
            return tail

        def conv_group(cc0, kind):
            gp = cnt["g"] % 2
            cnt["g"] += 1
            S.dma_op("sp", wst[0][:], k.w_in[:, cc0:cc0 + 512].rearrange("(k p) n -> p k n", p=128),
                     writes=[b_wst[0]])
            S.dma_op("sp", cw[0][:], k.conv_w[:, cc0:cc0 + 512].partition_broadcast(128),
                     writes=[b_cw[0]])
            for j in range(4):
                S.op("dve", lambda: nc.vector.tensor_tensor(
                    out=w4[gp][:, :, j, :], in0=wst[0][:],
                    in1=cw[0][:, j:j + 1, :].broadcast_to([128, 8, 512]), op=ALU.mult),
                    reads=[b_wst[0], b_cw[0]], writes=[b_w4[gp]])
            dst = {"q": k.QG, "k": k.KG, "v": k.VG}[kind]
            dcol = cc0 % 1024
            for i in range(NT):
                ps, bp = k.nextps()
                fns = []
                for j in range(4):
                    for kk in range(8):
                        fns.append(lambda j=j, kk=kk: nc.tensor.matmul(
                            ps[:], lhsT=nT[:, kk, i * 128 + j:i * 128 + j + 128], rhs=w4[gp][:, kk, j, :],
                            start=(j == 0 and kk == 0), stop=(j == 3 and kk == 7)))
                rd = [b_w4[gp], b_nT[i], b_nTpad] + ([b_nT[i - 1]] if i > 0 else [])
                S.group("pe", fns, reads=rd, writes=[bp])
                tp = cnt["t"] % 2
                t3 = cnt["t"] % 3
                cnt["t"] += 1
                o_, bo = ob[t3], b_ob[t3]
                if kind == "v":
                    S.op("act", lambda: nc.scalar.activation(out=o_[:], in_=ps[:], func=AF.Silu),
                         reads=[bp], writes=[bo])
                else:
                    y_, by = ysb[tp], b_ysb[tp]
                    q_, bq = sq[tp], b_sq[tp]
                    S.op("act", lambda: nc.scalar.activation(out=y_[:], in_=ps[:], func=AF.Silu),
                         reads=[bp], writes=[by])
                    for hh in range(4):
                        S.op("dve", lambda: nc.vector.scalar_tensor_tensor(
                            out=junkf[:], in0=y_[:, hh * 128:(hh + 1) * 128], scalar=1.0,
                            in1=y_[:, hh * 128:(hh + 1) * 128], op0=ALU.mult, op1=ALU.mult,
                            accum_out=q_[:, hh:hh + 1]),
                            reads=[by], writes=[b_junkf, bq])
                    sc = 128.0 if kind == "q" else 1.0
                    S.op("pool", lambda: nc.gpsimd.tensor_scalar(out=q_[:, 0:4], in0=q_[:, 0:4], scalar1=sc,
                                                                 scalar2=sc * EPS, op0=ALU.mult, op1=ALU.add),
                         reads=[bq], writes=[bq])
                    S.op("pool", lambda: nc.gpsimd.tensor_tensor(out=q_[:, 4:8], in0=q_[:, 0:4], in1=nhalf[:],
                                                                 op=ALU.pow),
                         reads=[bq, b_nhalf], writes=[bq])
                    for hh in range(4):
                        S.op("dve", lambda: nc.vector.tensor_scalar(
                            out=o_[:, hh * 128:(hh + 1) * 128], in0=y_[:, hh * 128:(hh + 1) * 128],
                            scalar1=q_[:, 4 + hh:5 + hh], scalar2=None, op0=ALU.mult),
                            reads=[by, bq], writes=[bo])
                S.dma_op("sp", dst[i * 128:(i + 1) * 128, dcol:dcol + 512], o_[:], reads=[bo])

        conv_X(0)
        pend_tail = None
        for bi in range(len(blocks)):
            if bi + 1 < len(blocks):
                conv_X(bi + 1)
            tl = conv_Y(bi)
            if pend_tail is not None:
                pend_tail()
            pend_tail = tl
        pend_tail()

        endsub(st)
        st = substack()
        wt = [sb("wt%d" % i, [128, 8, 512], BF16) for i in range(2)]
        b_wt = [Buf("wt%d" % i) for i in range(2)]
        albc = sb("albc", [128, 16], F32)
        b_albc = Buf("albc")
        S.dma_op("sp", albc[:, 0:8], k.a_log.partition_broadcast(128), writes=[b_albc])
        S.dma_op("sp", albc[:, 8:16], k.dt_bias.partition_broadcast(128), writes=[b_albc])
        S.op("act", lambda: nc.scalar.activation(out=albc[:, 0:8], in_=albc[:, 0:8], func=AF.Exp),
             reads=[b_albc], writes=[b_albc])

        def plain_group(c0, ncols, post):
            gp = cnt["g"] % 2
            cnt["g"] += 1
            S.dma_op("pool", wt[gp][:, :, 0:ncols], k.w_in[:, c0:c0 + ncols].rearrange("(k p) n -> p k n", p=128),
                     writes=[b_wt[gp]])
            for i in range(NT):
                ps, bp = k.nextps()
                fns = []
                for kk in range(8):
                    fns.append(lambda kk=kk: nc.tensor.matmul(
                        ps[:, 0:ncols], lhsT=nT[:, kk, 3 + i * 128:3 + (i + 1) * 128], rhs=wt[gp][:, kk, 0:ncols],
                        start=(kk == 0), stop=(kk == 7)))
                S.group("pe", fns, reads=[b_wt[gp], b_nT[i]], writes=[bp])
                post(i, ps, bp)

        def post_act(func, dst, dcol, ncols, bf, scale=1.0):
            def post(i, ps, bp):
                t3 = cnt["t"] % 3
                cnt["t"] += 1
                o_, bo = (ob[t3], b_ob[t3]) if bf else (of[t3], b_of[t3])
                S.op("act", lambda: nc.scalar.activation(out=o_[:, 0:ncols], in_=ps[:, 0:ncols], func=func,
                                                         scale=scale),
                     reads=[bp], writes=[bo])
                S.dma_op("sp", dst[i * 128:(i + 1) * 128, dcol:dcol + ncols], o_[:, 0:ncols], reads=[bo])
            return post

        for cb in range(2):
            plain_group(C_GZ + cb * 512, 512, post_act(AF.Silu, k.ZS, cb * 512, 512, False))
        for cb in range(2):
            plain_group(C_GTA + cb * 512, 512, post_act(AF.Sigmoid, k.SGA, cb * 512, 512, False))
        for cb in range(2):
            plain_group(C_GTB + cb * 512, 512, post_act(AF.Sigmoid, k.SGB, cb * 512, 512, False))
        plain_group(C_AV, 256, post_act(AF.Copy, k.Vd, 0, 256, True))
        plain_group(C_IW, 8, post_act(AF.Copy, k.IW, 0, 8, False, scale=512.0 ** -0.5))

        def post_bg(i, ps, bp):
            t3 = cnt["t"] % 3
            cnt["t"] += 1
            o_, bo = of[t3], b_of[t3]
            S.op("act", lambda: nc.scalar.activation(out=o_[:, 0:8], in_=ps[:, 0:8], func=AF.Sigmoid),
                 reads=[bp], writes=[bo])
            S.op("dve", lambda: nc.vector.tensor_tensor(out=o_[:, 16:24], in0=ps[:, 8:16], in1=albc[:, 8:16],
                                                        op=ALU.add),
                 reads=[bp, b_albc], writes=[bo])
            S.op("act", lambda: nc.scalar.activation(out=o_[:, 16:24], in_=o_[:, 16:24], func=AF.Exp),
                 reads=[bo], writes=[bo])
            S.op("act", lambda: nc.scalar.activation(out=o_[:, 16:24], in_=o_[:, 16:24], func=AF.Ln, bias=1.0),
                 reads=[bo], writes=[bo])
            S.op("dve", lambda: nc.vector.scalar_tensor_tensor(out=o_[:, 8:16], in0=o_[:, 16:24], scalar=-1.0,
                                                               in1=albc[:, 0:8], op0=ALU.mult, op1=ALU.mult),
                 reads=[bo, b_albc], writes=[bo])
            S.dma_op("sp", k.BG[i * 128:(i + 1) * 128, :], o_[:, 0:16], reads=[bo])
        plain_group(C_GB, 16, post_bg)

        endsub(st)
        st = substack()
        cosT = sb("cosT", [128, T], F32)
        sinT = sb("sinT", [128, T], F32)
        b_tab = Buf("ropetab")
        wf = [sb("wf%d" % i, [128, 8, 256], BF16) for i in range(2)]
        b_wf = [Buf("wf%d" % i) for i in range(2)]
        t1 = [sb("t1_%d" % i, [128, 512], F32) for i in range(2)]
        b_t1 = [Buf("t1_%d" % i) for i in range(2)]
        t2 = [sb("t2_%d" % i, [128, 512], F32) for i in range(2)]
        b_t2 = [Buf("t2_%d" % i) for i in range(2)]

        def load_tab(c, s):
            S.dma_op("sp", cosT[:], c, writes=[b_tab])
            S.dma_op("sp", sinT[:], s, writes=[b_tab])

        def rope_group(w_ap, dst):
            gp = cnt["g"] % 2
            cnt["g"] += 1
            S.dma_op("pool", wf[gp][:], w_ap.rearrange("(k p) n -> p k n", p=128), writes=[b_wf[gp]])
            for tb in range(9):
                c0 = tb * 512
                n = min(512, T - c0)
                tiles = list(range(c0 // 128, (c0 + n) // 128))
                psA, bpA = k.nextps()
                psB, bpB = k.nextps()
                for (ps, bp, off) in ((psA, bpA, 0), (psB, bpB, 128)):
                    fns = []
                    for kk in range(8):
                        fns.append(lambda kk=kk, ps=ps, off=off: nc.tensor.matmul(
                            ps[:, 0:n], lhsT=wf[gp][:, kk, off:off + 128], rhs=nT[:, kk, 3 + c0:3 + c0 + n],
                            start=(kk == 0), stop=(kk == 7)))
                    S.group("pe", fns, reads=[b_wf[gp]] + [b_nT[i] for i in tiles], writes=[bp])
                tp = cnt["t"] % 2
                t3 = cnt["t"] % 3
                cnt["t"] += 1
                S.op("dve", lambda: nc.vector.tensor_tensor(out=t1[tp][:, 0:n], in0=psA[:, 0:n],
                                                            in1=cosT[:, c0:c0 + n], op=ALU.mult),
                     reads=[bpA, b_tab], writes=[b_t1[tp]])
                S.op("dve", lambda: nc.vector.tensor_tensor(out=t2[tp][:, 0:n], in0=psB[:, 0:n],
                                                            in1=sinT[:, c0:c0 + n], op=ALU.mult),
                     reads=[bpB, b_tab], writes=[b_t2[tp]])
                o_, bo = ob[t3], b_ob[t3]
                S.op("pool", lambda: nc.gpsimd.tensor_tensor(out=o_[:, 0:n], in0=t1[tp][:, 0:n],
                                                             in1=t2[tp][:, 0:n], op=ALU.add),
                     reads=[b_t1[tp], b_t2[tp]], writes=[bo])
                S.dma_op("sp", dst[:, c0:c0 + n], o_[:, 0:n], reads=[bo])

        load_tab(k.cosA, k.sinA)
        for h in range(8):
            rope_group(k.w_sw[:, h * 256:(h + 1) * 256], k.QT[h])
        for h in range(2):
            rope_group(k.w_sw[:, (8 + h) * 256:(9 + h) * 256], k.KT[h])
        load_tab(k.cosI, k.sinI)
        for h in range(4):
            rope_group(k.w_sw[:, (10 + h) * 256:(11 + h) * 256], k.QIT[h])
        rope_group(k.w_sw[:, 14 * 256:15 * 256], k.KIT)
        endsub(st)


def phase_C(k):
    nc, S = k.nc, k.S
    NIT = 14
    with ExitStack() as es:
        def sb(name, shape, dt):
            return es.enter_context(nc.sbuf_tensor(name, list(shape), dt))
        kit = sb("kit", [128, T], BF16)
        kts = sb("kts", [128, 2, T], BF16)
        vs = sb("vs", [128, NT, 256], BF16)
        wd = sb("wd_sb", [128, 8, D], BF16)
        b_res = Buf("resC")
        S.dma_op("sp", kit[:], k.KIT, writes=[b_res])
        S.dma_op("sp", kts[:], k.KT.rearrange("g p t -> p g t"), writes=[b_res])
        S.dma_op("sp", vs[:], k.Vd.rearrange("(j p) c -> p j c", p=128), writes=[b_res])
        S.dma_op("pool", wd[:], k.wd.rearrange("(h p) n -> p h n", p=128), writes=[b_res])
        msk = sb("msk", [128, 3, 128], F32)
        S.dma_op("sp", msk[:], k.masks.rearrange("m p c -> p m c"), writes=[b_res])
        p2 = sb("p2", [128, NIT + 1], F32)
        S.dma_op("sp", p2[:], k.pow2.partition_broadcast(128), writes=[b_res])
        ones = sb("onesC", [128, 128], BF16)
        S.op("pool", lambda: nc.gpsimd.memset(ones[:], 1.0), writes=[b_res])
        S.barrier()

        I = sb("I", [128, T], F32); b_I = Buf("I")
        M = sb("M", [128, T], BF16); b_M = Buf("M")
        MT2 = [sb("MT%d" % i, [128, NT, 128], BF16) for i in range(2)]; b_MT2 = [Buf("MT%d" % i) for i in range(2)]
        jk = sb("jkC", [128, T], BF16); b_jk = Buf("jkC")
        qit = [sb("qit%d" % i, [128, 4, 128], BF16) for i in range(2)]; b_qit = [Buf("qit%d" % i) for i in range(2)]
        qt = [sb("qt%d" % i, [128, 8, 128], BF16) for i in range(2)]; b_qt = [Buf("qt%d" % i) for i in range(2)]
        iw = [sb("iw%d" % i, [128, 8], F32) for i in range(2)]; b_iw = [Buf("iw%d" % i) for i in range(2)]
        sgb = [sb("sgb%d" % i, [128, D], F32) for i in range(2)]; b_sgb = [Buf("sgb%d" % i) for i in range(2)]
        r_ = [sb("r_%d" % i, [128, 512], F32) for i in range(2)]; b_r = [Buf("r_%d" % i) for i in range(2)]
        e_ = [sb("e_%d" % i, [128, 512], BF16) for i in range(2)]; b_e = [Buf("e_%d" % i) for i in range(2)]
        p_ = [sb("p_%d" % i, [128, 512], BF16) for i in range(3)]; b_p = [Buf("p_%d" % i) for i in range(3)]
        rden = sb("rden", [128, 512], F32); b_rden = Buf("rden")
        ot = [sb("ot%d" % i, [128, 4, 128], BF16) for i in range(2)]; b_ot = [Buf("ot%d" % i) for i in range(2)]
        md = sb("md", [128, D], F32); b_md = Buf("md")
        st = sb("stC", [128, 8], F32); b_st = Buf("stC")
        w2 = sb("w2C", [128, NIT + 1], F32); b_w2 = Buf("w2C")
        tmpd = sb("tmpd", [128, 128], F32); b_tmpd = Buf("tmpd")
        midt = sb("midt", [128, 1], F32); b_mid = Buf("midt")
        cn = sb("cnC", [128, 1], F32); b_cn = Buf("cnC")
        sa = sb("saC", [128, 1], F32); b_sa = Buf("saC")
        gt = sb("gtC", [128, 2], F32); b_gt = Buf("gtC")
        jk2 = sb("jk2C", [128, T], BF16); b_jk2 = Buf("jk2C")
        k.ps_base = 5
        k.ps_n = 3
        k.ps_rr = 0
        psOg = [k.PS[3], k.PS[3]]
        bOg = [k.PB[3], k.PB[3]]
        psDg = [k.PS[4], k.PS[4]]
        bDg = [k.PB[4], k.PB[4]]
        att_rr = [0]

        def attps():
            i = att_rr[0] % 3
            att_rr[0] += 1
            return k.PS[i], k.PB[i]
        cnt = {"r": 0, "e": 0}
        SC = 128.0 ** -0.5

        def stage1a(i):
            nk = i + 1
            NK = nk * 128
            par = i % 2
            yield
            S.dma_op("sp", qit[par][:], k.QIT[:, :, i * 128:(i + 1) * 128].rearrange("g p t -> p g t"),
                     writes=[b_qit[par]])
            yield
            S.dma_op("sp", qt[par][:], k.QT[:, :, i * 128:(i + 1) * 128].rearrange("g p t -> p g t"),
                     writes=[b_qt[par]])
            yield
            S.dma_op("sp", iw[par][:], k.IW[i * 128:(i + 1) * 128, :], writes=[b_iw[par]])
            yield
            S.dma_op("sp", sgb[par][:], k.SGB[i * 128:(i + 1) * 128, :], writes=[b_sgb[par]])
            for c0 in range(0, NK, 512):
                n = min(512, NK - c0)
                for h in range(8):
                    g, hf = h // 2, h % 2
                    ps, bp = k.nextps()
                    yield
                    S.op("pe", lambda: nc.tensor.matmul(ps[:, 0:n], lhsT=qit[par][64 * hf:64 * hf + 64, g, :],
                                                        rhs=kit[64 * hf:64 * hf + 64, c0:c0 + n],
                                                        start=True, stop=True),
                         reads=[b_qit[par]], writes=[bp])
                    rp = cnt["r"] % 2
                    cnt["r"] += 1
                    yield
                    S.op("act", lambda: nc.scalar.activation(out=r_[rp][:, 0:n], in_=ps[:, 0:n], func=AF.Relu),
                         reads=[bp], writes=[b_r[rp]])
                    if h == 0:
                        yield
                        S.op("dve", lambda: nc.vector.tensor_scalar(out=I[:, c0:c0 + n], in0=r_[rp][:, 0:n],
                                                                    scalar1=iw[par][:, 0:1], scalar2=None,
                                                                    op0=ALU.mult),
                             reads=[b_r[rp], b_iw[par]], writes=[b_I])
                    else:
                        yield
                        S.op("dve", lambda: nc.vector.scalar_tensor_tensor(
                            out=I[:, c0:c0 + n], in0=r_[rp][:, 0:n], scalar=iw[par][:, h:h + 1],
                            in1=I[:, c0:c0 + n], op0=ALU.mult, op1=ALU.add),
                            reads=[b_r[rp], b_iw[par]], writes=[b_I])
            m0 = 1 if i == 0 else 2
            yield
            S.op("dve", lambda: nc.vector.tensor_tensor(out=I[:, 0:128], in0=I[:, 0:128], in1=msk[:, m0, :],
                                                        op=ALU.add), reads=[b_I], writes=[b_I])
            if i >= 1:
                yield
                S.op("dve", lambda: nc.vector.tensor_tensor(out=I[:, i * 128:NK], in0=I[:, i * 128:NK],
                                                            in1=msk[:, 0, :], op=ALU.add),
                     reads=[b_I], writes=[b_I])
            if i < 2:
                yield
                S.op("dve", lambda: nc.vector.memset(st[:, 6:7], -1e29), writes=[b_st])
            else:
                yield
                S.op("dve", lambda: nc.vector.tensor_reduce(out=st[:, 0:1], in_=I[:, 0:NK], axis=AX.X, op=ALU.max),
                     reads=[b_I], writes=[b_st])
                yield
                S.op("dve", lambda: nc.vector.tensor_reduce(out=st[:, 1:2], in_=I[:, PAD:i * 128], axis=AX.X,
                                                            op=ALU.min), reads=[b_I], writes=[b_st])
                if i == 2:
                    yield
                    S.op("dve", lambda: nc.vector.scalar_tensor_tensor(out=tmpd[:], in0=msk[:, 0, :], scalar=-2.0,
                                                                       in1=I[:, i * 128:NK], op0=ALU.mult,
                                                                       op1=ALU.add),
                         reads=[b_I], writes=[b_tmpd])
                    yield
                    S.op("dve", lambda: nc.vector.tensor_reduce(out=st[:, 7:8], in_=tmpd[:], axis=AX.X, op=ALU.min),
                         reads=[b_tmpd], writes=[b_st])
                    yield
                    S.op("dve", lambda: nc.vector.tensor_tensor(out=st[:, 1:2], in0=st[:, 1:2], in1=st[:, 7:8],
                                                                op=ALU.min), reads=[b_st], writes=[b_st])
                yield
                S.op("dve", lambda: nc.vector.tensor_tensor(out=st[:, 2:3], in0=st[:, 0:1], in1=st[:, 1:2],
                                                            op=ALU.subtract), reads=[b_st], writes=[b_st])
                yield
                S.op("dve", lambda: nc.vector.tensor_scalar(out=w2[:], in0=p2[:], scalar1=st[:, 2:3], scalar2=None,
                                                            op0=ALU.mult), reads=[b_st], writes=[b_w2])
                yield
                S.op("dve", lambda: nc.vector.tensor_tensor(out=st[:, 3:4], in0=st[:, 1:2], in1=w2[:, 0:1],
                                                            op=ALU.add), reads=[b_st, b_w2], writes=[b_st])
                hc = NK if nk <= 3 else 128 * max(1, int(round(0.45 * nk)))
                na = NK - hc
                yield
                S.op("dve", lambda: nc.vector.tensor_copy(midt[:], st[:, 3:4]), reads=[b_st], writes=[b_mid])
                for it in range(NIT):
                    if na > 0:
                        yield
                        S.op("act", lambda: nc.scalar.activation(out=jk2[:, 0:na], in_=I[:, hc:NK], func=AF.Sign,
                                                                 scale=-1.0, bias=midt[:, 0:1], accum_out=sa[:, 0:1]),
                             reads=[b_I, b_mid], writes=[b_jk2, b_sa])
                    yield
                    S.op("dve", lambda: nc.vector.tensor_scalar(out=jk[:, 0:hc], in0=I[:, 0:hc], scalar1=midt[:, 0:1],
                                                                scalar2=None, op0=ALU.is_ge, op1=ALU.add,
                                                                accum_out=cn[:, 0:1]),
                         reads=[b_I, b_mid], writes=[b_jk, b_cn])
                    if na > 0:
                        yield
                        S.op("dve", lambda: nc.vector.scalar_tensor_tensor(out=gt[:, 0:1], in0=sa[:, 0:1], scalar=-0.5,
                                                                           in1=cn[:, 0:1], op0=ALU.mult, op1=ALU.add),
                             reads=[b_sa, b_cn], writes=[b_gt])
                        src, bsrc = gt[:, 0:1], b_gt
                    else:
                        src, bsrc = cn[:, 0:1], b_cn
                    yield
                    S.op("dve", lambda: nc.vector.tensor_scalar(out=gt[:, 1:2], in0=src, scalar1=255.5 - 0.5 * na,
                                                                scalar2=-0.5, op0=ALU.is_ge, op1=ALU.add),
                         reads=[bsrc], writes=[b_gt])
                    yield
                    S.op("dve", lambda: nc.vector.scalar_tensor_tensor(out=midt[:, 0:1], in0=gt[:, 1:2],
                                                                       scalar=w2[:, it:it + 1], in1=midt[:, 0:1],
                                                                       op0=ALU.mult, op1=ALU.add),
                         reads=[b_gt, b_w2, b_mid], writes=[b_mid])
                yield
                S.op("dve", lambda: nc.vector.tensor_copy(st[:, 3:4], midt[:]), reads=[b_mid], writes=[b_st])
                yield
                S.op("dve", lambda: nc.vector.tensor_scalar(out=st[:, 6:7], in0=st[:, 3:4], scalar1=w2[:, NIT:NIT + 1],
                                                            scalar2=-1e29, op0=ALU.subtract, op1=ALU.max),
                     reads=[b_st, b_w2], writes=[b_st])
            yield
            S.op("dve", lambda: nc.vector.tensor_scalar(out=M[:, 0:NK], in0=I[:, 0:NK], scalar1=st[:, 6:7],
                                                        scalar2=None, op0=ALU.is_ge),
                 reads=[b_I, b_st], writes=[b_M])
        def stage1b(i):
            nk = i + 1
            MT, b_MT = MT2[i % 2], b_MT2[i % 2]
            for j0 in range(0, nk, 8):
                nj = min(8, nk - j0)
                ps, bp = k.nextps()
                psb = ps[:].bitcast(BF16).rearrange("p (k t) -> p k t", k=8)
                fns = [(lambda jj=jj: nc.tensor.transpose(psb[:, jj, :], M[:, (j0 + jj) * 128:(j0 + jj + 1) * 128],
                                                          k.identb[:])) for jj in range(nj)]
                yield
                S.group("pe", fns, reads=[b_M, k.b_ident], writes=[bp])
                yield
                S.op("act", lambda: nc.scalar.activation(out=MT[:, j0:j0 + nj, :], in_=psb[:, 0:nj, :], func=AF.Copy,
                                                         scale=30000.0, bias=-30000.0),
                     reads=[bp], writes=[b_MT])
        def stage2(i):
            nk = i + 1
            par = i % 2
            MT, b_MT = MT2[i % 2], b_MT2[i % 2]
            steps = [(g, j) for g in range(2) for j in range(nk)]

            def emitS(g, j):
                ps, bp = attps()
                S.group("pe", [
                    lambda: nc.tensor.matmul(ps[:], lhsT=kts[:, g, j * 128:(j + 1) * 128],
                                             rhs=qt[par][:, 4 * g:4 * g + 4, :], start=True, stop=False),
                    lambda: nc.tensor.matmul(ps[:], lhsT=k.identb[:],
                                             rhs=MT[:, j:j + 1, :].broadcast_to([128, 4, 128]),
                                             start=False, stop=True)],
                    reads=[b_qt[par], b_MT, k.b_ident], writes=[bp])
                return ps, bp
            pend = []
            for sidx in range(min(2, len(steps))):
                yield
                pend.append(emitS(*steps[sidx]))
            for sidx, (g, j) in enumerate(steps):
                ps, bp = pend.pop(0)
                ep = cnt["e"] % 3
                cnt["e"] += 1
                yield
                S.op("act", lambda: nc.scalar.activation(out=p_[ep][:], in_=ps[:], func=AF.Exp, scale=SC),
                     reads=[bp], writes=[b_p[ep]])
                if sidx + 2 < len(steps):
                    yield
                    pend.append(emitS(*steps[sidx + 2]))
                yield
                S.op("pe", lambda: nc.tensor.matmul(psOg[g][:], lhsT=vs[:, j, g * 128:(g + 1) * 128], rhs=p_[ep][:],
                                                    start=(j == 0), stop=(j == nk - 1)),
                     reads=[b_p[ep]], writes=[bOg[g]])
                yield
                S.op("pe", lambda: nc.tensor.matmul(psDg[g][:], lhsT=ones[:], rhs=p_[ep][:],
                                                    start=(j == 0), stop=(j == nk - 1)),
                     reads=[b_p[ep]], writes=[bDg[g]])
                if j == nk - 1:
                    yield
                    S.op("dve", lambda: nc.vector.tensor_scalar(out=rden[:], in0=psDg[g][:], scalar1=1e-20,
                                                                scalar2=None, op0=ALU.max),
                         reads=[bDg[g]], writes=[b_rden])
                    yield
                    S.op("dve", lambda: nc.vector.reciprocal(rden[:], rden[:]), reads=[b_rden], writes=[b_rden])
                    yield
                    S.op("dve", lambda: nc.vector.tensor_tensor(out=ot[g][:].rearrange("p h t -> p (h t)"),
                                                                in0=psOg[g][:], in1=rden[:], op=ALU.mult),
                         reads=[bOg[g], b_rden], writes=[b_ot[g]])
            for cb in range(2):
                ps, bp = attps()
                fns = [(lambda h=h: nc.tensor.matmul(ps[:], lhsT=ot[h // 4][:, h % 4, :],
                                                     rhs=wd[:, h, cb * 512:(cb + 1) * 512],
                                                     start=(h == 0), stop=(h == 7))) for h in range(8)]
                yield
                S.group("pe", fns, reads=[b_ot[0], b_ot[1]], writes=[bp])
                yield
                S.op("dve", lambda: nc.vector.tensor_tensor(out=md[:, cb * 512:(cb + 1) * 512], in0=ps[:],
                                                            in1=sgb[par][:, cb * 512:(cb + 1) * 512], op=ALU.mult),
                     reads=[bp, b_sgb[par]], writes=[b_md])
            yield
            S.dma_op("sp", k.MDSA[i * 128:(i + 1) * 128, :], md[:], reads=[b_md])

        def drain(g):
            for _ in g:
                pass

        def chain(*gs):
            for g in gs:
                yield from g

        def lockstep(gA, nA, gB, nB):
            aA = aB = True
            pA = pB = 0.0
            while aA or aB:
                if aA and (not aB or pA <= pB):
                    try:
                        next(gA)
                        pA += 1.0 / nA
                    except StopIteration:
                        aA = False
                else:
                    try:
                        next(gB)
                        pB += 1.0 / nB
                    except StopIteration:
                        aB = False

        def est1(i):
            nk = i + 1
            return 4 + ((nk + 3) // 4) * 24 + (NIT * 5 + 12 if i >= 2 else 3) + 2 * ((nk + 7) // 8)

        def est2(i):
            return 8 * (i + 1) + 12

        drain(chain(stage1a(0), stage1b(0)))
        for i in range(NT):
            if i + 1 < NT:
                lockstep(stage2(i), est2(i), chain(stage1a(i + 1), stage1b(i + 1)), est1(i + 1))
            else:
                drain(stage2(i))
        k.ps_base = 0
        k.ps_n = 8
        S.barrier()


def phase_B(k):
    nc, S = k.nc, k.S
    with ExitStack() as es:
        def sb(name, shape, dt):
            return es.enter_context(nc.sbuf_tensor(name, list(shape), dt))
        b_c = Buf("constB")
        gm = sb("gm", [128, 4, 128], F32)
        S.dma_op("sp", gm[:], k.gmasks.rearrange("m p c -> p m c"), writes=[b_c])
        cmask = sb("cmask_sb", [128, 2], F32)
        S.dma_op("sp", cmask[:], k.cmask, writes=[b_c])
        onesf = sb("onesf", [128, 128], F32)
        S.op("pool", lambda: nc.gpsimd.memset(onesf[:], 1.0), writes=[b_c])
        nhalf = sb("nhalfB", [128, 8], F32)
        S.op("pool", lambda: nc.gpsimd.memset(nhalf[:], -0.5), writes=[b_c])
        wg = sb("wg_sb", [128, 8, D], BF16)
        wo = sb("wo_sb", [128, 8, D], BF16)
        S.dma_op("pool", wg[:], k.wg.rearrange("(h p) n -> p h n", p=128), writes=[b_c])
        S.dma_op("pool", wo[:], k.wo.rearrange("(h p) n -> p h n", p=128), writes=[b_c])
        gn = sb("gn", [128, 128], F32)
        S.dma_op("sp", gn[:], k.gdn_norm.partition_broadcast(128), writes=[b_c])
        g2 = sb("g2", [128, D], F32)
        S.dma_op("sp", g2[:], k.norms[1:2, :].partition_broadcast(128), writes=[b_c])
        Sst = sb("Sst", [128, 8, 128], F32); b_S = Buf("Sst")
        Sbf = sb("Sbf", [128, 8, 128], BF16); b_Sbf = Buf("Sbf")
        S.op("pool", lambda: nc.gpsimd.memset(Sst[:], 0.0), writes=[b_S])
        S.op("pool", lambda: nc.gpsimd.memset(Sbf[:], 0.0), writes=[b_Sbf])
        S.barrier()

        def mk(name, shape, dt, n=1):
            ts = [sb("%s%d" % (name, i), shape, dt) for i in range(n)]
            bs = [Buf("%s%d" % (name, i)) for i in range(n)]
            return (ts, bs) if n > 1 else (ts[0], bs[0])
        qg, b_qg = mk("qgB", [128, 8, 128], BF16)
        kg, b_kg = mk("kgB", [128, 8, 128], BF16)
        vg, b_vg = mk("vgB", [128, 8, 128], BF16)
        zs2, b_zs2 = mk("zsB", [128, 8, 128], F32, 2)
        bg, b_bg = mk("bgB", [128, 16], F32)
        sga2, b_sga2 = mk("sgaB", [128, D], F32, 2)
        mdsa2, b_mdsa2 = mk("mdsaB", [128, D], F32, 2)
        hx2, b_hx2 = mk("hxB", [128, D], F32, 2)
        sc, b_sc = mk("scB", [128, 8, 8], F32)
        glb2, b_glb2 = mk("glb", [128, 2, 8], F32, 2)
        Bu, b_Bu = mk("Bu", [128, 8, 128], F32)
        B2, b_B2 = mk("B2", [128, 2, 8], F32)
        kb, b_kb = mk("kbB", [128, 8, 128], BF16)
        qd, b_qd = mk("qdB", [128, 8, 128], BF16)
        kbg, b_kbg = mk("kbgB", [128, 8, 128], BF16)
        ktl2, b_ktl2 = mk("ktlB", [128, 8, 128], BF16, 2)
        vb, b_vb = mk("vbB", [128, 8, 128], BF16)
        kT, b_kT = mk("kTB", [128, 8, 128], BF16)
        kbT, b_kbT = mk("kbTB", [128, 8, 128], BF16)
        qT, b_qT = mk("qTB", [128, 8, 128], BF16)
        qdT2, b_qdT2 = mk("qdTB", [128, 8, 128], BF16, 2)
        D1, b_D1 = mk("D1", [128, 8, 128], F32)
        Em, b_Em = mk("Em", [128, 8, 128], F32)
        ETm, b_ETm = mk("ETm", [128, 8, 128], F32)
        tA, b_tA = mk("tA", [128, 8, 128], F32)
        Bm, b_Bm = mk("Bm", [128, 8, 128], BF16, 2)
        Cm, b_Cm = mk("Cm", [128, 8, 128], BF16, 2)
        Ym, b_Ym = mk("Ym", [128, 8, 128], BF16, 2)
        Yf, b_Yf = mk("Yf", [128, 8, 128], F32, 2)
        aT2, b_aT2 = mk("aT", [128, 8, 128], BF16, 2)
        uS2, b_uS2 = mk("uS", [128, 8, 128], F32, 2)
        wT2, b_wT2 = mk("wT", [128, 8, 128], BF16, 2)
        vn, b_vn = mk("vn", [128, 8, 128], BF16)
        osb, b_osb = mk("osb", [128, 8, 128], F32)
        st8, b_st8 = mk("st8", [128, 24], F32)
        sqt, b_sqt = mk("sqtB", [128, 8, 128], F32)
        yb, b_yb = mk("ybB", [128, 8, 128], BF16)
        yT, b_yT = mk("yTB", [128, 8, 128], BF16)
        mg, b_mg = mk("mgB", [128, D], F32)
        mgb, b_mgb = mk("mgbB", [128, D], BF16)
        mT, b_mT = mk("mTB", [128, 8, 128], BF16)
        mx, b_mx = mk("mxB", [128, D], F32)
        jb, b_jb = mk("jbB", [128, D], BF16)

        def bc_h(ap2):
            return ap2.unsqueeze(2).broadcast_to([128, 8, 128])

        def bc_m(ap2):
            return ap2.unsqueeze(1).broadcast_to([128, 8, 128])

        pool_rr = {"par": 0, "seq": 0}
        cur_pool = ["par"]

        class PT(tuple):
            pass

        def two_banks():
            if cur_pool[0] == "par":
                m = pool_rr["par"] % 2
                pool_rr["par"] += 1
            else:
                m = 2 + pool_rr["seq"] % 2
                pool_rr["seq"] += 1
            pss = PT((k.PS[2 * m], k.PS[2 * m + 1]))
            pss.big = k.PSB[m]
            return pss, (k.PB[2 * m], k.PB[2 * m + 1])

        def nextps():
            pss, bps = two_banks()
            return pss[0], bps[0]

        def pv(ps, hh):
            return ps[hh // 4][:, (hh % 4) * 128:(hh % 4 + 1) * 128]

        def mm8(lhs_fn, rhs_fn, reads):
            pss, bps = two_banks()
            fns = [(lambda hh=hh: nc.tensor.matmul(pv(pss, hh), lhsT=lhs_fn(hh), rhs=rhs_fn(hh),
                                                   start=True, stop=True)) for hh in range(8)]
            S.group("pe", fns, reads=reads, writes=[bps[0], bps[1]])
            return pss, bps

        def ev8(eng, fn, pss, bps, reads, writes):
            S.op(eng, (lambda: fn(pss.big[:].rearrange("p (h t) -> p h t", h=8), slice(0, 8))),
                 reads=[bps[0], bps[1]] + reads, writes=writes)

        def tr8(src, b_src, dst, b_dst):
            ps, bp = nextps()
            psb = ps[:].bitcast(BF16).rearrange("p (k t) -> p k t", k=8)
            fns = [(lambda hh=hh: nc.tensor.transpose(psb[:, hh, :], src[:, hh, :], k.identb[:])) for hh in range(8)]
            S.group("pe", fns, reads=[b_src, k.b_ident], writes=[bp])
            S.op("act", lambda: nc.scalar.copy(out=dst[:], in_=psb), reads=[bp], writes=[b_dst])

        def tile_par(i):
            qdT, b_qdT = qdT2[i % 2], b_qdT2[i % 2]
            aT, b_aT = aT2[i % 2], b_aT2[i % 2]
            uS, b_uS = uS2[i % 2], b_uS2[i % 2]
            wT, b_wT = wT2[i % 2], b_wT2[i % 2]
            ktl, b_ktl = ktl2[i % 2], b_ktl2[i % 2]
            glb, b_glb = glb2[i % 2], b_glb2[i % 2]
            zs, b_zs = zs2[i % 2], b_zs2[i % 2]
            sga, b_sga = sga2[i % 2], b_sga2[i % 2]
            mdsa, b_mdsa = mdsa2[i % 2], b_mdsa2[i % 2]
            hx, b_hx = hx2[i % 2], b_hx2[i % 2]
            r0, r1 = i * 128, (i + 1) * 128
            yield
            S.dma_op("sp", qg[:], k.QG[r0:r1, :].rearrange("p (h d) -> p h d", h=8), writes=[b_qg])
            yield
            S.dma_op("sp", kg[:], k.KG[r0:r1, :].rearrange("p (h d) -> p h d", h=8), writes=[b_kg])
            yield
            S.dma_op("sp", vg[:], k.VG[r0:r1, :].rearrange("p (h d) -> p h d", h=8), writes=[b_vg])
            yield
            S.dma_op("sp", zs[:], k.ZS[r0:r1, :].rearrange("p (h d) -> p h d", h=8), writes=[b_zs])
            yield
            S.dma_op("sp", bg[:], k.BG[r0:r1, :], writes=[b_bg])
            yield
            S.dma_op("sp", sga[:], k.SGA[r0:r1, :], writes=[b_sga])
            yield
            S.dma_op("sp", mdsa[:], k.MDSA[r0:r1, :], writes=[b_mdsa])
            if i == 0:
                yield
                S.op("pool", lambda: nc.gpsimd.memset(hx[:], 0.0), writes=[b_hx])
                yield
                S.dma_op("sp", hx[PAD:128, :], k.meta, writes=[b_hx])
            else:
                yield
                S.dma_op("sp", hx[:], k.x[r0 - 128:r1 - 128, :], writes=[b_hx])
            beta = bg[:, 0:8]
            gg = bg[:, 8:16]
            yield
            ps, bp = nextps()
            yield
            S.op("pe", lambda: nc.tensor.matmul(ps[:, 0:8], lhsT=gm[:, 0, :], rhs=gg, start=True, stop=True),
                 reads=[b_bg, b_c], writes=[bp])
            yield
            S.op("pe", lambda: nc.tensor.matmul(ps[:, 8:16], lhsT=gm[:, 1, :], rhs=gg, start=True, stop=True),
                 reads=[b_bg, b_c], writes=[bp])
            yield
            S.op("dve", lambda: nc.vector.tensor_copy(sc[:, 0:2, :], ps[:, 0:16].rearrange("p (a h) -> p a h", a=2)),
                 reads=[bp], writes=[b_sc])
            yield
            S.op("act", lambda: nc.scalar.activation(out=sc[:, 2, :], in_=sc[:, 0, :], func=AF.Exp),
                 reads=[b_sc], writes=[b_sc])
            yield
            S.op("dve", lambda: nc.vector.tensor_tensor(out=sc[:, 3, :], in0=sc[:, 2, :], in1=beta, op=ALU.mult),
                 reads=[b_sc, b_bg], writes=[b_sc])
            yield
            S.op("dve", lambda: nc.vector.tensor_tensor(out=sc[:, 5, :], in0=sc[:, 1, :], in1=sc[:, 0, :],
                                                        op=ALU.subtract), reads=[b_sc], writes=[b_sc])
            yield
            S.op("act", lambda: nc.scalar.activation(out=sc[:, 4, :], in_=sc[:, 5, :], func=AF.Exp),
                 reads=[b_sc], writes=[b_sc])
            yield
            S.op("dve", lambda: nc.vector.tensor_tensor(out=B2[:], in0=gg.unsqueeze(1).broadcast_to([128, 2, 8]),
                                                        in1=cmask[:].unsqueeze(2).broadcast_to([128, 2, 8]),
                                                        op=ALU.mult), reads=[b_bg, b_c], writes=[b_B2])
            yield
            ps, bp = nextps()
            yield
            S.op("pe", lambda: nc.tensor.matmul(ps[:, 0:16], lhsT=onesf[:], rhs=B2[:].rearrange("p c h -> p (c h)"),
                                                start=True, stop=True), reads=[b_B2, b_c], writes=[bp])
            yield
            S.op("act", lambda: nc.scalar.activation(out=glb[:].rearrange("p c h -> p (c h)"), in_=ps[:, 0:16],
                                                     func=AF.Exp), reads=[bp], writes=[b_glb])
            yield
            S.op("dve", lambda: nc.vector.tensor_tensor(out=Bu[:], in0=bc_m(gm[:, 0, :]), in1=bc_h(gg), op=ALU.mult),
                 reads=[b_bg, b_c], writes=[b_Bu])
            pss, bps = two_banks()
            yield
            for half in range(2):
                yield
                S.op("pe", (lambda half=half: nc.tensor.matmul(
                    pss[half][:], lhsT=onesf[:], rhs=Bu[:, 4 * half:4 * half + 4, :].rearrange("p h t -> p (h t)"),
                    start=True, stop=True)), reads=[b_Bu, b_c], writes=[bps[half]])
            yield
            ev8("dve", lambda psv, sl: nc.vector.tensor_tensor(
                out=D1[:, sl, :], in0=psv,
                in1=sc[:, 0, :].unsqueeze(2).broadcast_to([128, 8, 128]), op=ALU.subtract),
                pss, bps, [b_sc], [b_D1])
            yield
            S.op("dve", lambda: nc.vector.tensor_scalar(out=Em[:], in0=D1[:], scalar1=0.0, scalar2=None, op0=ALU.max),
                 reads=[b_D1], writes=[b_Em])
            yield
            S.op("act", lambda: nc.scalar.activation(out=Em[:], in_=Em[:], func=AF.Exp, scale=-1.0),
                 reads=[b_Em], writes=[b_Em])
            yield
            S.op("dve", lambda: nc.vector.tensor_scalar(out=ETm[:], in0=D1[:], scalar1=0.0, scalar2=None, op0=ALU.min),
                 reads=[b_D1], writes=[b_ETm])
            yield
            S.op("act", lambda: nc.scalar.activation(out=ETm[:], in_=ETm[:], func=AF.Exp),
                 reads=[b_ETm], writes=[b_ETm])
            yield
            S.op("pool", lambda: nc.gpsimd.tensor_tensor(out=kb[:], in0=kg[:], in1=bc_h(beta), op=ALU.mult),
                 reads=[b_kg, b_bg], writes=[b_kb])
            yield
            S.op("pool", lambda: nc.gpsimd.tensor_tensor(out=qd[:], in0=qg[:], in1=bc_h(sc[:, 2, :]), op=ALU.mult),
                 reads=[b_qg, b_sc], writes=[b_qd])
            yield
            S.op("pool", lambda: nc.gpsimd.tensor_tensor(out=kbg[:], in0=kg[:], in1=bc_h(sc[:, 3, :]), op=ALU.mult),
                 reads=[b_kg, b_sc], writes=[b_kbg])
            yield
            S.op("pool", lambda: nc.gpsimd.tensor_tensor(out=ktl[:], in0=kg[:], in1=bc_h(sc[:, 4, :]), op=ALU.mult),
                 reads=[b_kg, b_sc], writes=[b_ktl])
            yield
            S.op("pool", lambda: nc.gpsimd.tensor_tensor(out=vb[:], in0=vg[:], in1=bc_h(beta), op=ALU.mult),
                 reads=[b_vg, b_bg], writes=[b_vb])
            yield
            tr8(kg, b_kg, kT, b_kT)
            yield
            tr8(kb, b_kb, kbT, b_kbT)
            yield
            tr8(qg, b_qg, qT, b_qT)
            yield
            tr8(qd, b_qd, qdT, b_qdT)
            yield
            S.op("pool", lambda: nc.gpsimd.tensor_tensor(out=tA[:], in0=Em[:], in1=bc_m(gm[:, 2, :]), op=ALU.mult),
                 reads=[b_Em, b_c], writes=[b_tA])
            yield
            pss, bps = mm8(lambda hh: kbT[:, hh, :], lambda hh: kT[:, hh, :], [b_kbT, b_kT])
            yield
            ev8("dve", lambda psv, sl: nc.vector.tensor_tensor(out=Bm[0][:, sl, :], in0=psv,
                                                                  in1=tA[:, sl, :], op=ALU.mult),
                pss, bps, [b_tA], [b_Bm[0]])
            yield
            S.op("pool", lambda: nc.gpsimd.tensor_tensor(out=tA[:], in0=ETm[:], in1=bc_m(gm[:, 3, :]), op=ALU.mult),
                 reads=[b_ETm, b_c], writes=[b_tA])
            yield
            pss, bps = mm8(lambda hh: kT[:, hh, :], lambda hh: kbT[:, hh, :], [b_kbT, b_kT])
            yield
            ev8("dve", lambda psv, sl: nc.vector.tensor_tensor(out=Cm[0][:, sl, :], in0=psv,
                                                                  in1=tA[:, sl, :], op=ALU.mult),
                pss, bps, [b_tA], [b_Cm[0]])
            yield
            S.op("pool", lambda: nc.gpsimd.tensor_tensor(out=tA[:], in0=ETm[:], in1=bc_m(gm[:, 0, :]), op=ALU.mult),
                 reads=[b_ETm, b_c], writes=[b_tA])
            yield
            pss, bps = mm8(lambda hh: kT[:, hh, :], lambda hh: qT[:, hh, :], [b_qT, b_kT])
            yield
            ev8("dve", lambda psv, sl: nc.vector.tensor_tensor(out=aT[:, sl, :], in0=psv,
                                                                  in1=tA[:, sl, :], op=ALU.mult),
                pss, bps, [b_tA], [b_aT])
            yield
            S.op("dve", lambda: nc.vector.tensor_tensor(out=Yf[0][:], in0=Cm[0][:], in1=bc_m(k.identf[:]), op=ALU.add),
                 reads=[b_Cm[0], k.b_ident], writes=[b_Yf[0]])
            yield
            S.op("act", lambda: nc.scalar.copy(out=Ym[0][:], in_=Yf[0][:]), reads=[b_Yf[0]], writes=[b_Ym[0]])
            cur = 0
            yield
            for lev in range(1, 6):
                nx = 1 - cur
                yield
                pss, bps = mm8(lambda hh: Cm[cur][:, hh, :], lambda hh: Bm[cur][:, hh, :], [b_Cm[cur], b_Bm[cur]])
                yield
                ev8("act", lambda psv, sl: nc.scalar.copy(out=Bm[nx][:, sl, :], in_=psv),
                    pss, bps, [], [b_Bm[nx]])
                if lev < 5:
                    yield
                    pss, bps = mm8(lambda hh: Bm[cur][:, hh, :], lambda hh: Cm[cur][:, hh, :], [b_Cm[cur], b_Bm[cur]])
                    yield
                    ev8("act", lambda psv, sl: nc.scalar.copy(out=Cm[nx][:, sl, :], in_=psv),
                        pss, bps, [], [b_Cm[nx]])
                yield
                pss, bps = mm8(lambda hh: Bm[nx][:, hh, :], lambda hh: Ym[cur][:, hh, :], [b_Bm[nx], b_Ym[cur]])
                yield
                ev8("dve", lambda psv, sl: nc.vector.tensor_tensor(out=Yf[nx][:, sl, :], in0=psv,
                                                                      in1=Yf[cur][:, sl, :],
                                                                      op=ALU.add),
                    pss, bps, [b_Yf[cur]], [b_Yf[nx]])
                yield
                S.op("act", lambda: nc.scalar.copy(out=Ym[nx][:], in_=Yf[nx][:]), reads=[b_Yf[nx]], writes=[b_Ym[nx]])
                cur = nx
            Y = Ym[cur]
            bY = b_Ym[cur]
            yield
            pss, bps = mm8(lambda hh: Y[:, hh, :], lambda hh: vb[:, hh, :], [bY, b_vb])
            yield
            ev8("act", lambda psv, sl: nc.scalar.copy(out=uS[:, sl, :], in_=psv),
                pss, bps, [], [b_uS])
            yield
            pss, bps = mm8(lambda hh: kbg[:, hh, :], lambda hh: Y[:, hh, :], [bY, b_kbg])
            yield
            ev8("act", lambda psv, sl: nc.scalar.copy(out=wT[:, sl, :], in_=psv),
                pss, bps, [], [b_wT])

        def tile_seq(i):
            r0, r1 = i * 128, (i + 1) * 128
            qdT, b_qdT = qdT2[i % 2], b_qdT2[i % 2]
            aT, b_aT = aT2[i % 2], b_aT2[i % 2]
            uS, b_uS = uS2[i % 2], b_uS2[i % 2]
            wT, b_wT = wT2[i % 2], b_wT2[i % 2]
            ktl, b_ktl = ktl2[i % 2], b_ktl2[i % 2]
            glb, b_glb = glb2[i % 2], b_glb2[i % 2]
            zs, b_zs = zs2[i % 2], b_zs2[i % 2]
            sga, b_sga = sga2[i % 2], b_sga2[i % 2]
            mdsa, b_mdsa = mdsa2[i % 2], b_mdsa2[i % 2]
            hx, b_hx = hx2[i % 2], b_hx2[i % 2]
            yield
            for c in range(2):
                c0, c1 = 64 * c, 64 * c + 64
                pss, bps = two_banks()
                fns = [(lambda hh=hh: nc.tensor.matmul(pv(pss, hh)[c0:c1, :], lhsT=wT[:, hh, c0:c1],
                                                       rhs=Sbf[:, hh, :], start=True, stop=True))
                       for hh in range(8)]
                yield
                S.group("pe", fns, reads=[b_wT, b_Sbf], writes=[bps[0], bps[1]])
                yield
                S.op("dve", lambda: nc.vector.tensor_tensor(
                    out=vn[c0:c1, :, :], in0=uS[c0:c1, :, :],
                    in1=pss.big[c0:c1, :].rearrange("p (h t) -> p h t", h=8), op=ALU.subtract),
                    reads=[bps[0], bps[1], b_uS], writes=[b_vn])
                pso, bpo = two_banks()
                fns = []
                for hh in range(8):
                    fns.append(lambda hh=hh: nc.tensor.matmul(pv(pso, hh)[c0:c1, :], lhsT=qdT[:, hh, c0:c1],
                                                              rhs=Sbf[:, hh, :], start=True, stop=False))
                    fns.append(lambda hh=hh: nc.tensor.matmul(pv(pso, hh)[c0:c1, :], lhsT=aT[c0:c1, hh, c0:c1],
                                                              rhs=vn[c0:c1, hh, :], start=False, stop=True))
                yield
                S.group("pe", fns, reads=[b_qdT, b_Sbf, b_aT, b_vn], writes=[bpo[0], bpo[1]])
                yield
                S.op("act", lambda: nc.scalar.copy(
                    out=osb[c0:c1, :, :], in_=pso.big[c0:c1, :].rearrange("p (h t) -> p h t", h=8)),
                    reads=[bpo[0], bpo[1]], writes=[b_osb])
                pss2, bps2 = two_banks()
                fns = [(lambda hh=hh: nc.tensor.matmul(pv(pss2, hh), lhsT=ktl[c0:c1, hh, :], rhs=vn[c0:c1, hh, :],
                                                       start=True, stop=True)) for hh in range(8)]
                yield
                S.group("pe", fns, reads=[b_ktl, b_vn], writes=[bps2[0], bps2[1]])
                yield
                S.op("pool", lambda: nc.gpsimd.tensor_tensor(out=Sst[:], in0=Sst[:], in1=bc_h(glb[:, c, :]),
                                                             op=ALU.mult), reads=[b_S, b_glb], writes=[b_S])
                yield
                S.op("dve", lambda: nc.vector.tensor_tensor(
                    out=Sst[:], in0=Sst[:], in1=pss2.big[:].rearrange("p (h t) -> p h t", h=8), op=ALU.add),
                    reads=[bps2[0], bps2[1], b_S], writes=[b_S])
                yield
                S.op("act", lambda: nc.scalar.copy(out=Sbf[:], in_=Sst[:]), reads=[b_S], writes=[b_Sbf])
            yield
            S.op("dve", lambda: nc.vector.tensor_tensor(out=sqt[:], in0=osb[:], in1=osb[:], op=ALU.mult),
                 reads=[b_osb], writes=[b_sqt])
            yield
            S.op("dve", lambda: nc.vector.tensor_reduce(out=st8[:, 0:8], in_=sqt[:], axis=AX.X, op=ALU.add),
                 reads=[b_sqt], writes=[b_st8])
            yield
            S.op("pool", lambda: nc.gpsimd.tensor_scalar(out=st8[:, 8:16], in0=st8[:, 0:8], scalar1=1.0 / 128,
                                                         scalar2=EPS, op0=ALU.mult, op1=ALU.add),
                 reads=[b_st8], writes=[b_st8])
            yield
            S.op("pool", lambda: nc.gpsimd.tensor_tensor(out=st8[:, 16:24], in0=st8[:, 8:16], in1=nhalf[:], op=ALU.pow),
                 reads=[b_st8, b_c], writes=[b_st8])
            yield
            S.op("pool", lambda: nc.gpsimd.tensor_tensor(out=zs[:], in0=zs[:], in1=bc_m(gn[:]), op=ALU.mult),
                 reads=[b_zs, b_c], writes=[b_zs])
            yield
            S.op("dve", lambda: nc.vector.tensor_tensor(out=osb[:], in0=osb[:], in1=bc_h(st8[:, 16:24]), op=ALU.mult),
                 reads=[b_osb, b_st8], writes=[b_osb])
            yield
            S.op("dve", lambda: nc.vector.tensor_tensor(out=yb[:], in0=osb[:], in1=zs[:], op=ALU.mult),
                 reads=[b_osb, b_zs], writes=[b_yb])
            if k.debug:
                yield
                S.dma_op("sp", k.YGDN[r0:r1, :].rearrange("p (h d) -> p h d", h=8), yb[:], reads=[b_yb])
            yield
            tr8(yb, b_yb, yT, b_yT)
            yield
            for cb in range(2):
                ps, bp = nextps()
                fns = [(lambda h=h: nc.tensor.matmul(ps[:], lhsT=yT[:, h, :], rhs=wg[:, h, cb * 512:(cb + 1) * 512],
                                                     start=(h == 0), stop=(h == 7))) for h in range(8)]
                yield
                S.group("pe", fns, reads=[b_yT, b_c], writes=[bp])
                yield
                S.op("dve", lambda: nc.vector.tensor_tensor(out=mg[:, cb * 512:(cb + 1) * 512], in0=ps[:],
                                                            in1=sga[:, cb * 512:(cb + 1) * 512], op=ALU.mult),
                     reads=[bp, b_sga], writes=[b_mg])
            yield
            S.op("pool", lambda: nc.gpsimd.tensor_tensor(out=mgb[:], in0=mg[:], in1=mdsa[:], op=ALU.add),
                 reads=[b_mg, b_mdsa], writes=[b_mgb])
            yield
            tr8(mgb[:].rearrange("p (h d) -> p h d", h=8), b_mgb, mT, b_mT)
            yield
            for cb in range(2):
                ps, bp = nextps()
                fns = [(lambda h=h: nc.tensor.matmul(ps[:], lhsT=mT[:, h, :], rhs=wo[:, h, cb * 512:(cb + 1) * 512],
                                                     start=(h == 0), stop=(h == 7))) for h in range(8)]
                yield
                S.group("pe", fns, reads=[b_mT, b_c], writes=[bp])
                yield
                S.op("act", lambda: nc.scalar.copy(out=mx[:, cb * 512:(cb + 1) * 512], in_=ps[:]),
                     reads=[bp], writes=[b_mx])
            yield
            S.op("act", lambda: nc.scalar.activation(out=jb[:], in_=mx[:], func=AF.Square, accum_out=st8[:, 0:1]),
                 reads=[b_mx], writes=[b_jb, b_st8])
            yield
            S.op("pool", lambda: nc.gpsimd.tensor_scalar(out=st8[:, 8:9], in0=st8[:, 0:1], scalar1=1.0 / D,
                                                         scalar2=EPS, op0=ALU.mult, op1=ALU.add),
                 reads=[b_st8], writes=[b_st8])
            yield
            S.op("pool", lambda: nc.gpsimd.tensor_tensor(out=st8[:, 16:17], in0=st8[:, 8:9], in1=nhalf[:, 0:1],
                                                         op=ALU.pow), reads=[b_st8, b_c], writes=[b_st8])
            yield
            S.op("dve", lambda: nc.vector.scalar_tensor_tensor(out=mx[:], in0=mx[:], scalar=st8[:, 16:17], in1=g2[:],
                                                               op0=ALU.mult, op1=ALU.mult),
                 reads=[b_mx, b_st8, b_c], writes=[b_mx])
            yield
            S.op("pool", lambda: nc.gpsimd.tensor_tensor(out=mx[:], in0=mx[:], in1=hx[:], op=ALU.add),
                 reads=[b_mx, b_hx], writes=[b_mx])
            yield
            S.dma_op("sp", k.H1[r0:r1, :], mx[:], reads=[b_mx])

        def drain(g, pool):
            cur_pool[0] = pool
            n = 0
            for _ in g:
                n += 1
            return n

        def lockstep(gS, nS, gP, nP):
            aS = aP = True
            pS = pP = 0.0
            cS = cP = 0
            while aS or aP:
                if aS and (not aP or pS <= pP):
                    cur_pool[0] = "seq"
                    try:
                        next(gS)
                        cS += 1
                        pS += 1.0 / nS
                    except StopIteration:
                        aS = False
                else:
                    cur_pool[0] = "par"
                    try:
                        next(gP)
                        cP += 1
                        pP += 1.0 / nP
                    except StopIteration:
                        aP = False
            return cS, cP

        nP = drain(tile_par(0), "par")
        nS = nP
        for i in range(NT):
            if i + 1 < NT:
                cS, cP = lockstep(tile_seq(i), nS, tile_par(i + 1), nP)
                nS, nP = max(cS, 1), max(cP, 1)
            else:
                drain(tile_seq(i), "seq")
        S.barrier()


def phase_E(k):
    nc, S = k.nc, k.S
    with ExitStack() as es:
        def sb(name, shape, dt):
            return es.enter_context(nc.sbuf_tensor(name, list(shape), dt))
        b_c = Buf("constE")
        wu = sb("wu_sb", [128, 8, 4096], BF16)
        wdn = sb("wdn_sb", [128, 32, D], BF16)
        for kk in range(8):
            S.dma_op("pool", wu[:, kk, :], k.w_up[kk * 128:(kk + 1) * 128, :], writes=[b_c])
        for kk in range(0, 32, 4):
            S.dma_op("pool", wdn[:, kk:kk + 4, :],
                     k.w_down[kk * 128:(kk + 4) * 128, :].rearrange("(f p) n -> p f n", p=128), writes=[b_c])
        g3 = sb("g3", [128, D], F32)
        g4 = sb("g4", [128, D], F32)
        S.dma_op("sp", g3[:], k.norms[2:3, :].partition_broadcast(128), writes=[b_c])
        S.dma_op("sp", g4[:], k.norms[3:4, :].partition_broadcast(128), writes=[b_c])
        nhalf = sb("nhalfE", [128, 1], F32)
        S.op("pool", lambda: nc.gpsimd.memset(nhalf[:], -0.5), writes=[b_c])
        S.barrier()
        h1 = [sb("h1_%d" % i, [128, 2, D], F32) for i in range(2)]; b_h1 = [Buf("h1_%d" % i) for i in range(2)]
        jb = sb("jbE", [128, D], BF16); b_jb = Buf("jbE")
        st = [sb("stE%d" % i, [128, 2, 8], F32) for i in range(2)]; b_st = [Buf("stE%d" % i) for i in range(2)]
        n2 = [sb("n2_%d" % i, [128, D], BF16) for i in range(2)]; b_n2 = [Buf("n2_%d" % i) for i in range(2)]
        n2T = [sb("n2T%d" % i, [128, 8, 256], BF16) for i in range(2)]; b_n2T = [Buf("n2T%d" % i) for i in range(2)]
        uT1 = sb("uT0", [128, 32, 256], BF16); b_uT1 = Buf("uT0")
        uT = [uT1, uT1]; b_uT = [b_uT1, b_uT1]
        rl = [sb("rl%d" % i, [128, 512], F32) for i in range(2)]; b_rl = [Buf("rl%d" % i) for i in range(2)]
        mo = [sb("mo%d" % i, [128, D], F32) for i in range(2)]; b_mo = [Buf("mo%d" % i) for i in range(2)]
        cnt = {"r": 0, "n": 0, "m": 0}
        NG = (NT - 1) // 2

        def head(gi):
            p = gi % 2
            for t in range(2):
                i = 1 + 2 * gi + t
                r0, r1 = i * 128, (i + 1) * 128
                S.dma_op("sp", h1[p][:, t, :], k.H1[r0:r1, :], writes=[b_h1[p]])
            for t in range(2):
                S.op("act", lambda: nc.scalar.activation(out=jb[:], in_=h1[p][:, t, :], func=AF.Square,
                                                         accum_out=st[p][:, t, 0:1]),
                     reads=[b_h1[p]], writes=[b_jb, b_st[p]])
                S.op("pool", lambda: nc.gpsimd.tensor_scalar(out=st[p][:, t, 1:2], in0=st[p][:, t, 0:1],
                                                             scalar1=1.0 / D, scalar2=EPS, op0=ALU.mult, op1=ALU.add),
                     reads=[b_st[p]], writes=[b_st[p]])
                S.op("pool", lambda: nc.gpsimd.tensor_tensor(out=st[p][:, t, 2:3], in0=st[p][:, t, 1:2], in1=nhalf[:],
                                                             op=ALU.pow), reads=[b_st[p], b_c], writes=[b_st[p]])
                np_ = cnt["n"] % 2
                cnt["n"] += 1
                S.op("dve", lambda: nc.vector.scalar_tensor_tensor(out=n2[np_][:], in0=h1[p][:, t, :],
                                                                   scalar=st[p][:, t, 2:3], in1=g3[:],
                                                                   op0=ALU.mult, op1=ALU.mult),
                     reads=[b_h1[p], b_st[p], b_c], writes=[b_n2[np_]])
                ps, bp = k.nextps()
                psb = ps[:].bitcast(BF16).rearrange("p (k t) -> p k t", k=8)
                fns = [(lambda kk=kk: nc.tensor.transpose(psb[:, kk, :], n2[np_][:, kk * 128:(kk + 1) * 128],
                                                          k.identb[:])) for kk in range(8)]
                S.group("pe", fns, reads=[b_n2[np_], k.b_ident], writes=[bp])
                S.op("act", lambda: nc.scalar.copy(out=n2T[p][:, :, t * 128:(t + 1) * 128], in_=psb),
                     reads=[bp], writes=[b_n2T[p]])

        def up(gi):
            p = gi % 2
            for fb in range(16):
                ps, bp = k.nextps()
                fns = []
                for f2 in range(2):
                    f = fb * 2 + f2
                    for kk in range(8):
                        fns.append(lambda f=f, f2=f2, kk=kk: nc.tensor.matmul(
                            ps[:, f2 * 256:(f2 + 1) * 256], lhsT=wu[:, kk, f * 128:(f + 1) * 128], rhs=n2T[p][:, kk, :],
                            start=(kk == 0), stop=(kk == 7)))
                S.group("pe", fns, reads=[b_n2T[p], b_c], writes=[bp])
                rp = cnt["r"] % 2
                cnt["r"] += 1
                S.op("act", lambda: nc.scalar.activation(out=rl[rp][:], in_=ps[:], func=AF.Relu),
                     reads=[bp], writes=[b_rl[rp]])
                S.op("dve", lambda: nc.vector.tensor_tensor(
                    out=uT[p][:, fb * 2:fb * 2 + 2, :].rearrange("p f t -> p (f t)"), in0=rl[rp][:], in1=rl[rp][:],
                    op=ALU.mult), reads=[b_rl[rp]], writes=[b_uT[p]])

        def down(gi):
            p = gi % 2
            for t in range(2):
                i = 1 + 2 * gi + t
                r0, r1 = i * 128, (i + 1) * 128
                mp = cnt["m"] % 2
                cnt["m"] += 1
                for cb in range(2):
                    ps, bp = k.nextps()
                    fns = [(lambda f=f: nc.tensor.matmul(ps[:], lhsT=uT[p][:, f, t * 128:(t + 1) * 128],
                                                         rhs=wdn[:, f, cb * 512:(cb + 1) * 512],
                                                         start=(f == 0), stop=(f == 31))) for f in range(32)]
                    S.group("pe", fns, reads=[b_uT[p], b_c], writes=[bp])
                    S.op("act", lambda: nc.scalar.copy(out=mo[mp][:, cb * 512:(cb + 1) * 512], in_=ps[:]),
                         reads=[bp], writes=[b_mo[mp]])
                S.op("act", lambda: nc.scalar.activation(out=jb[:], in_=mo[mp][:], func=AF.Square,
                                                         accum_out=st[p][:, t, 4:5]),
                     reads=[b_mo[mp]], writes=[b_jb, b_st[p]])
                S.op("pool", lambda: nc.gpsimd.tensor_scalar(out=st[p][:, t, 5:6], in0=st[p][:, t, 4:5],
                                                             scalar1=1.0 / D, scalar2=EPS, op0=ALU.mult, op1=ALU.add),
                     reads=[b_st[p]], writes=[b_st[p]])
                S.op("pool", lambda: nc.gpsimd.tensor_tensor(out=st[p][:, t, 6:7], in0=st[p][:, t, 5:6], in1=nhalf[:],
                                                             op=ALU.pow), reads=[b_st[p], b_c], writes=[b_st[p]])
                S.op("dve", lambda: nc.vector.scalar_tensor_tensor(out=mo[mp][:], in0=mo[mp][:],
                                                                   scalar=st[p][:, t, 6:7], in1=g4[:],
                                                                   op0=ALU.mult, op1=ALU.mult),
                     reads=[b_mo[mp], b_st[p], b_c], writes=[b_mo[mp]])
                S.op("pool", lambda: nc.gpsimd.tensor_tensor(out=mo[mp][:], in0=mo[mp][:], in1=h1[p][:, t, :],
                                                             op=ALU.add),
                     reads=[b_mo[mp], b_h1[p]], writes=[b_mo[mp]])
                S.dma_op("sp", k.out[r0 - 128:r1 - 128, :], mo[mp][:], reads=[b_mo[mp]])

        head(0)
        for gi in range(NG):
            up(gi)
            if gi + 1 < NG:
                head(gi + 1)
            down(gi)
        S.barrier()

def host_consts():
    pos = np.concatenate([np.zeros(PAD, np.float32), np.arange(T - PAD, dtype=np.float32)])

    def tabs(dim, reps):
        inv = (10000.0 ** (-np.arange(0, dim, 2, dtype=np.float32) / dim)).astype(np.float32)
        ang = pos[:, None] * inv[None, :]
        c = np.cos(ang).astype(np.float32)
        s = np.sin(ang).astype(np.float32)
        cT = np.concatenate([c, c], 1).T
        sT = np.concatenate([-s, s], 1).T
        return (np.ascontiguousarray(np.tile(cT, (reps, 1))), np.ascontiguousarray(np.tile(sT, (reps, 1))))
    cosA, sinA = tabs(128, 1)
    cosI, sinI = tabs(64, 2)
    r = np.arange(128)
    NEG = np.float32(-1e30)
    mdiag = np.where(r[None, :] <= r[:, None], 0.0, NEG).astype(np.float32)
    m0 = np.where((r[None, :] <= r[:, None]) & (r[None, :] >= PAD), 0.0, NEG).astype(np.float32)
    mpad = np.where(r[None, :] >= PAD, 0.0, NEG).astype(np.float32) * np.ones((128, 1), np.float32)
    masks = np.ascontiguousarray(np.stack([mdiag, m0, mpad], 0))
    pow2 = (2.0 ** -(np.arange(15, dtype=np.float32) + 1))[None, :].astype(np.float32)
    same = (r[:, None] // 64 == r[None, :] // 64)
    UT = (same & (r[:, None] <= r[None, :])).astype(np.float32)
    SAME = same.astype(np.float32)
    nSL = -(same & (r[:, None] > r[None, :])).astype(np.float32)
    nSU = -(same & (r[:, None] < r[None, :])).astype(np.float32)
    gmasks = np.ascontiguousarray(np.stack([UT, SAME, nSL, nSU], 0))
    cmask = np.stack([(r < 64), (r >= 64)], 1).astype(np.float32)
    return dict(cosA=cosA, sinA=sinA, cosI=cosI, sinI=sinI, ident=np.eye(128, dtype=np.float32),
                masks=masks, pow2=pow2, gmasks=gmasks, cmask=np.ascontiguousarray(cmask))


def swap_halves(w, hd):
    d, n = w.shape
    w = w.reshape(d, n // hd, 2, hd // 2)
    return np.ascontiguousarray(w[:, :, ::-1, :]).reshape(d, n)


def host_inputs(inputs):
    w_in = np.ascontiguousarray(inputs["w_in"][0])
    ik = w_in[:, C_IK:C_IK + 64]
    ikd = np.concatenate([ik, ik], 1)
    groups = []
    aq = w_in[:, C_AQ:C_AQ + 1024]
    aqs = swap_halves(aq, 128)
    for h in range(8):
        groups += [aq[:, h * 128:(h + 1) * 128], aqs[:, h * 128:(h + 1) * 128]]
    ak = w_in[:, C_AK:C_AK + 256]
    aks = swap_halves(ak, 128)
    for h in range(2):
        groups += [ak[:, h * 128:(h + 1) * 128], aks[:, h * 128:(h + 1) * 128]]
    iq = w_in[:, C_IQ:C_IQ + 512]
    iqs = swap_halves(iq, 64)
    for h in range(4):
        groups += [iq[:, h * 128:(h + 1) * 128], iqs[:, h * 128:(h + 1) * 128]]
    groups += [ikd, swap_halves(ikd, 64)]
    w_sw = np.concatenate(groups, 1)
    common = dict(
        meta=np.ascontiguousarray(inputs["meta_tokens"]),
        w_in=w_in, w_sw=np.ascontiguousarray(w_sw),
        conv_w=np.ascontiguousarray(inputs["conv_w"][0]),
        conv_wT=np.ascontiguousarray(inputs["conv_w"][0].T),
        a_log=np.ascontiguousarray(inputs["a_log"]), dt_bias=np.ascontiguousarray(inputs["dt_bias"]),
        gdn_norm=np.ascontiguousarray(inputs["gdn_norm"]),
        wg=np.ascontiguousarray(inputs["w_branch_gdn"][0]), wd=np.ascontiguousarray(inputs["w_branch_dsa"][0]),
        wo=np.ascontiguousarray(inputs["w_out"][0]),
        w_up=np.ascontiguousarray(inputs["w_up"][0]), w_down=np.ascontiguousarray(inputs["w_down"][0]),
        norms=np.ascontiguousarray(np.concatenate([inputs["pre_mix_norm"], inputs["post_mix_norm"],
                                                   inputs["pre_mlp_norm"], inputs["post_mlp_norm"]], 0)),
    )
    common.update(host_consts())
    return common


def kernel(**inputs):
    inputs = {k_: np.asarray(v) for k_, v in inputs.items()}
    nc = build()
    common = host_inputs(inputs)
    in_maps = []
    for b in range(8):
        m = dict(common)
        m["x"] = np.ascontiguousarray(inputs["x"][b])
        in_maps.append(m)
    res = run_bass_kernel_spmd(nc, in_maps, core_ids=list(range(8)))
    return np.stack([r["out"] for r in res.results], 0).astype(np.float32)
```

```python
import numpy as np
import concourse.bass as bass
import concourse.mybir as mybir
from concourse.bass_utils import run_bass_kernel_spmd
from contextlib import ExitStack

F32 = mybir.dt.float32
BF16 = mybir.dt.bfloat16
ALU = mybir.AluOpType
AF = mybir.ActivationFunctionType
AX = mybir.AxisListType

T = 4224
NT = 33
PAD = 112
D = 1024
EPS = 1e-6
C_GQ, C_GK, C_GV, C_GZ, C_GB, C_GA = 0, 1024, 2048, 3072, 4096, 4104
C_AQ, C_AK, C_AV, C_IQ, C_IK, C_IW, C_GTA, C_GTB = 4112, 5136, 5392, 5648, 6160, 6224, 6232, 7256
N_IN = 8280


class Buf:
    __slots__ = ("name", "w", "r")

    def __init__(self, name):
        self.name = name
        self.w = None
        self.r = {}


class Tok:
    __slots__ = ("key", "val", "hist")

    def __init__(self, key, val, hist):
        self.key = key
        self.val = val
        self.hist = hist


class Eng:
    def __init__(self, name, handle, sem, key):
        self.name = name
        self.h = handle
        self.sem = sem
        self.key = key
        self.count = 0
        self.seen = {}
        self.snap = {}
        self.nwaits = 0
        self.ninstr = 0


class Sched:
    NDMA = 24

    def __init__(self, nc, es):
        self.nc = nc
        self.es = es
        self.sems = []
        self.E = {}
        for name, h in [("pe", nc.tensor), ("dve", nc.vector), ("act", nc.scalar),
                        ("pool", nc.gpsimd), ("sp", nc.sync)]:
            sem = es.enter_context(nc.semaphore("s_" + name))
            e = Eng(name, h, sem, len(self.sems))
            self.sems.append(sem)
            self.E[name] = e
        self.dma = []
        for i in range(self.NDMA):
            sem = es.enter_context(nc.semaphore("d%d" % i))
            self.dma.append([len(self.sems), 0])
            self.sems.append(sem)
        self.dma_rr = 0

    def _wait(self, e, tok):
        if tok is None:
            return
        if e.seen.get(tok.key, 0) >= tok.val:
            return
        if tok.key == e.key and e.name == "pe":
            return
        e.h.wait_ge(self.sems[tok.key], tok.val)
        e.nwaits += 1
        e.seen[tok.key] = tok.val
        if tok.hist:
            for k, v in tok.hist.items():
                if e.seen.get(k, 0) < v:
                    e.seen[k] = v
        e.snap = None

    def _deps(self, e, reads, writes):
        for b in reads:
            self._wait(e, b.w)
        for b in writes:
            self._wait(e, b.w)
            for t in b.r.values():
                self._wait(e, t)

    def _commit(self, tok, reads, writes):
        for b in reads:
            o = b.r.get(tok.key)
            if o is None or o.val < tok.val:
                b.r[tok.key] = tok
        for b in writes:
            b.w = tok
            b.r = {}

    def op(self, eng, fn, reads=(), writes=()):
        e = self.E[eng]
        self._deps(e, reads, writes)
        ins = fn()
        e.count += 1
        e.ninstr += 1
        ins.then_inc(e.sem, 1)
        if e.snap is None:
            e.snap = dict(e.seen)
        tok = Tok(e.key, e.count, e.snap)
        self._commit(tok, reads, writes)
        return tok

    def group(self, eng, fns, reads=(), writes=()):
        e = self.E[eng]
        self._deps(e, reads, writes)
        ins = None
        for fn in fns:
            ins = fn()
            e.ninstr += 1
        e.count += 1
        ins.then_inc(e.sem, 1)
        if e.snap is None:
            e.snap = dict(e.seen)
        tok = Tok(e.key, e.count, e.snap)
        self._commit(tok, reads, writes)
        return tok

    def dma_op(self, eng, out, in_, reads=(), writes=(), **kw):
        e = self.E[eng]
        if eng == "pool":
            sem = self.es.enter_context(self.nc.semaphore("q%d" % len(self.sems)))
            slot = [len(self.sems), 0]
            self.sems.append(sem)
            self.dma.append(slot)
        else:
            slot = self.dma[self.dma_rr]
            self.dma_rr = (self.dma_rr + 1) % self.NDMA
        key = slot[0]
        if slot[1] > 0:
            self._wait(e, Tok(key, slot[1], None))
        self._deps(e, reads, writes)
        slot[1] += 16
        ins = e.h.dma_start(out=out, in_=in_, **kw)
        ins.then_inc(self.sems[key], 16)
        e.ninstr += 1
        tok = Tok(key, slot[1], None)
        self._commit(tok, reads, writes)
        return tok

    def barrier(self):
        for e in self.E.values():
            for o in self.E.values():
                if o is not e and o.count > 0:
                    self._wait(e, Tok(o.key, o.count, None))
            for key, val in self.dma:
                if val > 0:
                    self._wait(e, Tok(key, val, None))

    def finish(self):
        self.barrier()
        for e in self.E.values():
            if e.count > 0 and e.name != "pe":
                e.h.wait_ge(e.sem, e.count)

    def stats(self):
        return {n: (e.ninstr, e.nwaits) for n, e in self.E.items()}


class K:
    pass


def build(debug=False, phases="ABCDE"):
    nc = bass.Bass("TRN2", target_bir_lowering=False)
    k = K()
    k.nc = nc
    k.debug = debug

    def din(name, shape, dt=F32):
        return nc.dram_tensor(name, list(shape), dt, kind="ExternalInput").ap()

    def dscr(name, shape, dt):
        kind = "ExternalOutput" if debug else "Internal"
        return nc.dram_tensor(name, list(shape), dt, kind=kind).ap()

    k.x = din("x", [4096, D])
    k.meta = din("meta", [16, D])
    k.w_in = din("w_in", [D, N_IN])
    k.w_sw = din("w_sw", [D, 15 * 256])
    k.conv_w = din("conv_w", [4, 3072])
    k.conv_wT = din("conv_wT", [3072, 4])
    k.a_log = din("a_log", [1, 8])
    k.dt_bias = din("dt_bias", [1, 8])
    k.gdn_norm = din("gdn_norm", [1, 128])
    k.wg = din("wg", [D, D])
    k.wd = din("wd", [D, D])
    k.wo = din("wo", [D, D])
    k.w_up = din("w_up", [D, 4096])
    k.w_down = din("w_down", [4096, D])
    k.norms = din("norms", [4, D])
    k.ident_d = din("ident", [128, 128])
    k.cosA = din("cosA", [128, T])
    k.sinA = din("sinA", [128, T])
    k.cosI = din("cosI", [128, T])
    k.sinI = din("sinI", [128, T])
    k.out = nc.dram_tensor("out", [4096, D], F32, kind="ExternalOutput").ap()

    k.QG = dscr("QG", [T, 1024], BF16)
    k.KG = dscr("KG", [T, 1024], BF16)
    k.VG = dscr("VG", [T, 1024], BF16)
    k.ZS = dscr("ZS", [T, 1024], F32)
    k.BG = dscr("BG", [T, 16], F32)
    k.QT = dscr("QT", [8, 128, T], BF16)
    k.KT = dscr("KT", [2, 128, T], BF16)
    k.Vd = dscr("V", [T, 256], BF16)
    k.QIT = dscr("QIT", [4, 128, T], BF16)
    k.KIT = dscr("KIT", [128, T], BF16)
    k.IW = dscr("IW", [T, 8], F32)
    k.SGA = dscr("SGA", [T, 1024], F32)
    k.SGB = dscr("SGB", [T, 1024], F32)
    k.MDSA = dscr("MDSA", [T, 1024], F32)
    k.H1 = dscr("H1", [T, 1024], F32)
    if debug:
        k.YGDN = dscr("YGDN", [T, 1024], BF16)
    k.gmasks = din("gmasks", [4, 128, 128])
    k.cmask = din("cmask", [128, 2])
    k.masks = din("masks", [3, 128, 128])
    k.pow2 = din("pow2", [1, 15])

    with ExitStack() as es:
        S = Sched(nc, es)
        k.S = S
        k.es = es

        def sb(name, shape, dt, stack=es):
            return stack.enter_context(nc.sbuf_tensor(name, list(shape), dt))
        k.sb = sb

        k.PSB = [es.enter_context(nc.psum_tensor("psb%d" % i, [128, 1024], F32)) for i in range(4)]
        k.PS = [k.PSB[i // 2][:, (i % 2) * 512:(i % 2 + 1) * 512] for i in range(8)]
        k.PB = [Buf("ps%d" % i) for i in range(8)]
        k.ps_rr = 0
        k.ps_n = 8

        k.ps_base = 0

        k.cur_stream = None
        k.ps_pools = {}
        k.ps_rrs = {}

        def nextps():
            if k.cur_stream is not None:
                base, n = k.ps_pools[k.cur_stream]
                i = k.ps_rrs.get(k.cur_stream, 0) % n
                k.ps_rrs[k.cur_stream] = i + 1
                return k.PS[base + i], k.PB[base + i]
            i = k.ps_rr % k.ps_n
            k.ps_rr = (i + 1) % k.ps_n
            return k.PS[k.ps_base + i], k.PB[k.ps_base + i]
        k.nextps = nextps

        k.identf = sb("identf", [128, 128], F32)
        k.identb = sb("identb", [128, 128], BF16)
        k.b_ident = Buf("ident")
        S.dma_op("sp", k.identf[:], k.ident_d, writes=[k.b_ident])
        S.op("dve", lambda: nc.vector.tensor_copy(k.identb[:], k.identf[:]), reads=[k.b_ident], writes=[k.b_ident])

        if "A" in phases:
            phase_A(k)
            S.barrier()
        if "C" in phases:
            phase_C(k)
        if "B" in phases:
            phase_B(k)
        if "E" in phases:
            phase_E(k)
        S.finish()
        print("instr stats", S.stats())
    return nc


def phase_A(k):
    nc, S = k.nc, k.S
    with ExitStack() as es:
        cur = [es]

        def sb(name, shape, dt):
            return cur[0].enter_context(nc.sbuf_tensor(name, list(shape), dt))

        def substack():
            st = ExitStack()
            cur[0] = st
            return st

        def endsub(st):
            S.barrier()
            st.close()
            cur[0] = es

        nT = sb("nT", [128, 8, 3 + T], BF16)
        b_nT = [Buf("nT%d" % i) for i in range(NT)]
        b_nTpad = Buf("nTpad")
        S.op("pool", lambda: nc.gpsimd.memset(nT[:, :, 0:3], 0.0), writes=[b_nTpad])

        ysb = [sb("ysb%d" % i, [128, 512], F32) for i in range(2)]
        b_ysb = [Buf("ysb%d" % i) for i in range(2)]
        ob = [sb("ob%d" % i, [128, 512], BF16) for i in range(3)]
        b_ob = [Buf("ob%d" % i) for i in range(3)]
        of = [sb("of%d" % i, [128, 512], F32) for i in range(3)]
        b_of = [Buf("of%d" % i) for i in range(3)]
        sq = [sb("sq%d" % i, [128, 8], F32) for i in range(2)]
        b_sq = [Buf("sq%d" % i) for i in range(2)]
        nhalf = sb("nhalf", [128, 4], F32)
        b_nhalf = Buf("nhalf")
        S.op("pool", lambda: nc.gpsimd.memset(nhalf[:], -0.5), writes=[b_nhalf])
        junkf = sb("junkf", [128, 128], F32)
        b_junkf = Buf("junkf")

        st = substack()
        g1 = sb("g1", [128, D], F32)
        b_g1 = Buf("g1")
        S.dma_op("sp", g1[:], k.norms[0:1, :].partition_broadcast(128), writes=[b_g1])

        ht = [sb("ht%d" % i, [128, D], F32) for i in range(2)]
        b_ht = [Buf("ht%d" % i) for i in range(2)]
        junk = sb("junkA", [128, D], BF16)
        b_junk = Buf("junkA")
        nb = [sb("nb%d" % i, [128, D], BF16) for i in range(2)]
        b_nb = [Buf("nb%d" % i) for i in range(2)]
        ss = [sb("ssA%d" % i, [128, 4], F32) for i in range(2)]
        b_ss = [Buf("ssA%d" % i) for i in range(2)]

        for i in range(NT):
            p = i % 2
            h_, bh = ht[p], b_ht[p]
            if i == 0:
                S.op("pool", lambda: nc.gpsimd.memset(h_[:], 0.0), writes=[bh])
                S.dma_op("sp", h_[PAD:128, :], k.meta, writes=[bh])
            else:
                S.dma_op("sp", h_[:], k.x[(i - 1) * 128:i * 128, :], writes=[bh])
            s_, bs = ss[p], b_ss[p]
            S.op("act", lambda: nc.scalar.activation(out=junk[:], in_=h_[:], func=AF.Square,
                                                     accum_out=s_[:, 0:1]),
                 reads=[bh], writes=[b_junk, bs])
            S.op("act", lambda: nc.scalar.activation(out=s_[:, 1:2], in_=s_[:, 0:1], func=AF.Sqrt,
                                                     scale=1.0 / D, bias=EPS),
                 reads=[bs], writes=[bs])
            S.op("dve", lambda: nc.vector.reciprocal(s_[:, 2:3], s_[:, 1:2]), reads=[bs], writes=[bs])
            n_, bn = nb[p], b_nb[p]
            S.op("dve", lambda: nc.vector.scalar_tensor_tensor(out=n_[:], in0=h_[:], scalar=s_[:, 2:3],
                                                               in1=g1[:], op0=ALU.mult, op1=ALU.mult),
                 reads=[bh, bs, b_g1], writes=[bn])
            ps, bp = k.nextps()
            psb = ps[:].bitcast(BF16).rearrange("p (k t) -> p k t", k=8)
            fns = []
            for kk in range(8):
                fns.append(lambda kk=kk: nc.tensor.transpose(psb[:, kk, :], n_[:, kk * 128:(kk + 1) * 128],
                                                             k.identb[:]))
            S.group("pe", fns, reads=[bn, k.b_ident], writes=[bp])
            S.op("dve", lambda: nc.vector.tensor_copy(nT[:, :, 3 + i * 128:3 + (i + 1) * 128], psb),
                 reads=[bp], writes=[b_nT[i]])

        endsub(st)
        st = substack()
        cnt = {"g": 0, "t": 0, "g2": 0}
        wstg = sb("wstg", [128, 8, 128], F32); b_wstg = Buf("wstg")
        wc = [sb("wc%d" % i, [128, 8, 128], BF16) for i in range(2)]; b_wc = [Buf("wc%d" % i) for i in range(2)]
        cwT = [sb("cwT%d" % i, [128, 4], F32) for i in range(2)]; b_cwT = [Buf("cwT%d" % i) for i in range(2)]
        pT = [sb("pT%d" % i, [128, 3 + T], F32) for i in range(2)]
        b_pT = [[Buf("pT%d_%d" % (i, tb)) for tb in range(9)] for i in range(2)]
        b_pTpad = [Buf("pTpad%d" % i) for i in range(2)]
        for i in range(2):
            S.op("pool", (lambda i=i: nc.gpsimd.memset(pT[i][:, 0:3], 0.0)), writes=[b_pTpad[i]])
        accs = [sb("accs%d" % i, [128, 512], F32) for i in range(2)]; b_accs = [Buf("accs%d" % i) for i in range(2)]
        yTb = [sb("yTb%d" % i, [128, 512], BF16) for i in range(2)]; b_yTb = [Buf("yTb%d" % i) for i in range(2)]
        tmf = [sb("tmf%d" % i, [128, 4, 128], F32) for i in range(2)]; b_tmf = [Buf("tmf%d" % i) for i in range(2)]
        sqf = sb("sqf", [128, 4, 128], F32); b_sqf = Buf("sqf")
        obt = [sb("obt%d" % i, [128, 4, 128], BF16) for i in range(3)]; b_obt = [Buf("obt%d" % i) for i in range(3)]

        blocks = [(g, tb) for g in range(24) for tb in range(9)]
        gbuf = {}

        def conv_X(bi):
            g, tb = blocks[bi]
            cc0 = g * 128
            if tb == 0:
                gp = cnt["g"] % 2
                cnt["g"] += 1
                gbuf[g] = gp
                S.dma_op("sp", wstg[:], k.w_in[:, cc0:cc0 + 128].rearrange("(k p) n -> p k n", p=128),
                         writes=[b_wstg])
                S.op("pool", lambda: nc.gpsimd.tensor_copy(wc[gp][:], wstg[:]), reads=[b_wstg], writes=[b_wc[gp]])
                S.dma_op("sp", cwT[gp][:], k.conv_wT[cc0:cc0 + 128, :], writes=[b_cwT[gp]])
            gp = gbuf[g]
            c0 = tb * 512
            n = min(512, T - c0)
            tiles = list(range(c0 // 128, (c0 + n) // 128))
            ps, bp = k.nextps()
            fns = [(lambda kk=kk: nc.tensor.matmul(ps[:, 0:n], lhsT=wc[gp][:, kk, :],
                                                   rhs=nT[:, kk, 3 + c0:3 + c0 + n],
                                                   start=(kk == 0), stop=(kk == 7))) for kk in range(8)]
            S.group("pe", fns, reads=[b_wc[gp]] + [b_nT[i] for i in tiles], writes=[bp])
            S.op("act", lambda: nc.scalar.copy(out=pT[gp][:, 3 + c0:3 + c0 + n], in_=ps[:, 0:n]),
                 reads=[bp], writes=[b_pT[gp][tb]])
            rdp = [b_pT[gp][tb], b_pT[gp][tb - 1] if tb > 0 else b_pTpad[gp]]
            ap = bi % 2
            S.op("act", lambda: nc.scalar.activation(out=accs[ap][:, 0:n], in_=pT[gp][:, c0:c0 + n],
                                                     func=AF.Copy, scale=cwT[gp][:, 0:1]),
                 reads=rdp + [b_cwT[gp]], writes=[b_accs[ap]])
            for j in range(1, 4):
                eng = "dve"
                h = nc.vector
                S.op(eng, (lambda j=j, h=h: h.scalar_tensor_tensor(
                    out=accs[ap][:, 0:n], in0=pT[gp][:, c0 + j:c0 + j + n], scalar=cwT[gp][:, j:j + 1],
                    in1=accs[ap][:, 0:n], op0=ALU.mult, op1=ALU.add)),
                    reads=rdp + [b_cwT[gp], b_accs[ap]], writes=[b_accs[ap]])

        def conv_Y(bi):
            g, tb = blocks[bi]
            kind = "qkv"[g // 8]
            hd = g % 8
            dst = {"q": k.QG, "k": k.KG, "v": k.VG}[kind]
            c0 = tb * 512
            n = min(512, T - c0)
            nt = n // 128
            ap = bi % 2
            t3 = bi % 3
            S.op("act", lambda: nc.scalar.activation(out=yTb[ap][:, 0:n], in_=accs[ap][:, 0:n], func=AF.Silu),
                 reads=[b_accs[ap]], writes=[b_yTb[ap]])
            ps2, bp2 = k.nextps()
            psb = ps2[:].bitcast(BF16).rearrange("p (k t) -> p k t", k=8)
            fns = [(lambda t=t: nc.tensor.transpose(psb[:, t, :], yTb[ap][:, t * 128:(t + 1) * 128],
                                                    k.identb[:])) for t in range(nt)]
            S.group("pe", fns, reads=[b_yTb[ap], k.b_ident], writes=[bp2])
            o_, bo = obt[t3], b_obt[t3]
            if kind == "v":
                S.op("act", lambda: nc.scalar.copy(out=o_[:, 0:nt, :], in_=psb[:, 0:nt, :]),
                     reads=[bp2], writes=[bo])
            else:
                tm, btm = tmf[ap], b_tmf[ap]
                q_, bq = sq[ap], b_sq[ap]
                S.op("dve", lambda: nc.vector.tensor_copy(tm[:, 0:nt, :], psb[:, 0:nt, :]),
                     reads=[bp2], writes=[btm])
                S.op("dve", lambda: nc.vector.tensor_tensor(out=sqf[:, 0:nt, :], in0=tm[:, 0:nt, :],
                                                            in1=tm[:, 0:nt, :], op=ALU.mult),
                     reads=[btm], writes=[b_sqf])
                S.op("dve", lambda: nc.vector.tensor_reduce(out=q_[:, 0:nt], in_=sqf[:, 0:nt, :], axis=AX.X,
                                                            op=ALU.add), reads=[b_sqf], writes=[bq])
                sc = 128.0 if kind == "q" else 1.0
                S.op("pool", lambda: nc.gpsimd.tensor_scalar(out=q_[:, 0:nt], in0=q_[:, 0:nt], scalar1=sc,
                                                             scalar2=sc * EPS, op0=ALU.mult, op1=ALU.add),
                     reads=[bq], writes=[bq])
                S.op("pool", lambda: nc.gpsimd.tensor_tensor(out=q_[:, 4:4 + nt], in0=q_[:, 0:nt],
                                                             in1=nhalf[:, 0:nt], op=ALU.pow),
                     reads=[bq, b_nhalf], writes=[bq])


            def tail():
                if kind != "v":
                    S.op("dve", lambda: nc.vector.tensor_tensor(
                        out=o_[:, 0:nt, :], in0=tm[:, 0:nt, :],
                        in1=q_[:, 4:4 + nt].unsqueeze(2).broadcast_to([128, nt, 128]), op=ALU.mult),
                        reads=[btm, bq], writes=[bo])
                S.dma_op("sp", dst[c0:c0 + n, hd * 128:(hd + 1) * 128].rearrange("(t p) c -> p t c", p=128),
                         o_[:, 0:nt, :], reads=[bo])
            return tail

        def conv_group(cc0, kind):
            gp = cnt["g"] % 2
            cnt["g"] += 1
            S.dma_op("sp", wst[0][:], k.w_in[:, cc0:cc0 + 512].rearrange("(k p) n -> p k n", p=128),
                     writes=[b_wst[0]])
            S.dma_op("sp", cw[0][:], k.conv_w[:, cc0:cc0 + 512].partition_broadcast(128),
                     writes=[b_cw[0]])
            for j in range(4):
                S.op("dve", lambda: nc.vector.tensor_tensor(
                    out=w4[gp][:, :, j, :], in0=wst[0][:],
                    in1=cw[0][:, j:j + 1, :].broadcast_to([128, 8, 512]), op=ALU.mult),
                    reads=[b_wst[0], b_cw[0]], writes=[b_w4[gp]])
            dst = {"q": k.QG, "k": k.KG, "v": k.VG}[kind]
            dcol = cc0 % 1024
            for i in range(NT):
                ps, bp = k.nextps()
                fns = []
                for j in range(4):
                    for kk in range(8):
                        fns.append(lambda j=j, kk=kk: nc.tensor.matmul(
                            ps[:], lhsT=nT[:, kk, i * 128 + j:i * 128 + j + 128], rhs=w4[gp][:, kk, j, :],
                            start=(j == 0 and kk == 0), stop=(j == 3 and kk == 7)))
                rd = [b_w4[gp], b_nT[i], b_nTpad] + ([b_nT[i - 1]] if i > 0 else [])
                S.group("pe", fns, reads=rd, writes=[bp])
                tp = cnt["t"] % 2
                t3 = cnt["t"] % 3
                cnt["t"] += 1
                o_, bo = ob[t3], b_ob[t3]
                if kind == "v":
                    S.op("act", lambda: nc.scalar.activation(out=o_[:], in_=ps[:], func=AF.Silu),
                         reads=[bp], writes=[bo])
                else:
                    y_, by = ysb[tp], b_ysb[tp]
                    q_, bq = sq[tp], b_sq[tp]
                    S.op("act", lambda: nc.scalar.activation(out=y_[:], in_=ps[:], func=AF.Silu),
                         reads=[bp], writes=[by])
                    for hh in range(4):
                        S.op("dve", lambda: nc.vector.scalar_tensor_tensor(
                            out=junkf[:], in0=y_[:, hh * 128:(hh + 1) * 128], scalar=1.0,
                            in1=y_[:, hh * 128:(hh + 1) * 128], op0=ALU.mult, op1=ALU.mult,
                            accum_out=q_[:, hh:hh + 1]),
                            reads=[by], writes=[b_junkf, bq])
                    sc = 128.0 if kind == "q" else 1.0
                    S.op("pool", lambda: nc.gpsimd.tensor_scalar(out=q_[:, 0:4], in0=q_[:, 0:4], scalar1=sc,
                                                                 scalar2=sc * EPS, op0=ALU.mult, op1=ALU.add),
                         reads=[bq], writes=[bq])
                    S.op("pool", lambda: nc.gpsimd.tensor_tensor(out=q_[:, 4:8], in0=q_[:, 0:4], in1=nhalf[:],
                                                                 op=ALU.pow),
                         reads=[bq, b_nhalf], writes=[bq])
                    for hh in range(4):
                        S.op("dve", lambda: nc.vector.tensor_scalar(
                            out=o_[:, hh * 128:(hh + 1) * 128], in0=y_[:, hh * 128:(hh + 1) * 128],
                            scalar1=q_[:, 4 + hh:5 + hh], scalar2=None, op0=ALU.mult),
                            reads=[by, bq], writes=[bo])
                S.dma_op("sp", dst[i * 128:(i + 1) * 128, dcol:dcol + 512], o_[:], reads=[bo])

        def conv_stream():
            conv_X(0)
            pend_tail = None
            for bi in range(len(blocks)):
                if bi + 1 < len(blocks):
                    conv_X(bi + 1)
                yield
                tl = conv_Y(bi)
                if pend_tail is not None:
                    pend_tail()
                pend_tail = tl
                yield
            pend_tail()

        wt = [sb("wt%d" % i, [128, 8, 512], BF16) for i in range(2)]
        b_wt = [Buf("wt%d" % i) for i in range(2)]
        albc = sb("albc", [128, 16], F32)
        b_albc = Buf("albc")
        S.dma_op("sp", albc[:, 0:8], k.a_log.partition_broadcast(128), writes=[b_albc])
        S.dma_op("sp", albc[:, 8:16], k.dt_bias.partition_broadcast(128), writes=[b_albc])
        S.op("act", lambda: nc.scalar.activation(out=albc[:, 0:8], in_=albc[:, 0:8], func=AF.Exp),
             reads=[b_albc], writes=[b_albc])

        def plain_group(c0, ncols, post):
            gp = cnt["g2"] % 2
            cnt["g2"] += 1
            S.dma_op("pool", wt[gp][:, :, 0:ncols], k.w_in[:, c0:c0 + ncols].rearrange("(k p) n -> p k n", p=128),
                     writes=[b_wt[gp]])
            for i in range(NT):
                ps, bp = k.nextps()
                fns = []
                for kk in range(8):
                    fns.append(lambda kk=kk: nc.tensor.matmul(
                        ps[:, 0:ncols], lhsT=nT[:, kk, 3 + i * 128:3 + (i + 1) * 128], rhs=wt[gp][:, kk, 0:ncols],
                        start=(kk == 0), stop=(kk == 7)))
                S.group("pe", fns, reads=[b_wt[gp], b_nT[i]], writes=[bp])
                post(i, ps, bp)
                yield

        def post_act(func, dst, dcol, ncols, bf, scale=1.0):
            def post(i, ps, bp):
                t3 = cnt["t"] % 3
                cnt["t"] += 1
                o_, bo = (ob[t3], b_ob[t3]) if bf else (of[t3], b_of[t3])
                S.op("act", lambda: nc.scalar.activation(out=o_[:, 0:ncols], in_=ps[:, 0:ncols], func=func,
                                                         scale=scale),
                     reads=[bp], writes=[bo])
                S.dma_op("sp", dst[i * 128:(i + 1) * 128, dcol:dcol + ncols], o_[:, 0:ncols], reads=[bo])
            return post

        def post_bg(i, ps, bp):
            t3 = cnt["t"] % 3
            cnt["t"] += 1
            o_, bo = of[t3], b_of[t3]
            S.op("act", lambda: nc.scalar.activation(out=o_[:, 0:8], in_=ps[:, 0:8], func=AF.Sigmoid),
                 reads=[bp], writes=[bo])
            S.op("dve", lambda: nc.vector.tensor_tensor(out=o_[:, 16:24], in0=ps[:, 8:16], in1=albc[:, 8:16],
                                                        op=ALU.add),
                 reads=[bp, b_albc], writes=[bo])
            S.op("act", lambda: nc.scalar.activation(out=o_[:, 16:24], in_=o_[:, 16:24], func=AF.Exp),
                 reads=[bo], writes=[bo])
            S.op("act", lambda: nc.scalar.activation(out=o_[:, 16:24], in_=o_[:, 16:24], func=AF.Ln, bias=1.0),
                 reads=[bo], writes=[bo])
            S.op("dve", lambda: nc.vector.scalar_tensor_tensor(out=o_[:, 8:16], in0=o_[:, 16:24], scalar=-1.0,
                                                               in1=albc[:, 0:8], op0=ALU.mult, op1=ALU.mult),
                 reads=[bo, b_albc], writes=[bo])
            S.dma_op("sp", k.BG[i * 128:(i + 1) * 128, :], o_[:, 0:16], reads=[bo])
        cosT = sb("cosT", [128, T], F32)
        sinT = sb("sinT", [128, T], F32)
        b_tab = Buf("ropetab")
        wf = [sb("wf%d" % i, [128, 8, 256], BF16) for i in range(2)]
        b_wf = [Buf("wf%d" % i) for i in range(2)]
        t1 = [sb("t1_%d" % i, [128, 512], F32) for i in range(2)]
        b_t1 = [Buf("t1_%d" % i) for i in range(2)]
        t2 = [sb("t2_%d" % i, [128, 512], F32) for i in range(2)]
        b_t2 = [Buf("t2_%d" % i) for i in range(2)]

        def load_tab(c, s):
            S.dma_op("sp", cosT[:], c, writes=[b_tab])
            S.dma_op("sp", sinT[:], s, writes=[b_tab])

        def rope_group(w_ap, dst):
            gp = cnt["g2"] % 2
            cnt["g2"] += 1
            S.dma_op("pool", wf[gp][:], w_ap.rearrange("(k p) n -> p k n", p=128), writes=[b_wf[gp]])
            for tb in range(9):
                c0 = tb * 512
                n = min(512, T - c0)
                tiles = list(range(c0 // 128, (c0 + n) // 128))
                psA, bpA = k.nextps()
                psB, bpB = k.nextps()
                for (ps, bp, off) in ((psA, bpA, 0), (psB, bpB, 128)):
                    fns = []
                    for kk in range(8):
                        fns.append(lambda kk=kk, ps=ps, off=off: nc.tensor.matmul(
                            ps[:, 0:n], lhsT=wf[gp][:, kk, off:off + 128], rhs=nT[:, kk, 3 + c0:3 + c0 + n],
                            start=(kk == 0), stop=(kk == 7)))
                    S.group("pe", fns, reads=[b_wf[gp]] + [b_nT[i] for i in tiles], writes=[bp])
                tp = cnt["t"] % 2
                t3 = cnt["t"] % 3
                cnt["t"] += 1
                S.op("dve", lambda: nc.vector.tensor_tensor(out=t1[tp][:, 0:n], in0=psA[:, 0:n],
                                                            in1=cosT[:, c0:c0 + n], op=ALU.mult),
                     reads=[bpA, b_tab], writes=[b_t1[tp]])
                S.op("dve", lambda: nc.vector.tensor_tensor(out=t2[tp][:, 0:n], in0=psB[:, 0:n],
                                                            in1=sinT[:, c0:c0 + n], op=ALU.mult),
                     reads=[bpB, b_tab], writes=[b_t2[tp]])
                o_, bo = ob[t3], b_ob[t3]
                S.op("pool", lambda: nc.gpsimd.tensor_tensor(out=o_[:, 0:n], in0=t1[tp][:, 0:n],
                                                             in1=t2[tp][:, 0:n], op=ALU.add),
                     reads=[b_t1[tp], b_t2[tp]], writes=[bo])
                S.dma_op("sp", dst[:, c0:c0 + n], o_[:, 0:n], reads=[bo])
                yield

        def proj_stream():
            for cb in range(2):
                yield from plain_group(C_GZ + cb * 512, 512, post_act(AF.Silu, k.ZS, cb * 512, 512, False))
            for cb in range(2):
                yield from plain_group(C_GTA + cb * 512, 512, post_act(AF.Sigmoid, k.SGA, cb * 512, 512, False))
            for cb in range(2):
                yield from plain_group(C_GTB + cb * 512, 512, post_act(AF.Sigmoid, k.SGB, cb * 512, 512, False))
            yield from plain_group(C_AV, 256, post_act(AF.Copy, k.Vd, 0, 256, True))
            yield from plain_group(C_IW, 8, post_act(AF.Copy, k.IW, 0, 8, False, scale=512.0 ** -0.5))
            yield from plain_group(C_GB, 16, post_bg)
            load_tab(k.cosA, k.sinA)
            for h in range(8):
                yield from rope_group(k.w_sw[:, h * 256:(h + 1) * 256], k.QT[h])
            for h in range(2):
                yield from rope_group(k.w_sw[:, (8 + h) * 256:(9 + h) * 256], k.KT[h])
            load_tab(k.cosI, k.sinI)
            for h in range(4):
                yield from rope_group(k.w_sw[:, (10 + h) * 256:(11 + h) * 256], k.QIT[h])
            yield from rope_group(k.w_sw[:, 14 * 256:15 * 256], k.KIT)

        k.ps_pools = {"conv": (0, 3), "proj": (3, 5)}
        k.ps_rrs = {}
        gA, nA = conv_stream(), 2.0 * len(blocks)
        gB, nB = proj_stream(), 10.0 * NT + 15 * 9
        aA = aB = True
        pA = pB = 0.0
        while aA or aB:
            if aA and (not aB or pA <= pB):
                k.cur_stream = "conv"
                try:
                    next(gA)
                    pA += 1.0 / nA
                except StopIteration:
                    aA = False
            else:
                k.cur_stream = "proj"
                try:
                    next(gB)
                    pB += 1.0 / nB
                except StopIteration:
                    aB = False
        k.cur_stream = None
        endsub(st)


def phase_C(k):
    nc, S = k.nc, k.S
    NIT = 14
    with ExitStack() as es:
        def sb(name, shape, dt):
            return es.enter_context(nc.sbuf_tensor(name, list(shape), dt))
        kit = sb("kit", [128, T], BF16)
        kts = sb("kts", [128, 2, T], BF16)
        vs = sb("vs", [128, NT, 256], BF16)
        wd = sb("wd_sb", [128, 8, D], BF16)
        b_res = Buf("resC")
        S.dma_op("sp", kit[:], k.KIT, writes=[b_res])
        S.dma_op("sp", kts[:], k.KT.rearrange("g p t -> p g t"), writes=[b_res])
        S.dma_op("sp", vs[:], k.Vd.rearrange("(j p) c -> p j c", p=128), writes=[b_res])
        S.dma_op("pool", wd[:], k.wd.rearrange("(h p) n -> p h n", p=128), writes=[b_res])
        msk = sb("msk", [128, 3, 128], F32)
        S.dma_op("sp", msk[:], k.masks.rearrange("m p c -> p m c"), writes=[b_res])
        p2 = sb("p2", [128, NIT + 1], F32)
        S.dma_op("sp", p2[:], k.pow2.partition_broadcast(128), writes=[b_res])
        ones = sb("onesC", [128, 128], BF16)
        S.op("pool", lambda: nc.gpsimd.memset(ones[:], 1.0), writes=[b_res])
        S.barrier()

        I = sb("I", [128, T], F32); b_I = Buf("I")
        M = sb("M", [128, T], BF16); b_M = Buf("M")
        MT2 = [sb("MT%d" % i, [128, NT, 128], BF16) for i in range(2)]; b_MT2 = [Buf("MT%d" % i) for i in range(2)]
        jk = sb("jkC", [128, T], BF16); b_jk = Buf("jkC")
        qit = [sb("qit%d" % i, [128, 4, 128], BF16) for i in range(2)]; b_qit = [Buf("qit%d" % i) for i in range(2)]
        qt = [sb("qt%d" % i, [128, 8, 128], BF16) for i in range(2)]; b_qt = [Buf("qt%d" % i) for i in range(2)]
        iw = [sb("iw%d" % i, [128, 8], F32) for i in range(2)]; b_iw = [Buf("iw%d" % i) for i in range(2)]
        sgb = [sb("sgb%d" % i, [128, D], F32) for i in range(2)]; b_sgb = [Buf("sgb%d" % i) for i in range(2)]
        r_ = [sb("r_%d" % i, [128, 512], F32) for i in range(2)]; b_r = [Buf("r_%d" % i) for i in range(2)]
        e_ = [sb("e_%d" % i, [128, 512], BF16) for i in range(2)]; b_e = [Buf("e_%d" % i) for i in range(2)]
        p_ = [sb("p_%d" % i, [128, 512], BF16) for i in range(3)]; b_p = [Buf("p_%d" % i) for i in range(3)]
        rden = sb("rden", [128, 512], F32); b_rden = Buf("rden")
        ot = [sb("ot%d" % i, [128, 4, 128], BF16) for i in range(2)]; b_ot = [Buf("ot%d" % i) for i in range(2)]
        md = sb("md", [128, D], F32); b_md = Buf("md")
        st = sb("stC", [128, 8], F32); b_st = Buf("stC")
        w2 = sb("w2C", [128, NIT + 1], F32); b_w2 = Buf("w2C")
        tmpd = sb("tmpd", [128, 128], F32); b_tmpd = Buf("tmpd")
        midt = sb("midt", [128, 1], F32); b_mid = Buf("midt")
        cn = sb("cnC", [128, 1], F32); b_cn = Buf("cnC")
        sa = sb("saC", [128, 1], F32); b_sa = Buf("saC")
        gt = sb("gtC", [128, 2], F32); b_gt = Buf("gtC")
        jk2 = sb("jk2C", [128, T], BF16); b_jk2 = Buf("jk2C")
        k.ps_base = 5
        k.ps_n = 3
        k.ps_rr = 0
        psOg = [k.PS[3], k.PS[3]]
        bOg = [k.PB[3], k.PB[3]]
        psDg = [k.PS[4], k.PS[4]]
        bDg = [k.PB[4], k.PB[4]]
        att_rr = [0]

        def attps():
            i = att_rr[0] % 3
            att_rr[0] += 1
            return k.PS[i], k.PB[i]
        cnt = {"r": 0, "e": 0}
        SC = 128.0 ** -0.5

        def stage1a(i):
            nk = i + 1
            NK = nk * 128
            par = i % 2
            yield
            S.dma_op("sp", qit[par][:], k.QIT[:, :, i * 128:(i + 1) * 128].rearrange("g p t -> p g t"),
                     writes=[b_qit[par]])
            yield
            S.dma_op("sp", qt[par][:], k.QT[:, :, i * 128:(i + 1) * 128].rearrange("g p t -> p g t"),
                     writes=[b_qt[par]])
            yield
            S.dma_op("sp", iw[par][:], k.IW[i * 128:(i + 1) * 128, :], writes=[b_iw[par]])
            yield
            S.dma_op("sp", sgb[par][:], k.SGB[i * 128:(i + 1) * 128, :], writes=[b_sgb[par]])
            for c0 in range(0, NK, 512):
                n = min(512, NK - c0)
                for h in range(8):
                    g, hf = h // 2, h % 2
                    ps, bp = k.nextps()
                    yield
                    S.op("pe", lambda: nc.tensor.matmul(ps[:, 0:n], lhsT=qit[par][64 * hf:64 * hf + 64, g, :],
                                                        rhs=kit[64 * hf:64 * hf + 64, c0:c0 + n],
                                                        start=True, stop=True),
                         reads=[b_qit[par]], writes=[bp])
                    rp = cnt["r"] % 2
                    cnt["r"] += 1
                    yield
                    S.op("act", lambda: nc.scalar.activation(out=r_[rp][:, 0:n], in_=ps[:, 0:n], func=AF.Relu),
                         reads=[bp], writes=[b_r[rp]])
                    if h == 0:
                        yield
                        S.op("dve", lambda: nc.vector.tensor_scalar(out=I[:, c0:c0 + n], in0=r_[rp][:, 0:n],
                                                                    scalar1=iw[par][:, 0:1], scalar2=None,
                                                                    op0=ALU.mult),
                             reads=[b_r[rp], b_iw[par]], writes=[b_I])
                    else:
                        yield
                        S.op("dve", lambda: nc.vector.scalar_tensor_tensor(
                            out=I[:, c0:c0 + n], in0=r_[rp][:, 0:n], scalar=iw[par][:, h:h + 1],
                            in1=I[:, c0:c0 + n], op0=ALU.mult, op1=ALU.add),
                            reads=[b_r[rp], b_iw[par]], writes=[b_I])
            m0 = 1 if i == 0 else 2
            yield
            S.op("dve", lambda: nc.vector.tensor_tensor(out=I[:, 0:128], in0=I[:, 0:128], in1=msk[:, m0, :],
                                                        op=ALU.add), reads=[b_I], writes=[b_I])
            if i >= 1:
                yield
                S.op("dve", lambda: nc.vector.tensor_tensor(out=I[:, i * 128:NK], in0=I[:, i * 128:NK],
                                                            in1=msk[:, 0, :], op=ALU.add),
                     reads=[b_I], writes=[b_I])
            if i < 2:
                yield
                S.op("dve", lambda: nc.vector.memset(st[:, 6:7], -1e29), writes=[b_st])
            else:
                yield
                S.op("dve", lambda: nc.vector.tensor_reduce(out=st[:, 0:1], in_=I[:, 0:NK], axis=AX.X, op=ALU.max),
                     reads=[b_I], writes=[b_st])
                yield
                S.op("dve", lambda: nc.vector.tensor_reduce(out=st[:, 1:2], in_=I[:, PAD:i * 128], axis=AX.X,
                                                            op=ALU.min), reads=[b_I], writes=[b_st])
                if i == 2:
                    yield
                    S.op("dve", lambda: nc.vector.scalar_tensor_tensor(out=tmpd[:], in0=msk[:, 0, :], scalar=-2.0,
                                                                       in1=I[:, i * 128:NK], op0=ALU.mult,
                                                                       op1=ALU.add),
                         reads=[b_I], writes=[b_tmpd])
                    yield
                    S.op("dve", lambda: nc.vector.tensor_reduce(out=st[:, 7:8], in_=tmpd[:], axis=AX.X, op=ALU.min),
                         reads=[b_tmpd], writes=[b_st])
                    yield
                    S.op("dve", lambda: nc.vector.tensor_tensor(out=st[:, 1:2], in0=st[:, 1:2], in1=st[:, 7:8],
                                                                op=ALU.min), reads=[b_st], writes=[b_st])
                yield
                S.op("dve", lambda: nc.vector.tensor_tensor(out=st[:, 2:3], in0=st[:, 0:1], in1=st[:, 1:2],
                                                            op=ALU.subtract), reads=[b_st], writes=[b_st])
                yield
                S.op("dve", lambda: nc.vector.tensor_scalar(out=w2[:], in0=p2[:], scalar1=st[:, 2:3], scalar2=None,
                                                            op0=ALU.mult), reads=[b_st], writes=[b_w2])
                yield
                S.op("dve", lambda: nc.vector.tensor_tensor(out=st[:, 3:4], in0=st[:, 1:2], in1=w2[:, 0:1],
                                                            op=ALU.add), reads=[b_st, b_w2], writes=[b_st])
                hc = NK if nk <= 3 else 128 * max(1, int(round(0.45 * nk)))
                na = NK - hc
                yield
                S.op("dve", lambda: nc.vector.tensor_copy(midt[:], st[:, 3:4]), reads=[b_st], writes=[b_mid])
                for it in range(NIT):
                    if na > 0:
                        yield
                        S.op("act", lambda: nc.scalar.activation(out=jk2[:, 0:na], in_=I[:, hc:NK], func=AF.Sign,
                                                                 scale=-1.0, bias=midt[:, 0:1], accum_out=sa[:, 0:1]),
                             reads=[b_I, b_mid], writes=[b_jk2, b_sa])
                    yield
                    S.op("dve", lambda: nc.vector.tensor_scalar(out=jk[:, 0:hc], in0=I[:, 0:hc], scalar1=midt[:, 0:1],
                                                                scalar2=None, op0=ALU.is_ge, op1=ALU.add,
                                                                accum_out=cn[:, 0:1]),
                         reads=[b_I, b_mid], writes=[b_jk, b_cn])
                    if na > 0:
                        yield
                        S.op("dve", lambda: nc.vector.scalar_tensor_tensor(out=gt[:, 0:1], in0=sa[:, 0:1], scalar=-0.5,
                                                                           in1=cn[:, 0:1], op0=ALU.mult, op1=ALU.add),
                             reads=[b_sa, b_cn], writes=[b_gt])
                        src, bsrc = gt[:, 0:1], b_gt
                    else:
                        src, bsrc = cn[:, 0:1], b_cn
                    yield
                    S.op("dve", lambda: nc.vector.tensor_scalar(out=gt[:, 1:2], in0=src, scalar1=255.5 - 0.5 * na,
                                                                scalar2=-0.5, op0=ALU.is_ge, op1=ALU.add),
                         reads=[bsrc], writes=[b_gt])
                    yield
                    S.op("dve", lambda: nc.vector.scalar_tensor_tensor(out=midt[:, 0:1], in0=gt[:, 1:2],
                                                                       scalar=w2[:, it:it + 1], in1=midt[:, 0:1],
                                                                       op0=ALU.mult, op1=ALU.add),
                         reads=[b_gt, b_w2, b_mid], writes=[b_mid])
                yield
                S.op("dve", lambda: nc.vector.tensor_copy(st[:, 3:4], midt[:]), reads=[b_mid], writes=[b_st])
                yield
                S.op("dve", lambda: nc.vector.tensor_scalar(out=st[:, 6:7], in0=st[:, 3:4], scalar1=w2[:, NIT:NIT + 1],
                                                            scalar2=-1e29, op0=ALU.subtract, op1=ALU.max),
                     reads=[b_st, b_w2], writes=[b_st])
            yield
            S.op("dve", lambda: nc.vector.tensor_scalar(out=M[:, 0:NK], in0=I[:, 0:NK], scalar1=st[:, 6:7],
                                                        scalar2=None, op0=ALU.is_ge),
                 reads=[b_I, b_st], writes=[b_M])
        def stage1b(i):
            nk = i + 1
            MT, b_MT = MT2[i % 2], b_MT2[i % 2]
            for j0 in range(0, nk, 8):
                nj = min(8, nk - j0)
                ps, bp = k.nextps()
                psb = ps[:].bitcast(BF16).rearrange("p (k t) -> p k t", k=8)
                fns = [(lambda jj=jj: nc.tensor.transpose(psb[:, jj, :], M[:, (j0 + jj) * 128:(j0 + jj + 1) * 128],
                                                          k.identb[:])) for jj in range(nj)]
                yield
                S.group("pe", fns, reads=[b_M, k.b_ident], writes=[bp])
                yield
                S.op("act", lambda: nc.scalar.activation(out=MT[:, j0:j0 + nj, :], in_=psb[:, 0:nj, :], func=AF.Copy,
                                                         scale=30000.0, bias=-30000.0),
                     reads=[bp], writes=[b_MT])
        def stage2(i):
            nk = i + 1
            par = i % 2
            MT, b_MT = MT2[i % 2], b_MT2[i % 2]
            steps = [(g, j) for g in range(2) for j in range(nk)]

            def emitS(g, j):
                ps, bp = attps()
                S.group("pe", [
                    lambda: nc.tensor.matmul(ps[:], lhsT=kts[:, g, j * 128:(j + 1) * 128],
                                             rhs=qt[par][:, 4 * g:4 * g + 4, :], start=True, stop=False),
                    lambda: nc.tensor.matmul(ps[:], lhsT=k.identb[:],
                                             rhs=MT[:, j:j + 1, :].broadcast_to([128, 4, 128]),
                                             start=False, stop=True)],
                    reads=[b_qt[par], b_MT, k.b_ident], writes=[bp])
                return ps, bp
            pend = []
            for sidx in range(min(2, len(steps))):
                yield
                pend.append(emitS(*steps[sidx]))
            for sidx, (g, j) in enumerate(steps):
                ps, bp = pend.pop(0)
                ep = cnt["e"] % 3
                cnt["e"] += 1
                yield
                S.op("act", lambda: nc.scalar.activation(out=p_[ep][:], in_=ps[:], func=AF.Exp, scale=SC),
                     reads=[bp], writes=[b_p[ep]])
                if sidx + 2 < len(steps):
                    yield
                    pend.append(emitS(*steps[sidx + 2]))
                yield
                S.op("pe", lambda: nc.tensor.matmul(psOg[g][:], lhsT=vs[:, j, g * 128:(g + 1) * 128], rhs=p_[ep][:],
                                                    start=(j == 0), stop=(j == nk - 1)),
                     reads=[b_p[ep]], writes=[bOg[g]])
                yield
                S.op("pe", lambda: nc.tensor.matmul(psDg[g][:], lhsT=ones[:], rhs=p_[ep][:],
                                                    start=(j == 0), stop=(j == nk - 1)),
                     reads=[b_p[ep]], writes=[bDg[g]])
                if j == nk - 1:
                    yield
                    S.op("dve", lambda: nc.vector.tensor_scalar(out=rden[:], in0=psDg[g][:], scalar1=1e-20,
                                                                scalar2=None, op0=ALU.max),
                         reads=[bDg[g]], writes=[b_rden])
                    yield
                    S.op("dve", lambda: nc.vector.reciprocal(rden[:], rden[:]), reads=[b_rden], writes=[b_rden])
                    yield
                    S.op("dve", lambda: nc.vector.tensor_tensor(out=ot[g][:].rearrange("p h t -> p (h t)"),
                                                                in0=psOg[g][:], in1=rden[:], op=ALU.mult),
                         reads=[bOg[g], b_rden], writes=[b_ot[g]])
            for cb in range(2):
                ps, bp = attps()
                fns = [(lambda h=h: nc.tensor.matmul(ps[:], lhsT=ot[h // 4][:, h % 4, :],
                                                     rhs=wd[:, h, cb * 512:(cb + 1) * 512],
                                                     start=(h == 0), stop=(h == 7))) for h in range(8)]
                yield
                S.group("pe", fns, reads=[b_ot[0], b_ot[1]], writes=[bp])
                yield
                S.op("dve", lambda: nc.vector.tensor_tensor(out=md[:, cb * 512:(cb + 1) * 512], in0=ps[:],
                                                            in1=sgb[par][:, cb * 512:(cb + 1) * 512], op=ALU.mult),
                     reads=[bp, b_sgb[par]], writes=[b_md])
            yield
            S.dma_op("sp", k.MDSA[i * 128:(i + 1) * 128, :], md[:], reads=[b_md])

        def drain(g):
            for _ in g:
                pass

        def chain(*gs):
            for g in gs:
                yield from g

        def lockstep(gA, nA, gB, nB):
            aA = aB = True
            pA = pB = 0.0
            while aA or aB:
                if aA and (not aB or pA <= pB):
                    try:
                        next(gA)
                        pA += 1.0 / nA
                    except StopIteration:
                        aA = False
                else:
                    try:
                        next(gB)
                        pB += 1.0 / nB
                    except StopIteration:
                        aB = False

        def est1(i):
            nk = i + 1
            return 4 + ((nk + 3) // 4) * 24 + (NIT * 5 + 12 if i >= 2 else 3) + 2 * ((nk + 7) // 8)

        def est2(i):
            return 8 * (i + 1) + 12

        drain(chain(stage1a(0), stage1b(0)))
        for i in range(NT):
            if i + 1 < NT:
                lockstep(stage2(i), est2(i), chain(stage1a(i + 1), stage1b(i + 1)), est1(i + 1))
            else:
                drain(stage2(i))
        k.ps_base = 0
        k.ps_n = 8
        S.barrier()


def phase_B(k):
    nc, S = k.nc, k.S
    with ExitStack() as es:
        def sb(name, shape, dt):
            return es.enter_context(nc.sbuf_tensor(name, list(shape), dt))
        b_c = Buf("constB")
        gm = sb("gm", [128, 4, 128], F32)
        S.dma_op("sp", gm[:], k.gmasks.rearrange("m p c -> p m c"), writes=[b_c])
        cmask = sb("cmask_sb", [128, 2], F32)
        S.dma_op("sp", cmask[:], k.cmask, writes=[b_c])
        onesf = sb("onesf", [128, 128], F32)
        S.op("pool", lambda: nc.gpsimd.memset(onesf[:], 1.0), writes=[b_c])
        nhalf = sb("nhalfB", [128, 8], F32)
        S.op("pool", lambda: nc.gpsimd.memset(nhalf[:], -0.5), writes=[b_c])
        wg = sb("wg_sb", [128, 8, D], BF16)
        wo = sb("wo_sb", [128, 8, D], BF16)
        S.dma_op("pool", wg[:], k.wg.rearrange("(h p) n -> p h n", p=128), writes=[b_c])
        S.dma_op("pool", wo[:], k.wo.rearrange("(h p) n -> p h n", p=128), writes=[b_c])
        gn = sb("gn", [128, 128], F32)
        S.dma_op("sp", gn[:], k.gdn_norm.partition_broadcast(128), writes=[b_c])
        g2 = sb("g2", [128, D], F32)
        S.dma_op("sp", g2[:], k.norms[1:2, :].partition_broadcast(128), writes=[b_c])
        Sst = sb("Sst", [128, 8, 128], F32); b_S = Buf("Sst")
        Sbf = sb("Sbf", [128, 8, 128], BF16); b_Sbf = Buf("Sbf")
        S.op("pool", lambda: nc.gpsimd.memset(Sst[:], 0.0), writes=[b_S])
        S.op("pool", lambda: nc.gpsimd.memset(Sbf[:], 0.0), writes=[b_Sbf])
        S.barrier()

        def mk(name, shape, dt, n=1):
            ts = [sb("%s%d" % (name, i), shape, dt) for i in range(n)]
            bs = [Buf("%s%d" % (name, i)) for i in range(n)]
            return (ts, bs) if n > 1 else (ts[0], bs[0])
        qg, b_qg = mk("qgB", [128, 8, 128], BF16)
        kg, b_kg = mk("kgB", [128, 8, 128], BF16)
        vg, b_vg = mk("vgB", [128, 8, 128], BF16)
        zs2, b_zs2 = mk("zsB", [128, 8, 128], F32, 2)
        bg, b_bg = mk("bgB", [128, 16], F32)
        sga2, b_sga2 = mk("sgaB", [128, D], F32, 2)
        mdsa2, b_mdsa2 = mk("mdsaB", [128, D], F32, 2)
        hx2, b_hx2 = mk("hxB", [128, D], F32, 2)
        sc, b_sc = mk("scB", [128, 8, 8], F32)
        glb2, b_glb2 = mk("glb", [128, 2, 8], F32, 2)
        Bu, b_Bu = mk("Bu", [128, 8, 128], F32)
        B2, b_B2 = mk("B2", [128, 2, 8], F32)
        kb, b_kb = mk("kbB", [128, 8, 128], BF16)
        qd, b_qd = mk("qdB", [128, 8, 128], BF16)
        kbg, b_kbg = mk("kbgB", [128, 8, 128], BF16)
        ktl2, b_ktl2 = mk("ktlB", [128, 8, 128], BF16, 2)
        vb, b_vb = mk("vbB", [128, 8, 128], BF16)
        kT, b_kT = mk("kTB", [128, 8, 128], BF16)
        kbT, b_kbT = mk("kbTB", [128, 8, 128], BF16)
        qT, b_qT = mk("qTB", [128, 8, 128], BF16)
        qdT2, b_qdT2 = mk("qdTB", [128, 8, 128], BF16, 2)
        D1, b_D1 = mk("D1", [128, 8, 128], F32)
        Em, b_Em = mk("Em", [128, 8, 128], F32)
        ETm, b_ETm = mk("ETm", [128, 8, 128], F32)
        tA, b_tA = mk("tA", [128, 8, 128], F32)
        Bm, b_Bm = mk("Bm", [128, 8, 128], BF16, 2)
        Cm, b_Cm = mk("Cm", [128, 8, 128], BF16, 2)
        Ym, b_Ym = mk("Ym", [128, 8, 128], BF16, 2)
        Yf, b_Yf = mk("Yf", [128, 8, 128], F32, 2)
        aT2, b_aT2 = mk("aT", [128, 8, 128], BF16, 2)
        uS2, b_uS2 = mk("uS", [128, 8, 128], F32, 2)
        wT2, b_wT2 = mk("wT", [128, 8, 128], BF16, 2)
        vn, b_vn = mk("vn", [128, 8, 128], BF16)
        osb, b_osb = mk("osb", [128, 8, 128], F32)
        st8, b_st8 = mk("st8", [128, 24], F32)
        sqt, b_sqt = mk("sqtB", [128, 8, 128], F32)
        yb, b_yb = mk("ybB", [128, 8, 128], BF16)
        yT, b_yT = mk("yTB", [128, 8, 128], BF16)
        mg, b_mg = mk("mgB", [128, D], F32)
        mgb, b_mgb = mk("mgbB", [128, D], BF16)
        mT, b_mT = mk("mTB", [128, 8, 128], BF16)
        mx, b_mx = mk("mxB", [128, D], F32)
        jb, b_jb = mk("jbB", [128, D], BF16)

        def bc_h(ap2):
            return ap2.unsqueeze(2).broadcast_to([128, 8, 128])

        def bc_m(ap2):
            return ap2.unsqueeze(1).broadcast_to([128, 8, 128])

        pool_rr = {"par": 0, "seq": 0}
        cur_pool = ["par"]

        class PT(tuple):
            pass

        def two_banks():
            if cur_pool[0] == "par":
                m = pool_rr["par"] % 2
                pool_rr["par"] += 1
            else:
                m = 2 + pool_rr["seq"] % 2
                pool_rr["seq"] += 1
            pss = PT((k.PS[2 * m], k.PS[2 * m + 1]))
            pss.big = k.PSB[m]
            return pss, (k.PB[2 * m], k.PB[2 * m + 1])

        def nextps():
            pss, bps = two_banks()
            return pss[0], bps[0]

        def pv(ps, hh):
            return ps[hh // 4][:, (hh % 4) * 128:(hh % 4 + 1) * 128]

        def mm8(lhs_fn, rhs_fn, reads):
            pss, bps = two_banks()
            fns = [(lambda hh=hh: nc.tensor.matmul(pv(pss, hh), lhsT=lhs_fn(hh), rhs=rhs_fn(hh),
                                                   start=True, stop=True)) for hh in range(8)]
            S.group("pe", fns, reads=reads, writes=[bps[0], bps[1]])
            return pss, bps

        def ev8(eng, fn, pss, bps, reads, writes):
            S.op(eng, (lambda: fn(pss.big[:].rearrange("p (h t) -> p h t", h=8), slice(0, 8))),
                 reads=[bps[0], bps[1]] + reads, writes=writes)

        def tr8(src, b_src, dst, b_dst):
            ps, bp = nextps()
            psb = ps[:].bitcast(BF16).rearrange("p (k t) -> p k t", k=8)
            fns = [(lambda hh=hh: nc.tensor.transpose(psb[:, hh, :], src[:, hh, :], k.identb[:])) for hh in range(8)]
            S.group("pe", fns, reads=[b_src, k.b_ident], writes=[bp])
            S.op("act", lambda: nc.scalar.copy(out=dst[:], in_=psb), reads=[bp], writes=[b_dst])

        def tile_par(i):
            qdT, b_qdT = qdT2[i % 2], b_qdT2[i % 2]
            aT, b_aT = aT2[i % 2], b_aT2[i % 2]
            uS, b_uS = uS2[i % 2], b_uS2[i % 2]
            wT, b_wT = wT2[i % 2], b_wT2[i % 2]
            ktl, b_ktl = ktl2[i % 2], b_ktl2[i % 2]
            glb, b_glb = glb2[i % 2], b_glb2[i % 2]
            zs, b_zs = zs2[i % 2], b_zs2[i % 2]
            sga, b_sga = sga2[i % 2], b_sga2[i % 2]
            mdsa, b_mdsa = mdsa2[i % 2], b_mdsa2[i % 2]
            hx, b_hx = hx2[i % 2], b_hx2[i % 2]
            r0, r1 = i * 128, (i + 1) * 128
            yield
            S.dma_op("sp", qg[:], k.QG[r0:r1, :].rearrange("p (h d) -> p h d", h=8), writes=[b_qg])
            yield
            S.dma_op("sp", kg[:], k.KG[r0:r1, :].rearrange("p (h d) -> p h d", h=8), writes=[b_kg])
            yield
            S.dma_op("sp", vg[:], k.VG[r0:r1, :].rearrange("p (h d) -> p h d", h=8), writes=[b_vg])
            yield
            S.dma_op("sp", zs[:], k.ZS[r0:r1, :].rearrange("p (h d) -> p h d", h=8), writes=[b_zs])
            yield
            S.dma_op("sp", bg[:], k.BG[r0:r1, :], writes=[b_bg])
            yield
            S.dma_op("sp", sga[:], k.SGA[r0:r1, :], writes=[b_sga])
            yield
            S.dma_op("sp", mdsa[:], k.MDSA[r0:r1, :], writes=[b_mdsa])
            if i == 0:
                yield
                S.op("pool", lambda: nc.gpsimd.memset(hx[:], 0.0), writes=[b_hx])
                yield
                S.dma_op("sp", hx[PAD:128, :], k.meta, writes=[b_hx])
            else:
                yield
                S.dma_op("sp", hx[:], k.x[r0 - 128:r1 - 128, :], writes=[b_hx])
            beta = bg[:, 0:8]
            gg = bg[:, 8:16]
            yield
            ps, bp = nextps()
            yield
            S.op("pe", lambda: nc.tensor.matmul(ps[:, 0:8], lhsT=gm[:, 0, :], rhs=gg, start=True, stop=True),
                 reads=[b_bg, b_c], writes=[bp])
            yield
            S.op("pe", lambda: nc.tensor.matmul(ps[:, 8:16], lhsT=gm[:, 1, :], rhs=gg, start=True, stop=True),
                 reads=[b_bg, b_c], writes=[bp])
            yield
            S.op("dve", lambda: nc.vector.tensor_copy(sc[:, 0:2, :], ps[:, 0:16].rearrange("p (a h) -> p a h", a=2)),
                 reads=[bp], writes=[b_sc])
            yield
            S.op("act", lambda: nc.scalar.activation(out=sc[:, 2, :], in_=sc[:, 0, :], func=AF.Exp),
                 reads=[b_sc], writes=[b_sc])
            yield
            S.op("dve", lambda: nc.vector.tensor_tensor(out=sc[:, 3, :], in0=sc[:, 2, :], in1=beta, op=ALU.mult),
                 reads=[b_sc, b_bg], writes=[b_sc])
            yield
            S.op("dve", lambda: nc.vector.tensor_tensor(out=sc[:, 5, :], in0=sc[:, 1, :], in1=sc[:, 0, :],
                                                        op=ALU.subtract), reads=[b_sc], writes=[b_sc])
            yield
            S.op("act", lambda: nc.scalar.activation(out=sc[:, 4, :], in_=sc[:, 5, :], func=AF.Exp),
                 reads=[b_sc], writes=[b_sc])
            yield
            S.op("dve", lambda: nc.vector.tensor_tensor(out=B2[:], in0=gg.unsqueeze(1).broadcast_to([128, 2, 8]),
                                                        in1=cmask[:].unsqueeze(2).broadcast_to([128, 2, 8]),
                                                        op=ALU.mult), reads=[b_bg, b_c], writes=[b_B2])
            yield
            ps, bp = nextps()
            yield
            S.op("pe", lambda: nc.tensor.matmul(ps[:, 0:16], lhsT=onesf[:], rhs=B2[:].rearrange("p c h -> p (c h)"),
                                                start=True, stop=True), reads=[b_B2, b_c], writes=[bp])
            yield
            S.op("act", lambda: nc.scalar.activation(out=glb[:].rearrange("p c h -> p (c h)"), in_=ps[:, 0:16],
                                                     func=AF.Exp), reads=[bp], writes=[b_glb])
            yield
            S.op("dve", lambda: nc.vector.tensor_tensor(out=Bu[:], in0=bc_m(gm[:, 0, :]), in1=bc_h(gg), op=ALU.mult),
                 reads=[b_bg, b_c], writes=[b_Bu])
            pss, bps = two_banks()
            yield
            for half in range(2):
                yield
                S.op("pe", (lambda half=half: nc.tensor.matmul(
                    pss[half][:], lhsT=onesf[:], rhs=Bu[:, 4 * half:4 * half + 4, :].rearrange("p h t -> p (h t)"),
                    start=True, stop=True)), reads=[b_Bu, b_c], writes=[bps[half]])
            yield
            ev8("dve", lambda psv, sl: nc.vector.tensor_tensor(
                out=D1[:, sl, :], in0=psv,
                in1=sc[:, 0, :].unsqueeze(2).broadcast_to([128, 8, 128]), op=ALU.subtract),
                pss, bps, [b_sc], [b_D1])
            yield
            S.op("dve", lambda: nc.vector.tensor_scalar(out=Em[:], in0=D1[:], scalar1=0.0, scalar2=None, op0=ALU.max),
                 reads=[b_D1], writes=[b_Em])
            yield
            S.op("act", lambda: nc.scalar.activation(out=Em[:], in_=Em[:], func=AF.Exp, scale=-1.0),
                 reads=[b_Em], writes=[b_Em])
            yield
            S.op("dve", lambda: nc.vector.tensor_scalar(out=ETm[:], in0=D1[:], scalar1=0.0, scalar2=None, op0=ALU.min),
                 reads=[b_D1], writes=[b_ETm])
            yield
            S.op("act", lambda: nc.scalar.activation(out=ETm[:], in_=ETm[:], func=AF.Exp),
                 reads=[b_ETm], writes=[b_ETm])
            yield
            S.op("pool", lambda: nc.gpsimd.tensor_tensor(out=kb[:], in0=kg[:], in1=bc_h(beta), op=ALU.mult),
                 reads=[b_kg, b_bg], writes=[b_kb])
            yield
            S.op("pool", lambda: nc.gpsimd.tensor_tensor(out=qd[:], in0=qg[:], in1=bc_h(sc[:, 2, :]), op=ALU.mult),
                 reads=[b_qg, b_sc], writes=[b_qd])
            yield
            S.op("pool", lambda: nc.gpsimd.tensor_tensor(out=kbg[:], in0=kg[:], in1=bc_h(sc[:, 3, :]), op=ALU.mult),
                 reads=[b_kg, b_sc], writes=[b_kbg])
            yield
            S.op("pool", lambda: nc.gpsimd.tensor_tensor(out=ktl[:], in0=kg[:], in1=bc_h(sc[:, 4, :]), op=ALU.mult),
                 reads=[b_kg, b_sc], writes=[b_ktl])
            yield
            S.op("pool", lambda: nc.gpsimd.tensor_tensor(out=vb[:], in0=vg[:], in1=bc_h(beta), op=ALU.mult),
                 reads=[b_vg, b_bg], writes=[b_vb])
            yield
            tr8(kg, b_kg, kT, b_kT)
            yield
            tr8(kb, b_kb, kbT, b_kbT)
            yield
            tr8(qg, b_qg, qT, b_qT)
            yield
            tr8(qd, b_qd, qdT, b_qdT)
            yield
            S.op("pool", lambda: nc.gpsimd.tensor_tensor(out=tA[:], in0=Em[:], in1=bc_m(gm[:, 2, :]), op=ALU.mult),
                 reads=[b_Em, b_c], writes=[b_tA])
            yield
            pss, bps = mm8(lambda hh: kbT[:, hh, :], lambda hh: kT[:, hh, :], [b_kbT, b_kT])
            yield
            ev8("dve", lambda psv, sl: nc.vector.tensor_tensor(out=Bm[0][:, sl, :], in0=psv,
                                                                  in1=tA[:, sl, :], op=ALU.mult),
                pss, bps, [b_tA], [b_Bm[0]])
            yield
            S.op("pool", lambda: nc.gpsimd.tensor_tensor(out=tA[:], in0=ETm[:], in1=bc_m(gm[:, 3, :]), op=ALU.mult),
                 reads=[b_ETm, b_c], writes=[b_tA])
            yield
            pss, bps = mm8(lambda hh: kT[:, hh, :], lambda hh: kbT[:, hh, :], [b_kbT, b_kT])
            yield
            ev8("dve", lambda psv, sl: nc.vector.tensor_tensor(out=Cm[0][:, sl, :], in0=psv,
                                                                  in1=tA[:, sl, :], op=ALU.mult),
                pss, bps, [b_tA], [b_Cm[0]])
            yield
            S.op("pool", lambda: nc.gpsimd.tensor_tensor(out=tA[:], in0=ETm[:], in1=bc_m(gm[:, 0, :]), op=ALU.mult),
                 reads=[b_ETm, b_c], writes=[b_tA])
            yield
            pss, bps = mm8(lambda hh: kT[:, hh, :], lambda hh: qT[:, hh, :], [b_qT, b_kT])
            yield
            ev8("dve", lambda psv, sl: nc.vector.tensor_tensor(out=aT[:, sl, :], in0=psv,
                                                                  in1=tA[:, sl, :], op=ALU.mult),
                pss, bps, [b_tA], [b_aT])
            yield
            S.op("dve", lambda: nc.vector.tensor_tensor(out=Yf[0][:], in0=Cm[0][:], in1=bc_m(k.identf[:]), op=ALU.add),
                 reads=[b_Cm[0], k.b_ident], writes=[b_Yf[0]])
            yield
            S.op("act", lambda: nc.scalar.copy(out=Ym[0][:], in_=Yf[0][:]), reads=[b_Yf[0]], writes=[b_Ym[0]])
            cur = 0
            yield
            for lev in range(1, 6):
                nx = 1 - cur
                yield
                pss, bps = mm8(lambda hh: Cm[cur][:, hh, :], lambda hh: Bm[cur][:, hh, :], [b_Cm[cur], b_Bm[cur]])
                yield
                ev8("act", lambda psv, sl: nc.scalar.copy(out=Bm[nx][:, sl, :], in_=psv),
                    pss, bps, [], [b_Bm[nx]])
                if lev < 5:
                    yield
                    pss, bps = mm8(lambda hh: Bm[cur][:, hh, :], lambda hh: Cm[cur][:, hh, :], [b_Cm[cur], b_Bm[cur]])
                    yield
                    ev8("act", lambda psv, sl: nc.scalar.copy(out=Cm[nx][:, sl, :], in_=psv),
                        pss, bps, [], [b_Cm[nx]])
                yield
                pss, bps = mm8(lambda hh: Bm[nx][:, hh, :], lambda hh: Ym[cur][:, hh, :], [b_Bm[nx], b_Ym[cur]])
                yield
                ev8("dve", lambda psv, sl: nc.vector.tensor_tensor(out=Yf[nx][:, sl, :], in0=psv,
                                                                      in1=Yf[cur][:, sl, :],
                                                                      op=ALU.add),
                    pss, bps, [b_Yf[cur]], [b_Yf[nx]])
                yield
                S.op("act", lambda: nc.scalar.copy(out=Ym[nx][:], in_=Yf[nx][:]), reads=[b_Yf[nx]], writes=[b_Ym[nx]])
                cur = nx
            Y = Ym[cur]
            bY = b_Ym[cur]
            yield
            pss, bps = mm8(lambda hh: Y[:, hh, :], lambda hh: vb[:, hh, :], [bY, b_vb])
            yield
            ev8("act", lambda psv, sl: nc.scalar.copy(out=uS[:, sl, :], in_=psv),
                pss, bps, [], [b_uS])
            yield
            pss, bps = mm8(lambda hh: kbg[:, hh, :], lambda hh: Y[:, hh, :], [bY, b_kbg])
            yield
            ev8("act", lambda psv, sl: nc.scalar.copy(out=wT[:, sl, :], in_=psv),
                pss, bps, [], [b_wT])

        def tile_seq(i):
            r0, r1 = i * 128, (i + 1) * 128
            qdT, b_qdT = qdT2[i % 2], b_qdT2[i % 2]
            aT, b_aT = aT2[i % 2], b_aT2[i % 2]
            uS, b_uS = uS2[i % 2], b_uS2[i % 2]
            wT, b_wT = wT2[i % 2], b_wT2[i % 2]
            ktl, b_ktl = ktl2[i % 2], b_ktl2[i % 2]
            glb, b_glb = glb2[i % 2], b_glb2[i % 2]
            zs, b_zs = zs2[i % 2], b_zs2[i % 2]
            sga, b_sga = sga2[i % 2], b_sga2[i % 2]
            mdsa, b_mdsa = mdsa2[i % 2], b_mdsa2[i % 2]
            hx, b_hx = hx2[i % 2], b_hx2[i % 2]
            yield
            for c in range(2):
                c0, c1 = 64 * c, 64 * c + 64
                pss, bps = two_banks()
                fns = [(lambda hh=hh: nc.tensor.matmul(pv(pss, hh)[c0:c1, :], lhsT=wT[:, hh, c0:c1],
                                                       rhs=Sbf[:, hh, :], start=True, stop=True))
                       for hh in range(8)]
                yield
                S.group("pe", fns, reads=[b_wT, b_Sbf], writes=[bps[0], bps[1]])
                yield
                S.op("dve", lambda: nc.vector.tensor_tensor(
                    out=vn[c0:c1, :, :], in0=uS[c0:c1, :, :],
                    in1=pss.big[c0:c1, :].rearrange("p (h t) -> p h t", h=8), op=ALU.subtract),
                    reads=[bps[0], bps[1], b_uS], writes=[b_vn])
                pso, bpo = two_banks()
                fns = []
                for hh in range(8):
                    fns.append(lambda hh=hh: nc.tensor.matmul(pv(pso, hh)[c0:c1, :], lhsT=qdT[:, hh, c0:c1],
                                                              rhs=Sbf[:, hh, :], start=True, stop=False))
                    fns.append(lambda hh=hh: nc.tensor.matmul(pv(pso, hh)[c0:c1, :], lhsT=aT[c0:c1, hh, c0:c1],
                                                              rhs=vn[c0:c1, hh, :], start=False, stop=True))
                yield
                S.group("pe", fns, reads=[b_qdT, b_Sbf, b_aT, b_vn], writes=[bpo[0], bpo[1]])
                yield
                S.op("act", lambda: nc.scalar.copy(
                    out=osb[c0:c1, :, :], in_=pso.big[c0:c1, :].rearrange("p (h t) -> p h t", h=8)),
                    reads=[bpo[0], bpo[1]], writes=[b_osb])
                pss2, bps2 = two_banks()
                fns = [(lambda hh=hh: nc.tensor.matmul(pv(pss2, hh), lhsT=ktl[c0:c1, hh, :], rhs=vn[c0:c1, hh, :],
                                                       start=True, stop=True)) for hh in range(8)]
                yield
                S.group("pe", fns, reads=[b_ktl, b_vn], writes=[bps2[0], bps2[1]])
                yield
                S.op("pool", lambda: nc.gpsimd.tensor_tensor(out=Sst[:], in0=Sst[:], in1=bc_h(glb[:, c, :]),
                                                             op=ALU.mult), reads=[b_S, b_glb], writes=[b_S])
                yield
                S.op("dve", lambda: nc.vector.tensor_tensor(
                    out=Sst[:], in0=Sst[:], in1=pss2.big[:].rearrange("p (h t) -> p h t", h=8), op=ALU.add),
                    reads=[bps2[0], bps2[1], b_S], writes=[b_S])
                yield
                S.op("act", lambda: nc.scalar.copy(out=Sbf[:], in_=Sst[:]), reads=[b_S], writes=[b_Sbf])
            yield
            S.op("dve", lambda: nc.vector.tensor_tensor(out=sqt[:], in0=osb[:], in1=osb[:], op=ALU.mult),
                 reads=[b_osb], writes=[b_sqt])
            yield
            S.op("dve", lambda: nc.vector.tensor_reduce(out=st8[:, 0:8], in_=sqt[:], axis=AX.X, op=ALU.add),
                 reads=[b_sqt], writes=[b_st8])
            yield
            S.op("pool", lambda: nc.gpsimd.tensor_scalar(out=st8[:, 8:16], in0=st8[:, 0:8], scalar1=1.0 / 128,
                                                         scalar2=EPS, op0=ALU.mult, op1=ALU.add),
                 reads=[b_st8], writes=[b_st8])
            yield
            S.op("pool", lambda: nc.gpsimd.tensor_tensor(out=st8[:, 16:24], in0=st8[:, 8:16], in1=nhalf[:], op=ALU.pow),
                 reads=[b_st8, b_c], writes=[b_st8])
            yield
            S.op("pool", lambda: nc.gpsimd.tensor_tensor(out=zs[:], in0=zs[:], in1=bc_m(gn[:]), op=ALU.mult),
                 reads=[b_zs, b_c], writes=[b_zs])
            yield
            S.op("dve", lambda: nc.vector.tensor_tensor(out=osb[:], in0=osb[:], in1=bc_h(st8[:, 16:24]), op=ALU.mult),
                 reads=[b_osb, b_st8], writes=[b_osb])
            yield
            S.op("dve", lambda: nc.vector.tensor_tensor(out=yb[:], in0=osb[:], in1=zs[:], op=ALU.mult),
                 reads=[b_osb, b_zs], writes=[b_yb])
            if k.debug:
                yield
                S.dma_op("sp", k.YGDN[r0:r1, :].rearrange("p (h d) -> p h d", h=8), yb[:], reads=[b_yb])
            yield
            tr8(yb, b_yb, yT, b_yT)
            yield
            for cb in range(2):
                ps, bp = nextps()
                fns = [(lambda h=h: nc.tensor.matmul(ps[:], lhsT=yT[:, h, :], rhs=wg[:, h, cb * 512:(cb + 1) * 512],
                                                     start=(h == 0), stop=(h == 7))) for h in range(8)]
                yield
                S.group("pe", fns, reads=[b_yT, b_c], writes=[bp])
                yield
                S.op("dve", lambda: nc.vector.tensor_tensor(out=mg[:, cb * 512:(cb + 1) * 512], in0=ps[:],
                                                            in1=sga[:, cb * 512:(cb + 1) * 512], op=ALU.mult),
                     reads=[bp, b_sga], writes=[b_mg])
            yield
            S.op("pool", lambda: nc.gpsimd.tensor_tensor(out=mgb[:], in0=mg[:], in1=mdsa[:], op=ALU.add),
                 reads=[b_mg, b_mdsa], writes=[b_mgb])
            yield
            tr8(mgb[:].rearrange("p (h d) -> p h d", h=8), b_mgb, mT, b_mT)
            yield
            for cb in range(2):
                ps, bp = nextps()
                fns = [(lambda h=h: nc.tensor.matmul(ps[:], lhsT=mT[:, h, :], rhs=wo[:, h, cb * 512:(cb + 1) * 512],
                                                     start=(h == 0), stop=(h == 7))) for h in range(8)]
                yield
                S.group("pe", fns, reads=[b_mT, b_c], writes=[bp])
                yield
                S.op("act", lambda: nc.scalar.copy(out=mx[:, cb * 512:(cb + 1) * 512], in_=ps[:]),
                     reads=[bp], writes=[b_mx])
            yield
            S.op("act", lambda: nc.scalar.activation(out=jb[:], in_=mx[:], func=AF.Square, accum_out=st8[:, 0:1]),
                 reads=[b_mx], writes=[b_jb, b_st8])
            yield
            S.op("pool", lambda: nc.gpsimd.tensor_scalar(out=st8[:, 8:9], in0=st8[:, 0:1], scalar1=1.0 / D,
                                                         scalar2=EPS, op0=ALU.mult, op1=ALU.add),
                 reads=[b_st8], writes=[b_st8])
            yield
            S.op("pool", lambda: nc.gpsimd.tensor_tensor(out=st8[:, 16:17], in0=st8[:, 8:9], in1=nhalf[:, 0:1],
                                                         op=ALU.pow), reads=[b_st8, b_c], writes=[b_st8])
            yield
            S.op("dve", lambda: nc.vector.scalar_tensor_tensor(out=mx[:], in0=mx[:], scalar=st8[:, 16:17], in1=g2[:],
                                                               op0=ALU.mult, op1=ALU.mult),
                 reads=[b_mx, b_st8, b_c], writes=[b_mx])
            yield
            S.op("pool", lambda: nc.gpsimd.tensor_tensor(out=mx[:], in0=mx[:], in1=hx[:], op=ALU.add),
                 reads=[b_mx, b_hx], writes=[b_mx])
            yield
            S.dma_op("sp", k.H1[r0:r1, :], mx[:], reads=[b_mx])

        def drain(g, pool):
            cur_pool[0] = pool
            n = 0
            for _ in g:
                n += 1
            return n

        def lockstep(gS, nS, gP, nP):
            aS = aP = True
            pS = pP = 0.0
            cS = cP = 0
            while aS or aP:
                if aS and (not aP or pS <= pP):
                    cur_pool[0] = "seq"
                    try:
                        next(gS)
                        cS += 1
                        pS += 1.0 / nS
                    except StopIteration:
                        aS = False
                else:
                    cur_pool[0] = "par"
                    try:
                        next(gP)
                        cP += 1
                        pP += 1.0 / nP
                    except StopIteration:
                        aP = False
            return cS, cP

        nP = drain(tile_par(0), "par")
        nS = nP
        for i in range(NT):
            if i + 1 < NT:
                cS, cP = lockstep(tile_seq(i), nS, tile_par(i + 1), nP)
                nS, nP = max(cS, 1), max(cP, 1)
            else:
                drain(tile_seq(i), "seq")
        S.barrier()


def phase_E(k):
    nc, S = k.nc, k.S
    with ExitStack() as es:
        def sb(name, shape, dt):
            return es.enter_context(nc.sbuf_tensor(name, list(shape), dt))
        b_c = Buf("constE")
        wu = sb("wu_sb", [128, 8, 4096], BF16)
        wdn = sb("wdn_sb", [128, 32, D], BF16)
        for kk in range(8):
            S.dma_op("pool", wu[:, kk, :], k.w_up[kk * 128:(kk + 1) * 128, :], writes=[b_c])
        for kk in range(0, 32, 4):
            S.dma_op("pool", wdn[:, kk:kk + 4, :],
                     k.w_down[kk * 128:(kk + 4) * 128, :].rearrange("(f p) n -> p f n", p=128), writes=[b_c])
        g3 = sb("g3", [128, D], F32)
        g4 = sb("g4", [128, D], F32)
        S.dma_op("sp", g3[:], k.norms[2:3, :].partition_broadcast(128), writes=[b_c])
        S.dma_op("sp", g4[:], k.norms[3:4, :].partition_broadcast(128), writes=[b_c])
        nhalf = sb("nhalfE", [128, 1], F32)
        S.op("pool", lambda: nc.gpsimd.memset(nhalf[:], -0.5), writes=[b_c])
        S.barrier()
        h1 = [sb("h1_%d" % i, [128, 2, D], F32) for i in range(2)]; b_h1 = [Buf("h1_%d" % i) for i in range(2)]
        jb = sb("jbE", [128, D], BF16); b_jb = Buf("jbE")
        st = [sb("stE%d" % i, [128, 2, 8], F32) for i in range(2)]; b_st = [Buf("stE%d" % i) for i in range(2)]
        n2 = [sb("n2_%d" % i, [128, D], BF16) for i in range(2)]; b_n2 = [Buf("n2_%d" % i) for i in range(2)]
        n2T = [sb("n2T%d" % i, [128, 8, 256], BF16) for i in range(2)]; b_n2T = [Buf("n2T%d" % i) for i in range(2)]
        uT1 = sb("uT0", [128, 32, 256], BF16); b_uT1 = Buf("uT0")
        uT = [uT1, uT1]; b_uT = [b_uT1, b_uT1]
        rl = [sb("rl%d" % i, [128, 512], F32) for i in range(2)]; b_rl = [Buf("rl%d" % i) for i in range(2)]
        mo = [sb("mo%d" % i, [128, D], F32) for i in range(2)]; b_mo = [Buf("mo%d" % i) for i in range(2)]
        cnt = {"r": 0, "n": 0, "m": 0}
        NG = (NT - 1) // 2

        def head(gi):
            p = gi % 2
            for t in range(2):
                i = 1 + 2 * gi + t
                r0, r1 = i * 128, (i + 1) * 128
                S.dma_op("sp", h1[p][:, t, :], k.H1[r0:r1, :], writes=[b_h1[p]])
            for t in range(2):
                S.op("act", lambda: nc.scalar.activation(out=jb[:], in_=h1[p][:, t, :], func=AF.Square,
                                                         accum_out=st[p][:, t, 0:1]),
                     reads=[b_h1[p]], writes=[b_jb, b_st[p]])
                S.op("pool", lambda: nc.gpsimd.tensor_scalar(out=st[p][:, t, 1:2], in0=st[p][:, t, 0:1],
                                                             scalar1=1.0 / D, scalar2=EPS, op0=ALU.mult, op1=ALU.add),
                     reads=[b_st[p]], writes=[b_st[p]])
                S.op("pool", lambda: nc.gpsimd.tensor_tensor(out=st[p][:, t, 2:3], in0=st[p][:, t, 1:2], in1=nhalf[:],
                                                             op=ALU.pow), reads=[b_st[p], b_c], writes=[b_st[p]])
                np_ = cnt["n"] % 2
                cnt["n"] += 1
                S.op("dve", lambda: nc.vector.scalar_tensor_tensor(out=n2[np_][:], in0=h1[p][:, t, :],
                                                                   scalar=st[p][:, t, 2:3], in1=g3[:],
                                                                   op0=ALU.mult, op1=ALU.mult),
                     reads=[b_h1[p], b_st[p], b_c], writes=[b_n2[np_]])
                ps, bp = k.nextps()
                psb = ps[:].bitcast(BF16).rearrange("p (k t) -> p k t", k=8)
                fns = [(lambda kk=kk: nc.tensor.transpose(psb[:, kk, :], n2[np_][:, kk * 128:(kk + 1) * 128],
                                                          k.identb[:])) for kk in range(8)]
                S.group("pe", fns, reads=[b_n2[np_], k.b_ident], writes=[bp])
                S.op("act", lambda: nc.scalar.copy(out=n2T[p][:, :, t * 128:(t + 1) * 128], in_=psb),
                     reads=[bp], writes=[b_n2T[p]])

        def up(gi):
            p = gi % 2
            for fb in range(16):
                ps, bp = k.nextps()
                fns = []
                for f2 in range(2):
                    f = fb * 2 + f2
                    for kk in range(8):
                        fns.append(lambda f=f, f2=f2, kk=kk: nc.tensor.matmul(
                            ps[:, f2 * 256:(f2 + 1) * 256], lhsT=wu[:, kk, f * 128:(f + 1) * 128], rhs=n2T[p][:, kk, :],
                            start=(kk == 0), stop=(kk == 7)))
                S.group("pe", fns, reads=[b_n2T[p], b_c], writes=[bp])
                rp = cnt["r"] % 2
                cnt["r"] += 1
                S.op("act", lambda: nc.scalar.activation(out=rl[rp][:], in_=ps[:], func=AF.Relu),
                     reads=[bp], writes=[b_rl[rp]])
                S.op("dve", lambda: nc.vector.tensor_tensor(
                    out=uT[p][:, fb * 2:fb * 2 + 2, :].rearrange("p f t -> p (f t)"), in0=rl[rp][:], in1=rl[rp][:],
                    op=ALU.mult), reads=[b_rl[rp]], writes=[b_uT[p]])

        def down(gi):
            p = gi % 2
            for t in range(2):
                i = 1 + 2 * gi + t
                r0, r1 = i * 128, (i + 1) * 128
                mp = cnt["m"] % 2
                cnt["m"] += 1
                for cb in range(2):
                    ps, bp = k.nextps()
                    fns = [(lambda f=f: nc.tensor.matmul(ps[:], lhsT=uT[p][:, f, t * 128:(t + 1) * 128],
                                                         rhs=wdn[:, f, cb * 512:(cb + 1) * 512],
                                                         start=(f == 0), stop=(f == 31))) for f in range(32)]
                    S.group("pe", fns, reads=[b_uT[p], b_c], writes=[bp])
                    S.op("act", lambda: nc.scalar.copy(out=mo[mp][:, cb * 512:(cb + 1) * 512], in_=ps[:]),
                         reads=[bp], writes=[b_mo[mp]])
                S.op("act", lambda: nc.scalar.activation(out=jb[:], in_=mo[mp][:], func=AF.Square,
                                                         accum_out=st[p][:, t, 4:5]),
                     reads=[b_mo[mp]], writes=[b_jb, b_st[p]])
                S.op("pool", lambda: nc.gpsimd.tensor_scalar(out=st[p][:, t, 5:6], in0=st[p][:, t, 4:5],
                                                             scalar1=1.0 / D, scalar2=EPS, op0=ALU.mult, op1=ALU.add),
                     reads=[b_st[p]], writes=[b_st[p]])
                S.op("pool", lambda: nc.gpsimd.tensor_tensor(out=st[p][:, t, 6:7], in0=st[p][:, t, 5:6], in1=nhalf[:],
                                                             op=ALU.pow), reads=[b_st[p], b_c], writes=[b_st[p]])
                S.op("dve", lambda: nc.vector.scalar_tensor_tensor(out=mo[mp][:], in0=mo[mp][:],
                                                                   scalar=st[p][:, t, 6:7], in1=g4[:],
                                                                   op0=ALU.mult, op1=ALU.mult),
                     reads=[b_mo[mp], b_st[p], b_c], writes=[b_mo[mp]])
                S.op("pool", lambda: nc.gpsimd.tensor_tensor(out=mo[mp][:], in0=mo[mp][:], in1=h1[p][:, t, :],
                                                             op=ALU.add),
                     reads=[b_mo[mp], b_h1[p]], writes=[b_mo[mp]])
                S.dma_op("sp", k.out[r0 - 128:r1 - 128, :], mo[mp][:], reads=[b_mo[mp]])

        head(0)
        for gi in range(NG):
            up(gi)
            if gi + 1 < NG:
                head(gi + 1)
            down(gi)
        S.barrier()

def host_consts():
    pos = np.concatenate([np.zeros(PAD, np.float32), np.arange(T - PAD, dtype=np.float32)])

    def tabs(dim, reps):
        inv = (10000.0 ** (-np.arange(0, dim, 2, dtype=np.float32) / dim)).astype(np.float32)
        ang = pos[:, None] * inv[None, :]
        c = np.cos(ang).astype(np.float32)
        s = np.sin(ang).astype(np.float32)
        cT = np.concatenate([c, c], 1).T
        sT = np.concatenate([-s, s], 1).T
        return (np.ascontiguousarray(np.tile(cT, (reps, 1))), np.ascontiguousarray(np.tile(sT, (reps, 1))))
    cosA, sinA = tabs(128, 1)
    cosI, sinI = tabs(64, 2)
    r = np.arange(128)
    NEG = np.float32(-1e30)
    mdiag = np.where(r[None, :] <= r[:, None], 0.0, NEG).astype(np.float32)
    m0 = np.where((r[None, :] <= r[:, None]) & (r[None, :] >= PAD), 0.0, NEG).astype(np.float32)
    mpad = np.where(r[None, :] >= PAD, 0.0, NEG).astype(np.float32) * np.ones((128, 1), np.float32)
    masks = np.ascontiguousarray(np.stack([mdiag, m0, mpad], 0))
    pow2 = (2.0 ** -(np.arange(15, dtype=np.float32) + 1))[None, :].astype(np.float32)
    same = (r[:, None] // 64 == r[None, :] // 64)
    UT = (same & (r[:, None] <= r[None, :])).astype(np.float32)
    SAME = same.astype(np.float32)
    nSL = -(same & (r[:, None] > r[None, :])).astype(np.float32)
    nSU = -(same & (r[:, None] < r[None, :])).astype(np.float32)
    gmasks = np.ascontiguousarray(np.stack([UT, SAME, nSL, nSU], 0))
    cmask = np.stack([(r < 64), (r >= 64)], 1).astype(np.float32)
    return dict(cosA=cosA, sinA=sinA, cosI=cosI, sinI=sinI, ident=np.eye(128, dtype=np.float32),
                masks=masks, pow2=pow2, gmasks=gmasks, cmask=np.ascontiguousarray(cmask))


def swap_halves(w, hd):
    d, n = w.shape
    w = w.reshape(d, n // hd, 2, hd // 2)
    return np.ascontiguousarray(w[:, :, ::-1, :]).reshape(d, n)


def host_inputs(inputs):
    w_in = np.ascontiguousarray(inputs["w_in"][0])
    ik = w_in[:, C_IK:C_IK + 64]
    ikd = np.concatenate([ik, ik], 1)
    groups = []
    aq = w_in[:, C_AQ:C_AQ + 1024]
    aqs = swap_halves(aq, 128)
    for h in range(8):
        groups += [aq[:, h * 128:(h + 1) * 128], aqs[:, h * 128:(h + 1) * 128]]
    ak = w_in[:, C_AK:C_AK + 256]
    aks = swap_halves(ak, 128)
    for h in range(2):
        groups += [ak[:, h * 128:(h + 1) * 128], aks[:, h * 128:(h + 1) * 128]]
    iq = w_in[:, C_IQ:C_IQ + 512]
    iqs = swap_halves(iq, 64)
    for h in range(4):
        groups += [iq[:, h * 128:(h + 1) * 128], iqs[:, h * 128:(h + 1) * 128]]
    groups += [ikd, swap_halves(ikd, 64)]
    w_sw = np.concatenate(groups, 1)
    common = dict(
        meta=np.ascontiguousarray(inputs["meta_tokens"]),
        w_in=w_in, w_sw=np.ascontiguousarray(w_sw),
        conv_w=np.ascontiguousarray(inputs["conv_w"][0]),
        conv_wT=np.ascontiguousarray(inputs["conv_w"][0].T),
        a_log=np.ascontiguousarray(inputs["a_log"]), dt_bias=np.ascontiguousarray(inputs["dt_bias"]),
        gdn_norm=np.ascontiguousarray(inputs["gdn_norm"]),
        wg=np.ascontiguousarray(inputs["w_branch_gdn"][0]), wd=np.ascontiguousarray(inputs["w_branch_dsa"][0]),
        wo=np.ascontiguousarray(inputs["w_out"][0]),
        w_up=np.ascontiguousarray(inputs["w_up"][0]), w_down=np.ascontiguousarray(inputs["w_down"][0]),
        norms=np.ascontiguousarray(np.concatenate([inputs["pre_mix_norm"], inputs["post_mix_norm"],
                                                   inputs["pre_mlp_norm"], inputs["post_mlp_norm"]], 0)),
    )
    common.update(host_consts())
    return common


def kernel(**inputs):
    inputs = {k_: np.asarray(v) for k_, v in inputs.items()}
    nc = build()
    common = host_inputs(inputs)
    in_maps = []
    for b in range(8):
        m = dict(common)
        m["x"] = np.ascontiguousarray(inputs["x"][b])
        in_maps.append(m)
    res = run_bass_kernel_spmd(nc, in_maps, core_ids=list(range(8)))
    return np.stack([r["out"] for r in res.results], 0).astype(np.float32)
```

```python
import numpy as np
import concourse.bass as bass
import concourse.mybir as mybir
from concourse.bass_utils import run_bass_kernel_spmd
from contextlib import ExitStack

F32 = mybir.dt.float32
BF16 = mybir.dt.bfloat16
ALU = mybir.AluOpType
AF = mybir.ActivationFunctionType
AX = mybir.AxisListType

T = 4224
NT = 33
PAD = 112
D = 1024
EPS = 1e-6
C_GQ, C_GK, C_GV, C_GZ, C_GB, C_GA = 0, 1024, 2048, 3072, 4096, 4104
C_AQ, C_AK, C_AV, C_IQ, C_IK, C_IW, C_GTA, C_GTB = 4112, 5136, 5392, 5648, 6160, 6224, 6232, 7256
N_IN = 8280


class Buf:
    __slots__ = ("name", "w", "r")

    def __init__(self, name):
        self.name = name
        self.w = None
        self.r = {}


class Tok:
    __slots__ = ("key", "val", "hist")

    def __init__(self, key, val, hist):
        self.key = key
        self.val = val
        self.hist = hist


class Eng:
    def __init__(self, name, handle, sem, key):
        self.name = name
        self.h = handle
        self.sem = sem
        self.key = key
        self.count = 0
        self.seen = {}
        self.snap = {}
        self.nwaits = 0
        self.ninstr = 0


class Sched:
    NDMA = 24

    def __init__(self, nc, es):
        self.nc = nc
        self.es = es
        self.sems = []
        self.E = {}
        for name, h in [("pe", nc.tensor), ("dve", nc.vector), ("act", nc.scalar),
                        ("pool", nc.gpsimd), ("sp", nc.sync)]:
            sem = es.enter_context(nc.semaphore("s_" + name))
            e = Eng(name, h, sem, len(self.sems))
            self.sems.append(sem)
            self.E[name] = e
        self.dma = []
        for i in range(self.NDMA):
            sem = es.enter_context(nc.semaphore("d%d" % i))
            self.dma.append([len(self.sems), 0])
            self.sems.append(sem)
        self.dma_rr = 0

    def _wait(self, e, tok):
        if tok is None:
            return
        if e.seen.get(tok.key, 0) >= tok.val:
            return
        if tok.key == e.key and e.name == "pe":
            return
        e.h.wait_ge(self.sems[tok.key], tok.val)
        e.nwaits += 1
        e.seen[tok.key] = tok.val
        if tok.hist:
            for k, v in tok.hist.items():
                if e.seen.get(k, 0) < v:
                    e.seen[k] = v
        e.snap = None

    def _deps(self, e, reads, writes):
        for b in reads:
            self._wait(e, b.w)
        for b in writes:
            self._wait(e, b.w)
            for t in b.r.values():
                self._wait(e, t)

    def _commit(self, tok, reads, writes):
        for b in reads:
            o = b.r.get(tok.key)
            if o is None or o.val < tok.val:
                b.r[tok.key] = tok
        for b in writes:
            b.w = tok
            b.r = {}

    def op(self, eng, fn, reads=(), writes=()):
        e = self.E[eng]
        self._deps(e, reads, writes)
        ins = fn()
        e.count += 1
        e.ninstr += 1
        ins.then_inc(e.sem, 1)
        if e.snap is None:
            e.snap = dict(e.seen)
        tok = Tok(e.key, e.count, e.snap)
        self._commit(tok, reads, writes)
        return tok

    def group(self, eng, fns, reads=(), writes=()):
        e = self.E[eng]
        self._deps(e, reads, writes)
        ins = None
        for fn in fns:
            ins = fn()
            e.ninstr += 1
        e.count += 1
        ins.then_inc(e.sem, 1)
        if e.snap is None:
            e.snap = dict(e.seen)
        tok = Tok(e.key, e.count, e.snap)
        self._commit(tok, reads, writes)
        return tok

    def dma_op(self, eng, out, in_, reads=(), writes=(), **kw):
        e = self.E[eng]
        if eng == "pool":
            sem = self.es.enter_context(self.nc.semaphore("q%d" % len(self.sems)))
            slot = [len(self.sems), 0]
            self.sems.append(sem)
            self.dma.append(slot)
        else:
            slot = self.dma[self.dma_rr]
            self.dma_rr = (self.dma_rr + 1) % self.NDMA
        key = slot[0]
        if slot[1] > 0:
            self._wait(e, Tok(key, slot[1], None))
        self._deps(e, reads, writes)
        slot[1] += 16
        ins = e.h.dma_start(out=out, in_=in_, **kw)
        ins.then_inc(self.sems[key], 16)
        e.ninstr += 1
        tok = Tok(key, slot[1], None)
        self._commit(tok, reads, writes)
        return tok

    def barrier(self):
        for e in self.E.values():
            for o in self.E.values():
                if o is not e and o.count > 0:
                    self._wait(e, Tok(o.key, o.count, None))
            for key, val in self.dma:
                if val > 0:
                    self._wait(e, Tok(key, val, None))

    def finish(self):
        self.barrier()
        for e in self.E.values():
            if e.count > 0 and e.name != "pe":
                e.h.wait_ge(e.sem, e.count)

    def stats(self):
        return {n: (e.ninstr, e.nwaits) for n, e in self.E.items()}


class K:
    pass


def build(debug=False, phases="ABCDE"):
    nc = bass.Bass("TRN2", target_bir_lowering=False)
    k = K()
    k.nc = nc
    k.debug = debug

    def din(name, shape, dt=F32):
        return nc.dram_tensor(name, list(shape), dt, kind="ExternalInput").ap()

    def dscr(name, shape, dt):
        kind = "ExternalOutput" if debug else "Internal"
        return nc.dram_tensor(name, list(shape), dt, kind=kind).ap()

    k.x = din("x", [4096, D])
    k.meta = din("meta", [16, D])
    k.w_in = din("w_in", [D, N_IN])
    k.w_sw = din("w_sw", [D, 15 * 256])
    k.conv_w = din("conv_w", [4, 3072])
    k.conv_wT = din("conv_wT", [3072, 4])
    k.a_log = din("a_log", [1, 8])
    k.dt_bias = din("dt_bias", [1, 8])
    k.gdn_norm = din("gdn_norm", [1, 128])
    k.wg = din("wg", [D, D])
    k.wd = din("wd", [D, D])
    k.wo = din("wo", [D, D])
    k.w_up = din("w_up", [D, 4096])
    k.w_down = din("w_down", [4096, D])
    k.norms = din("norms", [4, D])
    k.ident_d = din("ident", [128, 128])
    k.cosA = din("cosA", [128, T])
    k.sinA = din("sinA", [128, T])
    k.cosI = din("cosI", [128, T])
    k.sinI = din("sinI", [128, T])
    k.out = nc.dram_tensor("out", [4096, D], F32, kind="ExternalOutput").ap()

    k.QG = dscr("QG", [T, 1024], BF16)
    k.KG = dscr("KG", [T, 1024], BF16)
    k.VG = dscr("VG", [T, 1024], BF16)
    k.ZS = dscr("ZS", [T, 1024], F32)
    k.BG = dscr("BG", [T, 16], F32)
    k.QT = dscr("QT", [8, 128, T], BF16)
    k.KT = dscr("KT", [2, 128, T], BF16)
    k.Vd = dscr("V", [T, 256], BF16)
    k.QIT = dscr("QIT", [4, 128, T], BF16)
    k.KIT = dscr("KIT", [128, T], BF16)
    k.IW = dscr("IW", [T, 8], F32)
    k.SGA = dscr("SGA", [T, 1024], F32)
    k.SGB = dscr("SGB", [T, 1024], F32)
    k.MDSA = dscr("MDSA", [T, 1024], F32)
    k.H1 = dscr("H1", [T, 1024], F32)
    if debug:
        k.YGDN = dscr("YGDN", [T, 1024], BF16)
    k.gmasks = din("gmasks", [4, 128, 128])
    k.cmask = din("cmask", [128, 2])
    k.masks = din("masks", [3, 128, 128])
    k.pow2 = din("pow2", [1, 15])

    with ExitStack() as es:
        S = Sched(nc, es)
        k.S = S
        k.es = es

        def sb(name, shape, dt, stack=es):
            return stack.enter_context(nc.sbuf_tensor(name, list(shape), dt))
        k.sb = sb

        k.PSB = [es.enter_context(nc.psum_tensor("psb%d" % i, [128, 1024], F32)) for i in range(4)]
        k.PS = [k.PSB[i // 2][:, (i % 2) * 512:(i % 2 + 1) * 512] for i in range(8)]
        k.PB = [Buf("ps%d" % i) for i in range(8)]
        k.ps_rr = 0
        k.ps_n = 8

        k.ps_base = 0

        k.cur_stream = None
        k.ps_pools = {}
        k.ps_rrs = {}

        def nextps():
            if k.cur_stream is not None:
                base, n = k.ps_pools[k.cur_stream]
                i = k.ps_rrs.get(k.cur_stream, 0) % n
                k.ps_rrs[k.cur_stream] = i + 1
                return k.PS[base + i], k.PB[base + i]
            i = k.ps_rr % k.ps_n
            k.ps_rr = (i + 1) % k.ps_n
            return k.PS[k.ps_base + i], k.PB[k.ps_base + i]
        k.nextps = nextps

        k.identf = sb("identf", [128, 128], F32)
        k.identb = sb("identb", [128, 128], BF16)
        k.b_ident = Buf("ident")
        S.dma_op("sp", k.identf[:], k.ident_d, writes=[k.b_ident])
        S.op("dve", lambda: nc.vector.tensor_copy(k.identb[:], k.identf[:]), reads=[k.b_ident], writes=[k.b_ident])

        if "A" in phases:
            phase_A(k)
            S.barrier()
        if "C" in phases:
            phase_C(k)
        if "B" in phases:
            phase_B(k)
        if "E" in phases:
            phase_E(k)
        S.finish()
        print("instr stats", S.stats())
    return nc


def phase_A(k):
    nc, S = k.nc, k.S
    with ExitStack() as es:
        cur = [es]

        def sb(name, shape, dt):
            return cur[0].enter_context(nc.sbuf_tensor(name, list(shape), dt))

        def substack():
            st = ExitStack()
            cur[0] = st
            return st

        def endsub(st):
            S.barrier()
            st.close()
            cur[0] = es

        nT = sb("nT", [128, 8, 3 + T], BF16)
        b_nT = [Buf("nT%d" % i) for i in range(NT)]
        b_nTpad = Buf("nTpad")
        S.op("pool", lambda: nc.gpsimd.memset(nT[:, :, 0:3], 0.0), writes=[b_nTpad])

        ysb = [sb("ysb%d" % i, [128, 512], F32) for i in range(2)]
        b_ysb = [Buf("ysb%d" % i) for i in range(2)]
        ob = [sb("ob%d" % i, [128, 512], BF16) for i in range(3)]
        b_ob = [Buf("ob%d" % i) for i in range(3)]
        of = [sb("of%d" % i, [128, 512], F32) for i in range(3)]
        b_of = [Buf("of%d" % i) for i in range(3)]
        sq = [sb("sq%d" % i, [128, 8], F32) for i in range(2)]
        b_sq = [Buf("sq%d" % i) for i in range(2)]
        nhalf = sb("nhalf", [128, 4], F32)
        b_nhalf = Buf("nhalf")
        S.op("pool", lambda: nc.gpsimd.memset(nhalf[:], -0.5), writes=[b_nhalf])
        junkf = sb("junkf", [128, 128], F32)
        b_junkf = Buf("junkf")

        st = substack()
        g1 = sb("g1", [128, D], F32)
        b_g1 = Buf("g1")
        S.dma_op("sp", g1[:], k.norms[0:1, :].partition_broadcast(128), writes=[b_g1])

        ht = [sb("ht%d" % i, [128, D], F32) for i in range(2)]
        b_ht = [Buf("ht%d" % i) for i in range(2)]
        junk = sb("junkA", [128, D], BF16)
        b_junk = Buf("junkA")
        nb = [sb("nb%d" % i, [128, D], BF16) for i in range(2)]
        b_nb = [Buf("nb%d" % i) for i in range(2)]
        ss = [sb("ssA%d" % i, [128, 4], F32) for i in range(2)]
        b_ss = [Buf("ssA%d" % i) for i in range(2)]

        for i in range(NT):
            p = i % 2
            h_, bh = ht[p], b_ht[p]
            if i == 0:
                S.op("pool", lambda: nc.gpsimd.memset(h_[:], 0.0), writes=[bh])
                S.dma_op("sp", h_[PAD:128, :], k.meta, writes=[bh])
            else:
                S.dma_op("sp", h_[:], k.x[(i - 1) * 128:i * 128, :], writes=[bh])
            s_, bs = ss[p], b_ss[p]
            S.op("act", lambda: nc.scalar.activation(out=junk[:], in_=h_[:], func=AF.Square,
                                                     accum_out=s_[:, 0:1]),
                 reads=[bh], writes=[b_junk, bs])
            S.op("act", lambda: nc.scalar.activation(out=s_[:, 1:2], in_=s_[:, 0:1], func=AF.Sqrt,
                                                     scale=1.0 / D, bias=EPS),
                 reads=[bs], writes=[bs])
            S.op("dve", lambda: nc.vector.reciprocal(s_[:, 2:3], s_[:, 1:2]), reads=[bs], writes=[bs])
            n_, bn = nb[p], b_nb[p]
            S.op("dve", lambda: nc.vector.scalar_tensor_tensor(out=n_[:], in0=h_[:], scalar=s_[:, 2:3],
                                                               in1=g1[:], op0=ALU.mult, op1=ALU.mult),
                 reads=[bh, bs, b_g1], writes=[bn])
            ps, bp = k.nextps()
            psb = ps[:].bitcast(BF16).rearrange("p (k t) -> p k t", k=8)
            fns = []
            for kk in range(8):
                fns.append(lambda kk=kk: nc.tensor.transpose(psb[:, kk, :], n_[:, kk * 128:(kk + 1) * 128],
                                                             k.identb[:]))
            S.group("pe", fns, reads=[bn, k.b_ident], writes=[bp])
            S.op("dve", lambda: nc.vector.tensor_copy(nT[:, :, 3 + i * 128:3 + (i + 1) * 128], psb),
                 reads=[bp], writes=[b_nT[i]])

        endsub(st)
        st = substack()
        cnt = {"g": 0, "t": 0, "g2": 0}
        wstg = sb("wstg", [128, 8, 128], F32); b_wstg = Buf("wstg")
        wc = [sb("wc%d" % i, [128, 8, 128], BF16) for i in range(2)]; b_wc = [Buf("wc%d" % i) for i in range(2)]
        cwT = [sb("cwT%d" % i, [128, 4], F32) for i in range(2)]; b_cwT = [Buf("cwT%d" % i) for i in range(2)]
        pT = [sb("pT%d" % i, [128, 3 + T], F32) for i in range(2)]
        b_pT = [[Buf("pT%d_%d" % (i, tb)) for tb in range(9)] for i in range(2)]
        b_pTpad = [Buf("pTpad%d" % i) for i in range(2)]
        for i in range(2):
            S.op("pool", (lambda i=i: nc.gpsimd.memset(pT[i][:, 0:3], 0.0)), writes=[b_pTpad[i]])
        accs = [sb("accs%d" % i, [128, 512], F32) for i in range(2)]; b_accs = [Buf("accs%d" % i) for i in range(2)]
        yTb = [sb("yTb%d" % i, [128, 512], BF16) for i in range(2)]; b_yTb = [Buf("yTb%d" % i) for i in range(2)]
        tmf = [sb("tmf%d" % i, [128, 4, 128], F32) for i in range(2)]; b_tmf = [Buf("tmf%d" % i) for i in range(2)]
        sqf = sb("sqf", [128, 4, 128], F32); b_sqf = Buf("sqf")
        obt = [sb("obt%d" % i, [128, 4, 128], BF16) for i in range(3)]; b_obt = [Buf("obt%d" % i) for i in range(3)]

        blocks = [(g, tb) for g in range(24) for tb in range(9)]
        gbuf = {}

        def conv_X(bi):
            g, tb = blocks[bi]
            cc0 = g * 128
            if tb == 0:
                gp = cnt["g"] % 2
                cnt["g"] += 1
                gbuf[g] = gp
                S.dma_op("sp", wstg[:], k.w_in[:, cc0:cc0 + 128].rearrange("(k p) n -> p k n", p=128),
                         writes=[b_wstg])
                S.op("pool", lambda: nc.gpsimd.tensor_copy(wc[gp][:], wstg[:]), reads=[b_wstg], writes=[b_wc[gp]])
                S.dma_op("sp", cwT[gp][:], k.conv_wT[cc0:cc0 + 128, :], writes=[b_cwT[gp]])
            gp = gbuf[g]
            c0 = tb * 512
            n = min(512, T - c0)
            tiles = list(range(c0 // 128, (c0 + n) // 128))
            ps, bp = k.nextps()
            fns = [(lambda kk=kk: nc.tensor.matmul(ps[:, 0:n], lhsT=wc[gp][:, kk, :],
                                                   rhs=nT[:, kk, 3 + c0:3 + c0 + n],
                                                   start=(kk == 0), stop=(kk == 7))) for kk in range(8)]
            S.group("pe", fns, reads=[b_wc[gp]] + [b_nT[i] for i in tiles], writes=[bp])
            S.op("act", lambda: nc.scalar.copy(out=pT[gp][:, 3 + c0:3 + c0 + n], in_=ps[:, 0:n]),
                 reads=[bp], writes=[b_pT[gp][tb]])
            rdp = [b_pT[gp][tb], b_pT[gp][tb - 1] if tb > 0 else b_pTpad[gp]]
            ap = bi % 2
            S.op("act", lambda: nc.scalar.activation(out=accs[ap][:, 0:n], in_=pT[gp][:, c0:c0 + n],
                                                     func=AF.Copy, scale=cwT[gp][:, 0:1]),
                 reads=rdp + [b_cwT[gp]], writes=[b_accs[ap]])
            for j in range(1, 4):
                eng = "dve"
                h = nc.vector
                S.op(eng, (lambda j=j, h=h: h.scalar_tensor_tensor(
                    out=accs[ap][:, 0:n], in0=pT[gp][:, c0 + j:c0 + j + n], scalar=cwT[gp][:, j:j + 1],
                    in1=accs[ap][:, 0:n], op0=ALU.mult, op1=ALU.add)),
                    reads=rdp + [b_cwT[gp], b_accs[ap]], writes=[b_accs[ap]])

        def conv_Y(bi):
            g, tb = blocks[bi]
            kind = "qkv"[g // 8]
            hd = g % 8
            dst = {"q": k.QG, "k": k.KG, "v": k.VG}[kind]
            c0 = tb * 512
            n = min(512, T - c0)
            nt = n // 128
            ap = bi % 2
            t3 = bi % 3
            S.op("act", lambda: nc.scalar.activation(out=yTb[ap][:, 0:n], in_=accs[ap][:, 0:n], func=AF.Silu),
                 reads=[b_accs[ap]], writes=[b_yTb[ap]])
            ps2, bp2 = k.nextps()
            psb = ps2[:].bitcast(BF16).rearrange("p (k t) -> p k t", k=8)
            fns = [(lambda t=t: nc.tensor.transpose(psb[:, t, :], yTb[ap][:, t * 128:(t + 1) * 128],
                                                    k.identb[:])) for t in range(nt)]
            S.group("pe", fns, reads=[b_yTb[ap], k.b_ident], writes=[bp2])
            o_, bo = obt[t3], b_obt[t3]
            if kind == "v":
                S.op("act", lambda: nc.scalar.copy(out=o_[:, 0:nt, :], in_=psb[:, 0:nt, :]),
                     reads=[bp2], writes=[bo])
            else:
                tm, btm = tmf[ap], b_tmf[ap]
                q_, bq = sq[ap], b_sq[ap]
                S.op("dve", lambda: nc.vector.tensor_copy(tm[:, 0:nt, :], psb[:, 0:nt, :]),
                     reads=[bp2], writes=[btm])
                S.op("dve", lambda: nc.vector.tensor_tensor(out=sqf[:, 0:nt, :], in0=tm[:, 0:nt, :],
                                                            in1=tm[:, 0:nt, :], op=ALU.mult),
                     reads=[btm], writes=[b_sqf])
                S.op("dve", lambda: nc.vector.tensor_reduce(out=q_[:, 0:nt], in_=sqf[:, 0:nt, :], axis=AX.X,
                                                            op=ALU.add), reads=[b_sqf], writes=[bq])
                sc = 128.0 if kind == "q" else 1.0
                S.op("pool", lambda: nc.gpsimd.tensor_scalar(out=q_[:, 0:nt], in0=q_[:, 0:nt], scalar1=sc,
                                                             scalar2=sc * EPS, op0=ALU.mult, op1=ALU.add),
                     reads=[bq], writes=[bq])
                S.op("pool", lambda: nc.gpsimd.tensor_tensor(out=q_[:, 4:4 + nt], in0=q_[:, 0:nt],
                                                             in1=nhalf[:, 0:nt], op=ALU.pow),
                     reads=[bq, b_nhalf], writes=[bq])


            def tail():
                if kind != "v":
                    S.op("dve", lambda: nc.vector.tensor_tensor(
                        out=o_[:, 0:nt, :], in0=tm[:, 0:nt, :],
                        in1=q_[:, 4:4 + nt].unsqueeze(2).broadcast_to([128, nt, 128]), op=ALU.mult),
                        reads=[btm, bq], writes=[bo])
                S.dma_op("sp", dst[c0:c0 + n, hd * 128:(hd + 1) * 128].rearrange("(t p) c -> p t c", p=128),
                         o_[:, 0:nt, :], reads=[bo])
            return tail

        def conv_group(cc0, kind):
            gp = cnt["g"] % 2
            cnt["g"] += 1
            S.dma_op("sp", wst[0][:], k.w_in[:, cc0:cc0 + 512].rearrange("(k p) n -> p k n", p=128),
                     writes=[b_wst[0]])
            S.dma_op("sp", cw[0][:], k.conv_w[:, cc0:cc0 + 512].partition_broadcast(128),
                     writes=[b_cw[0]])
            for j in range(4):
                S.op("dve", lambda: nc.vector.tensor_tensor(
                    out=w4[gp][:, :, j, :], in0=wst[0][:],
                    in1=cw[0][:, j:j + 1, :].broadcast_to([128, 8, 512]), op=ALU.mult),
                    reads=[b_wst[0], b_cw[0]], writes=[b_w4[gp]])
            dst = {"q": k.QG, "k": k.KG, "v": k.VG}[kind]
            dcol = cc0 % 1024
            for i in range(NT):
                ps, bp = k.nextps()
                fns = []
                for j in range(4):
                    for kk in range(8):
                        fns.append(lambda j=j, kk=kk: nc.tensor.matmul(
                            ps[:], lhsT=nT[:, kk, i * 128 + j:i * 128 + j + 128], rhs=w4[gp][:, kk, j, :],
                            start=(j == 0 and kk == 0), stop=(j == 3 and kk == 7)))
                rd = [b_w4[gp], b_nT[i], b_nTpad] + ([b_nT[i - 1]] if i > 0 else [])
                S.group("pe", fns, reads=rd, writes=[bp])
                tp = cnt["t"] % 2
                t3 = cnt["t"] % 3
                cnt["t"] += 1
                o_, bo = ob[t3], b_ob[t3]
                if kind == "v":
                    S.op("act", lambda: nc.scalar.activation(out=o_[:], in_=ps[:], func=AF.Silu),
                         reads=[bp], writes=[bo])
                else:
                    y_, by = ysb[tp], b_ysb[tp]
                    q_, bq = sq[tp], b_sq[tp]
                    S.op("act", lambda: nc.scalar.activation(out=y_[:], in_=ps[:], func=AF.Silu),
                         reads=[bp], writes=[by])
                    for hh in range(4):
                        S.op("dve", lambda: nc.vector.scalar_tensor_tensor(
                            out=junkf[:], in0=y_[:, hh * 128:(hh + 1) * 128], scalar=1.0,
                            in1=y_[:, hh * 128:(hh + 1) * 128], op0=ALU.mult, op1=ALU.mult,
                            accum_out=q_[:, hh:hh + 1]),
                            reads=[by], writes=[b_junkf, bq])
                    sc = 128.0 if kind == "q" else 1.0
                    S.op("pool", lambda: nc.gpsimd.tensor_scalar(out=q_[:, 0:4], in0=q_[:, 0:4], scalar1=sc,
                                                                 scalar2=sc * EPS, op0=ALU.mult, op1=ALU.add),
                         reads=[bq], writes=[bq])
                    S.op("pool", lambda: nc.gpsimd.tensor_tensor(out=q_[:, 4:8], in0=q_[:, 0:4], in1=nhalf[:],
                                                                 op=ALU.pow),
                         reads=[bq, b_nhalf], writes=[bq])
                    for hh in range(4):
                        S.op("dve", lambda: nc.vector.tensor_scalar(
                            out=o_[:, hh * 128:(hh + 1) * 128], in0=y_[:, hh * 128:(hh + 1) * 128],
                            scalar1=q_[:, 4 + hh:5 + hh], scalar2=None, op0=ALU.mult),
                            reads=[by, bq], writes=[bo])
                S.dma_op("sp", dst[i * 128:(i + 1) * 128, dcol:dcol + 512], o_[:], reads=[bo])

        def conv_stream():
            conv_X(0)
            pend_tail = None
            for bi in range(len(blocks)):
                if bi + 1 < len(blocks):
                    conv_X(bi + 1)
                yield
                tl = conv_Y(bi)
                if pend_tail is not None:
                    pend_tail()
                pend_tail = tl
                yield
            pend_tail()

        wt = [sb("wt%d" % i, [128, 8, 512], BF16) for i in range(2)]
        b_wt = [Buf("wt%d" % i) for i in range(2)]
        albc = sb("albc", [128, 16], F32)
        b_albc = Buf("albc")
        S.dma_op("sp", albc[:, 0:8], k.a_log.partition_broadcast(128), writes=[b_albc])
        S.dma_op("sp", albc[:, 8:16], k.dt_bias.partition_broadcast(128), writes=[b_albc])
        S.op("act", lambda: nc.scalar.activation(out=albc[:, 0:8], in_=albc[:, 0:8], func=AF.Exp),
             reads=[b_albc], writes=[b_albc])

        def plain_group(c0, ncols, post):
            gp = cnt["g2"] % 2
            cnt["g2"] += 1
            S.dma_op("pool", wt[gp][:, :, 0:ncols], k.w_in[:, c0:c0 + ncols].rearrange("(k p) n -> p k n", p=128),
                     writes=[b_wt[gp]])
            for i in range(NT):
                ps, bp = k.nextps()
                fns = []
                for kk in range(8):
                    fns.append(lambda kk=kk: nc.tensor.matmul(
                        ps[:, 0:ncols], lhsT=nT[:, kk, 3 + i * 128:3 + (i + 1) * 128], rhs=wt[gp][:, kk, 0:ncols],
                        start=(kk == 0), stop=(kk == 7)))
                S.group("pe", fns, reads=[b_wt[gp], b_nT[i]], writes=[bp])
                post(i, ps, bp)
                yield

        def post_act(func, dst, dcol, ncols, bf, scale=1.0):
            def post(i, ps, bp):
                t3 = cnt["t"] % 3
                cnt["t"] += 1
                o_, bo = (ob[t3], b_ob[t3]) if bf else (of[t3], b_of[t3])
                S.op("act", lambda: nc.scalar.activation(out=o_[:, 0:ncols], in_=ps[:, 0:ncols], func=func,
                                                         scale=scale),
                     reads=[bp], writes=[bo])
                S.dma_op("sp", dst[i * 128:(i + 1) * 128, dcol:dcol + ncols], o_[:, 0:ncols], reads=[bo])
            return post

        def post_bg(i, ps, bp):
            t3 = cnt["t"] % 3
            cnt["t"] += 1
            o_, bo = of[t3], b_of[t3]
            S.op("act", lambda: nc.scalar.activation(out=o_[:, 0:8], in_=ps[:, 0:8], func=AF.Sigmoid),
                 reads=[bp], writes=[bo])
            S.op("dve", lambda: nc.vector.tensor_tensor(out=o_[:, 16:24], in0=ps[:, 8:16], in1=albc[:, 8:16],
                                                        op=ALU.add),
                 reads=[bp, b_albc], writes=[bo])
            S.op("act", lambda: nc.scalar.activation(out=o_[:, 16:24], in_=o_[:, 16:24], func=AF.Exp),
                 reads=[bo], writes=[bo])
            S.op("act", lambda: nc.scalar.activation(out=o_[:, 16:24], in_=o_[:, 16:24], func=AF.Ln, bias=1.0),
                 reads=[bo], writes=[bo])
            S.op("dve", lambda: nc.vector.scalar_tensor_tensor(out=o_[:, 8:16], in0=o_[:, 16:24], scalar=-1.0,
                                                               in1=albc[:, 0:8], op0=ALU.mult, op1=ALU.mult),
                 reads=[bo, b_albc], writes=[bo])
            S.dma_op("sp", k.BG[i * 128:(i + 1) * 128, :], o_[:, 0:16], reads=[bo])
        cosT = sb("cosT", [128, T], F32)
        sinT = sb("sinT", [128, T], F32)
        b_tab = Buf("ropetab")
        wf = [sb("wf%d" % i, [128, 8, 256], BF16) for i in range(2)]
        b_wf = [Buf("wf%d" % i) for i in range(2)]
        t1 = [sb("t1_%d" % i, [128, 512], F32) for i in range(2)]
        b_t1 = [Buf("t1_%d" % i) for i in range(2)]
        t2 = [sb("t2_%d" % i, [128, 512], F32) for i in range(2)]
        b_t2 = [Buf("t2_%d" % i) for i in range(2)]

        def load_tab(c, s):
            S.dma_op("sp", cosT[:], c, writes=[b_tab])
            S.dma_op("sp", sinT[:], s, writes=[b_tab])

        def rope_group(w_ap, dst):
            gp = cnt["g2"] % 2
            cnt["g2"] += 1
            S.dma_op("pool", wf[gp][:], w_ap.rearrange("(k p) n -> p k n", p=128), writes=[b_wf[gp]])
            for tb in range(9):
                c0 = tb * 512
                n = min(512, T - c0)
                tiles = list(range(c0 // 128, (c0 + n) // 128))
                psA, bpA = k.nextps()
                psB, bpB = k.nextps()
                for (ps, bp, off) in ((psA, bpA, 0), (psB, bpB, 128)):
                    fns = []
                    for kk in range(8):
                        fns.append(lambda kk=kk, ps=ps, off=off: nc.tensor.matmul(
                            ps[:, 0:n], lhsT=wf[gp][:, kk, off:off + 128], rhs=nT[:, kk, 3 + c0:3 + c0 + n],
                            start=(kk == 0), stop=(kk == 7)))
                    S.group("pe", fns, reads=[b_wf[gp]] + [b_nT[i] for i in tiles], writes=[bp])
                tp = cnt["t"] % 2
                t3 = cnt["t"] % 3
                cnt["t"] += 1
                S.op("dve", lambda: nc.vector.tensor_tensor(out=t1[tp][:, 0:n], in0=psA[:, 0:n],
                                                            in1=cosT[:, c0:c0 + n], op=ALU.mult),
                     reads=[bpA, b_tab], writes=[b_t1[tp]])
                S.op("dve", lambda: nc.vector.tensor_tensor(out=t2[tp][:, 0:n], in0=psB[:, 0:n],
                                                            in1=sinT[:, c0:c0 + n], op=ALU.mult),
                     reads=[bpB, b_tab], writes=[b_t2[tp]])
                o_, bo = ob[t3], b_ob[t3]
                S.op("pool", lambda: nc.gpsimd.tensor_tensor(out=o_[:, 0:n], in0=t1[tp][:, 0:n],
                                                             in1=t2[tp][:, 0:n], op=ALU.add),
                     reads=[b_t1[tp], b_t2[tp]], writes=[bo])
                S.dma_op("sp", dst[:, c0:c0 + n], o_[:, 0:n], reads=[bo])
                yield

        def proj_stream():
            for cb in range(2):
                yield from plain_group(C_GZ + cb * 512, 512, post_act(AF.Silu, k.ZS, cb * 512, 512, False))
            for cb in range(2):
                yield from plain_group(C_GTA + cb * 512, 512, post_act(AF.Sigmoid, k.SGA, cb * 512, 512, False))
            for cb in range(2):
                yield from plain_group(C_GTB + cb * 512, 512, post_act(AF.Sigmoid, k.SGB, cb * 512, 512, False))
            yield from plain_group(C_AV, 256, post_act(AF.Copy, k.Vd, 0, 256, True))
            yield from plain_group(C_IW, 8, post_act(AF.Copy, k.IW, 0, 8, False, scale=512.0 ** -0.5))
            yield from plain_group(C_GB, 16, post_bg)
            load_tab(k.cosA, k.sinA)
            for h in range(8):
                yield from rope_group(k.w_sw[:, h * 256:(h + 1) * 256], k.QT[h])
            for h in range(2):
                yield from rope_group(k.w_sw[:, (8 + h) * 256:(9 + h) * 256], k.KT[h])
            load_tab(k.cosI, k.sinI)
            for h in range(4):
                yield from rope_group(k.w_sw[:, (10 + h) * 256:(11 + h) * 256], k.QIT[h])
            yield from rope_group(k.w_sw[:, 14 * 256:15 * 256], k.KIT)

        k.ps_pools = {"conv": (0, 3), "proj": (3, 5)}
        k.ps_rrs = {}
        gA, nA = conv_stream(), 2.0 * len(blocks)
        gB, nB = proj_stream(), 10.0 * NT + 15 * 9
        aA = aB = True
        pA = pB = 0.0
        while aA or aB:
            if aA and (not aB or pA <= pB):
                k.cur_stream = "conv"
                try:
                    next(gA)
                    pA += 1.0 / nA
                except StopIteration:
                    aA = False
            else:
                k.cur_stream = "proj"
                try:
                    next(gB)
                    pB += 1.0 / nB
                except StopIteration:
                    aB = False
        k.cur_stream = None
        endsub(st)


def phase_C(k):
    nc, S = k.nc, k.S
    NIT = 14
    with ExitStack() as es:
        def sb(name, shape, dt):
            return es.enter_context(nc.sbuf_tensor(name, list(shape), dt))
        kit = sb("kit", [128, T], BF16)
        kts = sb("kts", [128, 2, T], BF16)
        vs = sb("vs", [128, NT, 256], BF16)
        wd = sb("wd_sb", [128, 8, D], BF16)
        b_res = Buf("resC")
        S.dma_op("sp", kit[:], k.KIT, writes=[b_res])
        S.dma_op("sp", kts[:], k.KT.rearrange("g p t -> p g t"), writes=[b_res])
        S.dma_op("sp", vs[:], k.Vd.rearrange("(j p) c -> p j c", p=128), writes=[b_res])
        S.dma_op("pool", wd[:], k.wd.rearrange("(h p) n -> p h n", p=128), writes=[b_res])
        msk = sb("msk", [128, 3, 128], F32)
        S.dma_op("sp", msk[:], k.masks.rearrange("m p c -> p m c"), writes=[b_res])
        p2 = sb("p2", [128, NIT + 1], F32)
        S.dma_op("sp", p2[:], k.pow2.partition_broadcast(128), writes=[b_res])
        ones = sb("onesC", [128, 128], BF16)
        S.op("pool", lambda: nc.gpsimd.memset(ones[:], 1.0), writes=[b_res])
        S.barrier()

        I2 = [sb("I%d" % i, [128, T], F32) for i in range(2)]; b_I2 = [Buf("I%d" % i) for i in range(2)]
        M = sb("M", [128, T], BF16); b_M = Buf("M")
        MT2 = [sb("MT%d" % i, [128, NT, 128], BF16) for i in range(2)]; b_MT2 = [Buf("MT%d" % i) for i in range(2)]
        jk = sb("jkC", [128, T], BF16); b_jk = Buf("jkC")
        qit = [sb("qit%d" % i, [128, 4, 128], BF16) for i in range(2)]; b_qit = [Buf("qit%d" % i) for i in range(2)]
        qt = [sb("qt%d" % i, [128, 8, 128], BF16) for i in range(2)]; b_qt = [Buf("qt%d" % i) for i in range(2)]
        iw = [sb("iw%d" % i, [128, 8], F32) for i in range(2)]; b_iw = [Buf("iw%d" % i) for i in range(2)]
        sgb = [sb("sgb%d" % i, [128, D], F32) for i in range(2)]; b_sgb = [Buf("sgb%d" % i) for i in range(2)]
        r_ = [sb("r_%d" % i, [128, 512], F32) for i in range(2)]; b_r = [Buf("r_%d" % i) for i in range(2)]
        e_ = [sb("e_%d" % i, [128, 512], BF16) for i in range(2)]; b_e = [Buf("e_%d" % i) for i in range(2)]
        p_ = [sb("p_%d" % i, [128, 512], BF16) for i in range(3)]; b_p = [Buf("p_%d" % i) for i in range(3)]
        rden = sb("rden", [128, 512], F32); b_rden = Buf("rden")
        ot = [sb("ot%d" % i, [128, 4, 128], BF16) for i in range(2)]; b_ot = [Buf("ot%d" % i) for i in range(2)]
        md = sb("md", [128, D], F32); b_md = Buf("md")
        st = sb("stC", [128, 8], F32); b_st = Buf("stC")
        w2 = sb("w2C", [128, NIT + 1], F32); b_w2 = Buf("w2C")
        tmpd = sb("tmpd", [128, 128], F32); b_tmpd = Buf("tmpd")
        midt = sb("midt", [128, 1], F32); b_mid = Buf("midt")
        cn = sb("cnC", [128, 1], F32); b_cn = Buf("cnC")
        sa = sb("saC", [128, 1], F32); b_sa = Buf("saC")
        gt = sb("gtC", [128, 2], F32); b_gt = Buf("gtC")
        jk2 = sb("jk2C", [128, T], BF16); b_jk2 = Buf("jk2C")
        k.ps_pools = {"idx": (4, 2), "bis": (6, 2)}
        k.ps_rrs = {}
        psOg = [k.PS[2], k.PS[2]]
        bOg = [k.PB[2], k.PB[2]]
        psDg = [k.PS[3], k.PS[3]]
        bDg = [k.PB[3], k.PB[3]]
        att_rr = [0]

        def attps():
            i = att_rr[0] % 2
            att_rr[0] += 1
            return k.PS[i], k.PB[i]
        cnt = {"r": 0, "e": 0}
        SC = 128.0 ** -0.5

        def stage_idx(i):
            nk = i + 1
            NK = nk * 128
            par = i % 2
            I, b_I = I2[i % 2], b_I2[i % 2]
            yield
            S.dma_op("sp", qit[par][:], k.QIT[:, :, i * 128:(i + 1) * 128].rearrange("g p t -> p g t"),
                     writes=[b_qit[par]])
            yield
            S.dma_op("sp", iw[par][:], k.IW[i * 128:(i + 1) * 128, :], writes=[b_iw[par]])
            for c0 in range(0, NK, 512):
                n = min(512, NK - c0)
                for h in range(8):
                    g, hf = h // 2, h % 2
                    ps, bp = k.nextps()
                    yield
                    S.op("pe", lambda: nc.tensor.matmul(ps[:, 0:n], lhsT=qit[par][64 * hf:64 * hf + 64, g, :],
                                                        rhs=kit[64 * hf:64 * hf + 64, c0:c0 + n],
                                                        start=True, stop=True),
                         reads=[b_qit[par]], writes=[bp])
                    rp = cnt["r"] % 2
                    cnt["r"] += 1
                    yield
                    S.op("act", lambda: nc.scalar.activation(out=r_[rp][:, 0:n], in_=ps[:, 0:n], func=AF.Relu),
                         reads=[bp], writes=[b_r[rp]])
                    if h == 0:
                        yield
                        S.op("dve", lambda: nc.vector.tensor_scalar(out=I[:, c0:c0 + n], in0=r_[rp][:, 0:n],
                                                                    scalar1=iw[par][:, 0:1], scalar2=None,
                                                                    op0=ALU.mult),
                             reads=[b_r[rp], b_iw[par]], writes=[b_I])
                    else:
                        yield
                        S.op("dve", lambda: nc.vector.scalar_tensor_tensor(
                            out=I[:, c0:c0 + n], in0=r_[rp][:, 0:n], scalar=iw[par][:, h:h + 1],
                            in1=I[:, c0:c0 + n], op0=ALU.mult, op1=ALU.add),
                            reads=[b_r[rp], b_iw[par]], writes=[b_I])
            m0 = 1 if i == 0 else 2
            yield
            S.op("dve", lambda: nc.vector.tensor_tensor(out=I[:, 0:128], in0=I[:, 0:128], in1=msk[:, m0, :],
                                                        op=ALU.add), reads=[b_I], writes=[b_I])
            if i >= 1:
                yield
                S.op("dve", lambda: nc.vector.tensor_tensor(out=I[:, i * 128:NK], in0=I[:, i * 128:NK],
                                                            in1=msk[:, 0, :], op=ALU.add),
                     reads=[b_I], writes=[b_I])

        def stage_bis(i):
            nk = i + 1
            NK = nk * 128
            par = i % 2
            I, b_I = I2[i % 2], b_I2[i % 2]
            yield
            S.dma_op("sp", qt[par][:], k.QT[:, :, i * 128:(i + 1) * 128].rearrange("g p t -> p g t"),
                     writes=[b_qt[par]])
            yield
            S.dma_op("sp", sgb[par][:], k.SGB[i * 128:(i + 1) * 128, :], writes=[b_sgb[par]])
            if i < 2:
                yield
                S.op("dve", lambda: nc.vector.memset(st[:, 6:7], -1e29), writes=[b_st])
            else:
                yield
                S.op("dve", lambda: nc.vector.tensor_reduce(out=st[:, 0:1], in_=I[:, 0:NK], axis=AX.X, op=ALU.max),
                     reads=[b_I], writes=[b_st])
                yield
                S.op("dve", lambda: nc.vector.tensor_reduce(out=st[:, 1:2], in_=I[:, PAD:i * 128], axis=AX.X,
                                                            op=ALU.min), reads=[b_I], writes=[b_st])
                if i == 2:
                    yield
                    S.op("dve", lambda: nc.vector.scalar_tensor_tensor(out=tmpd[:], in0=msk[:, 0, :], scalar=-2.0,
                                                                       in1=I[:, i * 128:NK], op0=ALU.mult,
                                                                       op1=ALU.add),
                         reads=[b_I], writes=[b_tmpd])
                    yield
                    S.op("dve", lambda: nc.vector.tensor_reduce(out=st[:, 7:8], in_=tmpd[:], axis=AX.X, op=ALU.min),
                         reads=[b_tmpd], writes=[b_st])
                    yield
                    S.op("dve", lambda: nc.vector.tensor_tensor(out=st[:, 1:2], in0=st[:, 1:2], in1=st[:, 7:8],
                                                                op=ALU.min), reads=[b_st], writes=[b_st])
                yield
                S.op("dve", lambda: nc.vector.tensor_tensor(out=st[:, 2:3], in0=st[:, 0:1], in1=st[:, 1:2],
                                                            op=ALU.subtract), reads=[b_st], writes=[b_st])
                yield
                S.op("dve", lambda: nc.vector.tensor_scalar(out=w2[:], in0=p2[:], scalar1=st[:, 2:3], scalar2=None,
                                                            op0=ALU.mult), reads=[b_st], writes=[b_w2])
                yield
                S.op("dve", lambda: nc.vector.tensor_tensor(out=st[:, 3:4], in0=st[:, 1:2], in1=w2[:, 0:1],
                                                            op=ALU.add), reads=[b_st, b_w2], writes=[b_st])
                hc = NK if nk <= 3 else 128 * max(1, int(round(0.45 * nk)))
                na = NK - hc
                yield
                S.op("dve", lambda: nc.vector.tensor_copy(midt[:], st[:, 3:4]), reads=[b_st], writes=[b_mid])
                for it in range(NIT):
                    if na > 0:
                        yield
                        S.op("act", lambda: nc.scalar.activation(out=jk2[:, 0:na], in_=I[:, hc:NK], func=AF.Sign,
                                                                 scale=-1.0, bias=midt[:, 0:1], accum_out=sa[:, 0:1]),
                             reads=[b_I, b_mid], writes=[b_jk2, b_sa])
                    yield
                    S.op("dve", lambda: nc.vector.tensor_scalar(out=jk[:, 0:hc], in0=I[:, 0:hc], scalar1=midt[:, 0:1],
                                                                scalar2=None, op0=ALU.is_ge, op1=ALU.add,
                                                                accum_out=cn[:, 0:1]),
                         reads=[b_I, b_mid], writes=[b_jk, b_cn])
                    if na > 0:
                        yield
                        S.op("dve", lambda: nc.vector.scalar_tensor_tensor(out=gt[:, 0:1], in0=sa[:, 0:1], scalar=-0.5,
                                                                           in1=cn[:, 0:1], op0=ALU.mult, op1=ALU.add),
                             reads=[b_sa, b_cn], writes=[b_gt])
                        src, bsrc = gt[:, 0:1], b_gt
                    else:
                        src, bsrc = cn[:, 0:1], b_cn
                    yield
                    S.op("dve", lambda: nc.vector.tensor_scalar(out=gt[:, 1:2], in0=src, scalar1=255.5 - 0.5 * na,
                                                                scalar2=-0.5, op0=ALU.is_ge, op1=ALU.add),
                         reads=[bsrc], writes=[b_gt])
                    yield
                    S.op("dve", lambda: nc.vector.scalar_tensor_tensor(out=midt[:, 0:1], in0=gt[:, 1:2],
                                                                       scalar=w2[:, it:it + 1], in1=midt[:, 0:1],
                                                                       op0=ALU.mult, op1=ALU.add),
                         reads=[b_gt, b_w2, b_mid], writes=[b_mid])
                yield
                S.op("dve", lambda: nc.vector.tensor_copy(st[:, 3:4], midt[:]), reads=[b_mid], writes=[b_st])
                yield
                S.op("dve", lambda: nc.vector.tensor_scalar(out=st[:, 6:7], in0=st[:, 3:4], scalar1=w2[:, NIT:NIT + 1],
                                                            scalar2=-1e29, op0=ALU.subtract, op1=ALU.max),
                     reads=[b_st, b_w2], writes=[b_st])
            yield
            S.op("dve", lambda: nc.vector.tensor_scalar(out=M[:, 0:NK], in0=I[:, 0:NK], scalar1=st[:, 6:7],
                                                        scalar2=None, op0=ALU.is_ge),
                 reads=[b_I, b_st], writes=[b_M])
        def stage1b(i):
            nk = i + 1
            MT, b_MT = MT2[i % 2], b_MT2[i % 2]
            for j0 in range(0, nk, 8):
                nj = min(8, nk - j0)
                ps, bp = k.nextps()
                psb = ps[:].bitcast(BF16).rearrange("p (k t) -> p k t", k=8)
                fns = [(lambda jj=jj: nc.tensor.transpose(psb[:, jj, :], M[:, (j0 + jj) * 128:(j0 + jj + 1) * 128],
                                                          k.identb[:])) for jj in range(nj)]
                yield
                S.group("pe", fns, reads=[b_M, k.b_ident], writes=[bp])
                yield
                S.op("act", lambda: nc.scalar.activation(out=MT[:, j0:j0 + nj, :], in_=psb[:, 0:nj, :], func=AF.Copy,
                                                         scale=30000.0, bias=-30000.0),
                     reads=[bp], writes=[b_MT])
        def stage2(i):
            nk = i + 1
            par = i % 2
            MT, b_MT = MT2[i % 2], b_MT2[i % 2]
            steps = [(g, j) for g in range(2) for j in range(nk)]

            def emitS(g, j):
                ps, bp = attps()
                S.group("pe", [
                    lambda: nc.tensor.matmul(ps[:], lhsT=kts[:, g, j * 128:(j + 1) * 128],
                                             rhs=qt[par][:, 4 * g:4 * g + 4, :], start=True, stop=False),
                    lambda: nc.tensor.matmul(ps[:], lhsT=k.identb[:],
                                             rhs=MT[:, j:j + 1, :].broadcast_to([128, 4, 128]),
                                             start=False, stop=True)],
                    reads=[b_qt[par], b_MT, k.b_ident], writes=[bp])
                return ps, bp
            pend = []
            for sidx in range(min(1, len(steps))):
                yield
                pend.append(emitS(*steps[sidx]))
            for sidx, (g, j) in enumerate(steps):
                ps, bp = pend.pop(0)
                ep = cnt["e"] % 3
                cnt["e"] += 1
                yield
                S.op("act", lambda: nc.scalar.activation(out=p_[ep][:], in_=ps[:], func=AF.Exp, scale=SC),
                     reads=[bp], writes=[b_p[ep]])
                if sidx + 1 < len(steps):
                    yield
                    pend.append(emitS(*steps[sidx + 1]))
                yield
                S.op("pe", lambda: nc.tensor.matmul(psOg[g][:], lhsT=vs[:, j, g * 128:(g + 1) * 128], rhs=p_[ep][:],
                                                    start=(j == 0), stop=(j == nk - 1)),
                     reads=[b_p[ep]], writes=[bOg[g]])
                yield
                S.op("pe", lambda: nc.tensor.matmul(psDg[g][:], lhsT=ones[:], rhs=p_[ep][:],
                                                    start=(j == 0), stop=(j == nk - 1)),
                     reads=[b_p[ep]], writes=[bDg[g]])
                if j == nk - 1:
                    yield
                    S.op("dve", lambda: nc.vector.tensor_scalar(out=rden[:], in0=psDg[g][:], scalar1=1e-20,
                                                                scalar2=None, op0=ALU.max),
                         reads=[bDg[g]], writes=[b_rden])
                    yield
                    S.op("dve", lambda: nc.vector.reciprocal(rden[:], rden[:]), reads=[b_rden], writes=[b_rden])
                    yield
                    S.op("dve", lambda: nc.vector.tensor_tensor(out=ot[g][:].rearrange("p h t -> p (h t)"),
                                                                in0=psOg[g][:], in1=rden[:], op=ALU.mult),
                         reads=[bOg[g], b_rden], writes=[b_ot[g]])
            for cb in range(2):
                ps, bp = attps()
                fns = [(lambda h=h: nc.tensor.matmul(ps[:], lhsT=ot[h // 4][:, h % 4, :],
                                                     rhs=wd[:, h, cb * 512:(cb + 1) * 512],
                                                     start=(h == 0), stop=(h == 7))) for h in range(8)]
                yield
                S.group("pe", fns, reads=[b_ot[0], b_ot[1]], writes=[bp])
                yield
                S.op("dve", lambda: nc.vector.tensor_tensor(out=md[:, cb * 512:(cb + 1) * 512], in0=ps[:],
                                                            in1=sgb[par][:, cb * 512:(cb + 1) * 512], op=ALU.mult),
                     reads=[bp, b_sgb[par]], writes=[b_md])
            yield
            S.dma_op("sp", k.MDSA[i * 128:(i + 1) * 128, :], md[:], reads=[b_md])

        def drain(g):
            for _ in g:
                pass

        def chain(*gs):
            for g in gs:
                yield from g

        def lock3(streams):
            prog = [0.0] * len(streams)
            alive = [True] * len(streams)
            while any(alive):
                j = min((x for x in range(len(streams)) if alive[x]), key=lambda x: prog[x])
                k.cur_stream = streams[j][2]
                try:
                    next(streams[j][0])
                    prog[j] += 1.0 / streams[j][1]
                except StopIteration:
                    alive[j] = False
            k.cur_stream = None

        def est_idx(i):
            return 2 + (((i + 1) + 3) // 4) * 24 + 3

        def est_bis(i):
            return 2 + (NIT * 5 + 12 if i >= 2 else 3) + 2 * (((i + 1) + 7) // 8)

        def est2(i):
            return 6 * (i + 1) + 12

        lock3([[stage_idx(0), 1.0, "idx"]])
        lock3([[chain(stage_bis(0), stage1b(0)), 1.0, "bis"], [stage_idx(1), 1.0, "idx"]])
        for i in range(NT):
            st_ = [[stage2(i), est2(i), None]]
            if i + 1 < NT:
                st_.append([chain(stage_bis(i + 1), stage1b(i + 1)), est_bis(i + 1), "bis"])
            if i + 2 < NT:
                st_.append([stage_idx(i + 2), est_idx(i + 2), "idx"])
            lock3(st_)
        k.ps_base = 0
        k.ps_n = 8
        S.barrier()


def phase_B(k):
    nc, S = k.nc, k.S
    with ExitStack() as es:
        def sb(name, shape, dt):
            return es.enter_context(nc.sbuf_tensor(name, list(shape), dt))
        b_c = Buf("constB")
        gm = sb("gm", [128, 4, 128], F32)
        S.dma_op("sp", gm[:], k.gmasks.rearrange("m p c -> p m c"), writes=[b_c])
        cmask = sb("cmask_sb", [128, 2], F32)
        S.dma_op("sp", cmask[:], k.cmask, writes=[b_c])
        onesf = sb("onesf", [128, 128], F32)
        S.op("pool", lambda: nc.gpsimd.memset(onesf[:], 1.0), writes=[b_c])
        nhalf = sb("nhalfB", [128, 8], F32)
        S.op("pool", lambda: nc.gpsimd.memset(nhalf[:], -0.5), writes=[b_c])
        wg = sb("wg_sb", [128, 8, D], BF16)
        wo = sb("wo_sb", [128, 8, D], BF16)
        S.dma_op("pool", wg[:], k.wg.rearrange("(h p) n -> p h n", p=128), writes=[b_c])
        S.dma_op("pool", wo[:], k.wo.rearrange("(h p) n -> p h n", p=128), writes=[b_c])
        gn = sb("gn", [128, 128], F32)
        S.dma_op("sp", gn[:], k.gdn_norm.partition_broadcast(128), writes=[b_c])
        g2 = sb("g2", [128, D], F32)
        S.dma_op("sp", g2[:], k.norms[1:2, :].partition_broadcast(128), writes=[b_c])
        Sst = sb("Sst", [128, 8, 128], F32); b_S = Buf("Sst")
        Sbf = sb("Sbf", [128, 8, 128], BF16); b_Sbf = Buf("Sbf")
        S.op("pool", lambda: nc.gpsimd.memset(Sst[:], 0.0), writes=[b_S])
        S.op("pool", lambda: nc.gpsimd.memset(Sbf[:], 0.0), writes=[b_Sbf])
        S.barrier()

        def mk(name, shape, dt, n=1):
            ts = [sb("%s%d" % (name, i), shape, dt) for i in range(n)]
            bs = [Buf("%s%d" % (name, i)) for i in range(n)]
            return (ts, bs) if n > 1 else (ts[0], bs[0])
        qg, b_qg = mk("qgB", [128, 8, 128], BF16)
        kg, b_kg = mk("kgB", [128, 8, 128], BF16)
        vg, b_vg = mk("vgB", [128, 8, 128], BF16)
        zs2, b_zs2 = mk("zsB", [128, 8, 128], F32, 2)
        bg, b_bg = mk("bgB", [128, 16], F32)
        sga2, b_sga2 = mk("sgaB", [128, D], F32, 2)
        mdsa2, b_mdsa2 = mk("mdsaB", [128, D], F32, 2)
        hx2, b_hx2 = mk("hxB", [128, D], F32, 2)
        sc, b_sc = mk("scB", [128, 8, 8], F32)
        glb2, b_glb2 = mk("glb", [128, 2, 8], F32, 2)
        Bu, b_Bu = mk("Bu", [128, 8, 128], F32)
        B2, b_B2 = mk("B2", [128, 2, 8], F32)
        kb, b_kb = mk("kbB", [128, 8, 128], BF16)
        qd, b_qd = mk("qdB", [128, 8, 128], BF16)
        kbg, b_kbg = mk("kbgB", [128, 8, 128], BF16)
        ktl2, b_ktl2 = mk("ktlB", [128, 8, 128], BF16, 2)
        vb, b_vb = mk("vbB", [128, 8, 128], BF16)
        kT, b_kT = mk("kTB", [128, 8, 128], BF16)
        kbT, b_kbT = mk("kbTB", [128, 8, 128], BF16)
        qT, b_qT = mk("qTB", [128, 8, 128], BF16)
        qdT2, b_qdT2 = mk("qdTB", [128, 8, 128], BF16, 2)
        D1, b_D1 = mk("D1", [128, 8, 128], F32)
        Em, b_Em = mk("Em", [128, 8, 128], F32)
        ETm, b_ETm = mk("ETm", [128, 8, 128], F32)
        tA, b_tA = mk("tA", [128, 8, 128], F32)
        Bm, b_Bm = mk("Bm", [128, 8, 128], BF16, 2)
        Cm, b_Cm = mk("Cm", [128, 8, 128], BF16, 2)
        Ym, b_Ym = mk("Ym", [128, 8, 128], BF16, 2)
        Yf, b_Yf = mk("Yf", [128, 8, 128], F32, 2)
        aT2, b_aT2 = mk("aT", [128, 8, 128], BF16, 2)
        uS2, b_uS2 = mk("uS", [128, 8, 128], F32, 2)
        wT2, b_wT2 = mk("wT", [128, 8, 128], BF16, 2)
        vn, b_vn = mk("vn", [128, 8, 128], BF16)
        osb, b_osb = mk("osb", [128, 8, 128], F32)
        st8, b_st8 = mk("st8", [128, 24], F32)
        sqt, b_sqt = mk("sqtB", [128, 8, 128], F32)
        yb, b_yb = mk("ybB", [128, 8, 128], BF16)
        yT, b_yT = mk("yTB", [128, 8, 128], BF16)
        mg, b_mg = mk("mgB", [128, D], F32)
        mgb, b_mgb = mk("mgbB", [128, D], BF16)
        mT, b_mT = mk("mTB", [128, 8, 128], BF16)
        mx, b_mx = mk("mxB", [128, D], F32)
        jb, b_jb = mk("jbB", [128, D], BF16)

        def bc_h(ap2):
            return ap2.unsqueeze(2).broadcast_to([128, 8, 128])

        def bc_m(ap2):
            return ap2.unsqueeze(1).broadcast_to([128, 8, 128])

        pool_rr = {"par": 0, "seq": 0}
        cur_pool = ["par"]

        class PT(tuple):
            pass

        def two_banks():
            if cur_pool[0] == "par":
                m = pool_rr["par"] % 2
                pool_rr["par"] += 1
            else:
                m = 2 + pool_rr["seq"] % 2
                pool_rr["seq"] += 1
            pss = PT((k.PS[2 * m], k.PS[2 * m + 1]))
            pss.big = k.PSB[m]
            return pss, (k.PB[2 * m], k.PB[2 * m + 1])

        def nextps():
            pss, bps = two_banks()
            return pss[0], bps[0]

        def pv(ps, hh):
            return ps[hh // 4][:, (hh % 4) * 128:(hh % 4 + 1) * 128]

        def mm8(lhs_fn, rhs_fn, reads):
            pss, bps = two_banks()
            fns = [(lambda hh=hh: nc.tensor.matmul(pv(pss, hh), lhsT=lhs_fn(hh), rhs=rhs_fn(hh),
                                                   start=True, stop=True)) for hh in range(8)]
            S.group("pe", fns, reads=reads, writes=[bps[0], bps[1]])
            return pss, bps

        def ev8(eng, fn, pss, bps, reads, writes):
            S.op(eng, (lambda: fn(pss.big[:].rearrange("p (h t) -> p h t", h=8), slice(0, 8))),
                 reads=[bps[0], bps[1]] + reads, writes=writes)

        def tr8(src, b_src, dst, b_dst):
            ps, bp = nextps()
            psb = ps[:].bitcast(BF16).rearrange("p (k t) -> p k t", k=8)
            fns = [(lambda hh=hh: nc.tensor.transpose(psb[:, hh, :], src[:, hh, :], k.identb[:])) for hh in range(8)]
            S.group("pe", fns, reads=[b_src, k.b_ident], writes=[bp])
            S.op("act", lambda: nc.scalar.copy(out=dst[:], in_=psb), reads=[bp], writes=[b_dst])

        def tile_par(i):
            qdT, b_qdT = qdT2[i % 2], b_qdT2[i % 2]
            aT, b_aT = aT2[i % 2], b_aT2[i % 2]
            uS, b_uS = uS2[i % 2], b_uS2[i % 2]
            wT, b_wT = wT2[i % 2], b_wT2[i % 2]
            ktl, b_ktl = ktl2[i % 2], b_ktl2[i % 2]
            glb, b_glb = glb2[i % 2], b_glb2[i % 2]
            zs, b_zs = zs2[i % 2], b_zs2[i % 2]
            sga, b_sga = sga2[i % 2], b_sga2[i % 2]
            mdsa, b_mdsa = mdsa2[i % 2], b_mdsa2[i % 2]
            hx, b_hx = hx2[i % 2], b_hx2[i % 2]
            r0, r1 = i * 128, (i + 1) * 128
            yield
            S.dma_op("sp", qg[:], k.QG[r0:r1, :].rearrange("p (h d) -> p h d", h=8), writes=[b_qg])
            yield
            S.dma_op("sp", kg[:], k.KG[r0:r1, :].rearrange("p (h d) -> p h d", h=8), writes=[b_kg])
            yield
            S.dma_op("sp", vg[:], k.VG[r0:r1, :].rearrange("p (h d) -> p h d", h=8), writes=[b_vg])
            yield
            S.dma_op("sp", zs[:], k.ZS[r0:r1, :].rearrange("p (h d) -> p h d", h=8), writes=[b_zs])
            yield
            S.dma_op("sp", bg[:], k.BG[r0:r1, :], writes=[b_bg])
            yield
            S.dma_op("sp", sga[:], k.SGA[r0:r1, :], writes=[b_sga])
            yield
            S.dma_op("sp", mdsa[:], k.MDSA[r0:r1, :], writes=[b_mdsa])
            if i == 0:
                yield
                S.op("pool", lambda: nc.gpsimd.memset(hx[:], 0.0), writes=[b_hx])
                yield
                S.dma_op("sp", hx[PAD:128, :], k.meta, writes=[b_hx])
            else:
                yield
                S.dma_op("sp", hx[:], k.x[r0 - 128:r1 - 128, :], writes=[b_hx])
            beta = bg[:, 0:8]
            gg = bg[:, 8:16]
            yield
            ps, bp = nextps()
            yield
            S.op("pe", lambda: nc.tensor.matmul(ps[:, 0:8], lhsT=gm[:, 0, :], rhs=gg, start=True, stop=True),
                 reads=[b_bg, b_c], writes=[bp])
            yield
            S.op("pe", lambda: nc.tensor.matmul(ps[:, 8:16], lhsT=gm[:, 1, :], rhs=gg, start=True, stop=True),
                 reads=[b_bg, b_c], writes=[bp])
            yield
            S.op("dve", lambda: nc.vector.tensor_copy(sc[:, 0:2, :], ps[:, 0:16].rearrange("p (a h) -> p a h", a=2)),
                 reads=[bp], writes=[b_sc])
            yield
            S.op("act", lambda: nc.scalar.activation(out=sc[:, 2, :], in_=sc[:, 0, :], func=AF.Exp),
                 reads=[b_sc], writes=[b_sc])
            yield
            S.op("dve", lambda: nc.vector.tensor_tensor(out=sc[:, 3, :], in0=sc[:, 2, :], in1=beta, op=ALU.mult),
                 reads=[b_sc, b_bg], writes=[b_sc])
            yield
            S.op("dve", lambda: nc.vector.tensor_tensor(out=sc[:, 5, :], in0=sc[:, 1, :], in1=sc[:, 0, :],
                                                        op=ALU.subtract), reads=[b_sc], writes=[b_sc])
            yield
            S.op("act", lambda: nc.scalar.activation(out=sc[:, 4, :], in_=sc[:, 5, :], func=AF.Exp),
                 reads=[b_sc], writes=[b_sc])
            yield
            S.op("dve", lambda: nc.vector.tensor_tensor(out=B2[:], in0=gg.unsqueeze(1).broadcast_to([128, 2, 8]),
                                                        in1=cmask[:].unsqueeze(2).broadcast_to([128, 2, 8]),
                                                        op=ALU.mult), reads=[b_bg, b_c], writes=[b_B2])
            yield
            ps, bp = nextps()
            yield
            S.op("pe", lambda: nc.tensor.matmul(ps[:, 0:16], lhsT=onesf[:], rhs=B2[:].rearrange("p c h -> p (c h)"),
                                                start=True, stop=True), reads=[b_B2, b_c], writes=[bp])
            yield
            S.op("act", lambda: nc.scalar.activation(out=glb[:].rearrange("p c h -> p (c h)"), in_=ps[:, 0:16],
                                                     func=AF.Exp), reads=[bp], writes=[b_glb])
            yield
            S.op("dve", lambda: nc.vector.tensor_tensor(out=Bu[:], in0=bc_m(gm[:, 0, :]), in1=bc_h(gg), op=ALU.mult),
                 reads=[b_bg, b_c], writes=[b_Bu])
            pss, bps = two_banks()
            yield
            for half in range(2):
                yield
                S.op("pe", (lambda half=half: nc.tensor.matmul(
                    pss[half][:], lhsT=onesf[:], rhs=Bu[:, 4 * half:4 * half + 4, :].rearrange("p h t -> p (h t)"),
                    start=True, stop=True)), reads=[b_Bu, b_c], writes=[bps[half]])
            yield
            ev8("dve", lambda psv, sl: nc.vector.tensor_tensor(
                out=D1[:, sl, :], in0=psv,
                in1=sc[:, 0, :].unsqueeze(2).broadcast_to([128, 8, 128]), op=ALU.subtract),
                pss, bps, [b_sc], [b_D1])
            yield
            S.op("dve", lambda: nc.vector.tensor_scalar(out=Em[:], in0=D1[:], scalar1=0.0, scalar2=None, op0=ALU.max),
                 reads=[b_D1], writes=[b_Em])
            yield
            S.op("act", lambda: nc.scalar.activation(out=Em[:], in_=Em[:], func=AF.Exp, scale=-1.0),
                 reads=[b_Em], writes=[b_Em])
            yield
            S.op("dve", lambda: nc.vector.tensor_scalar(out=ETm[:], in0=D1[:], scalar1=0.0, scalar2=None, op0=ALU.min),
                 reads=[b_D1], writes=[b_ETm])
            yield
            S.op("act", lambda: nc.scalar.activation(out=ETm[:], in_=ETm[:], func=AF.Exp),
                 reads=[b_ETm], writes=[b_ETm])
            yield
            S.op("pool", lambda: nc.gpsimd.tensor_tensor(out=kb[:], in0=kg[:], in1=bc_h(beta), op=ALU.mult),
                 reads=[b_kg, b_bg], writes=[b_kb])
            yield
            S.op("pool", lambda: nc.gpsimd.tensor_tensor(out=qd[:], in0=qg[:], in1=bc_h(sc[:, 2, :]), op=ALU.mult),
                 reads=[b_qg, b_sc], writes=[b_qd])
            yield
            S.op("pool", lambda: nc.gpsimd.tensor_tensor(out=kbg[:], in0=kg[:], in1=bc_h(sc[:, 3, :]), op=ALU.mult),
                 reads=[b_kg, b_sc], writes=[b_kbg])
            yield
            S.op("pool", lambda: nc.gpsimd.tensor_tensor(out=ktl[:], in0=kg[:], in1=bc_h(sc[:, 4, :]), op=ALU.mult),
                 reads=[b_kg, b_sc], writes=[b_ktl])
            yield
            S.op("pool", lambda: nc.gpsimd.tensor_tensor(out=vb[:], in0=vg[:], in1=bc_h(beta), op=ALU.mult),
                 reads=[b_vg, b_bg], writes=[b_vb])
            yield
            tr8(kg, b_kg, kT, b_kT)
            yield
            tr8(kb, b_kb, kbT, b_kbT)
            yield
            tr8(qg, b_qg, qT, b_qT)
            yield
            tr8(qd, b_qd, qdT, b_qdT)
            yield
            S.op("pool", lambda: nc.gpsimd.tensor_tensor(out=tA[:], in0=Em[:], in1=bc_m(gm[:, 2, :]), op=ALU.mult),
                 reads=[b_Em, b_c], writes=[b_tA])
            yield
            pss, bps = mm8(lambda hh: kbT[:, hh, :], lambda hh: kT[:, hh, :], [b_kbT, b_kT])
            yield
            ev8("dve", lambda psv, sl: nc.vector.tensor_tensor(out=Bm[0][:, sl, :], in0=psv,
                                                                  in1=tA[:, sl, :], op=ALU.mult),
                pss, bps, [b_tA], [b_Bm[0]])
            yield
            S.op("pool", lambda: nc.gpsimd.tensor_tensor(out=tA[:], in0=ETm[:], in1=bc_m(gm[:, 3, :]), op=ALU.mult),
                 reads=[b_ETm, b_c], writes=[b_tA])
            yield
            pss, bps = mm8(lambda hh: kT[:, hh, :], lambda hh: kbT[:, hh, :], [b_kbT, b_kT])
            yield
            ev8("dve", lambda psv, sl: nc.vector.tensor_tensor(out=Cm[0][:, sl, :], in0=psv,
                                                                  in1=tA[:, sl, :], op=ALU.mult),
                pss, bps, [b_tA], [b_Cm[0]])
            yield
            S.op("pool", lambda: nc.gpsimd.tensor_tensor(out=tA[:], in0=ETm[:], in1=bc_m(gm[:, 0, :]), op=ALU.mult),
                 reads=[b_ETm, b_c], writes=[b_tA])
            yield
            pss, bps = mm8(lambda hh: kT[:, hh, :], lambda hh: qT[:, hh, :], [b_qT, b_kT])
            yield
            ev8("dve", lambda psv, sl: nc.vector.tensor_tensor(out=aT[:, sl, :], in0=psv,
                                                                  in1=tA[:, sl, :], op=ALU.mult),
                pss, bps, [b_tA], [b_aT])
            yield
            S.op("dve", lambda: nc.vector.tensor_tensor(out=Yf[0][:], in0=Cm[0][:], in1=bc_m(k.identf[:]), op=ALU.add),
                 reads=[b_Cm[0], k.b_ident], writes=[b_Yf[0]])
            yield
            S.op("act", lambda: nc.scalar.copy(out=Ym[0][:], in_=Yf[0][:]), reads=[b_Yf[0]], writes=[b_Ym[0]])
            cur = 0
            yield
            for lev in range(1, 6):
                nx = 1 - cur
                yield
                pss, bps = mm8(lambda hh: Cm[cur][:, hh, :], lambda hh: Bm[cur][:, hh, :], [b_Cm[cur], b_Bm[cur]])
                yield
                ev8("act", lambda psv, sl: nc.scalar.copy(out=Bm[nx][:, sl, :], in_=psv),
                    pss, bps, [], [b_Bm[nx]])
                if lev < 5:
                    yield
                    pss, bps = mm8(lambda hh: Bm[cur][:, hh, :], lambda hh: Cm[cur][:, hh, :], [b_Cm[cur], b_Bm[cur]])
                    yield
                    ev8("act", lambda psv, sl: nc.scalar.copy(out=Cm[nx][:, sl, :], in_=psv),
                        pss, bps, [], [b_Cm[nx]])
                yield
                pss, bps = mm8(lambda hh: Bm[nx][:, hh, :], lambda hh: Ym[cur][:, hh, :], [b_Bm[nx], b_Ym[cur]])
                yield
                ev8("dve", lambda psv, sl: nc.vector.tensor_tensor(out=Yf[nx][:, sl, :], in0=psv,
                                                                      in1=Yf[cur][:, sl, :],
                                                                      op=ALU.add),
                    pss, bps, [b_Yf[cur]], [b_Yf[nx]])
                yield
                S.op("act", lambda: nc.scalar.copy(out=Ym[nx][:], in_=Yf[nx][:]), reads=[b_Yf[nx]], writes=[b_Ym[nx]])
                cur = nx
            Y = Ym[cur]
            bY = b_Ym[cur]
            yield
            pss, bps = mm8(lambda hh: Y[:, hh, :], lambda hh: vb[:, hh, :], [bY, b_vb])
            yield
            ev8("act", lambda psv, sl: nc.scalar.copy(out=uS[:, sl, :], in_=psv),
                pss, bps, [], [b_uS])
            yield
            pss, bps = mm8(lambda hh: kbg[:, hh, :], lambda hh: Y[:, hh, :], [bY, b_kbg])
            yield
            ev8("act", lambda psv, sl: nc.scalar.copy(out=wT[:, sl, :], in_=psv),
                pss, bps, [], [b_wT])

        def tile_seq(i):
            r0, r1 = i * 128, (i + 1) * 128
            qdT, b_qdT = qdT2[i % 2], b_qdT2[i % 2]
            aT, b_aT = aT2[i % 2], b_aT2[i % 2]
            uS, b_uS = uS2[i % 2], b_uS2[i % 2]
            wT, b_wT = wT2[i % 2], b_wT2[i % 2]
            ktl, b_ktl = ktl2[i % 2], b_ktl2[i % 2]
            glb, b_glb = glb2[i % 2], b_glb2[i % 2]
            zs, b_zs = zs2[i % 2], b_zs2[i % 2]
            sga, b_sga = sga2[i % 2], b_sga2[i % 2]
            mdsa, b_mdsa = mdsa2[i % 2], b_mdsa2[i % 2]
            hx, b_hx = hx2[i % 2], b_hx2[i % 2]
            yield
            for c in range(2):
                c0, c1 = 64 * c, 64 * c + 64
                pss, bps = two_banks()
                fns = [(lambda hh=hh: nc.tensor.matmul(pv(pss, hh)[c0:c1, :], lhsT=wT[:, hh, c0:c1],
                                                       rhs=Sbf[:, hh, :], start=True, stop=True))
                       for hh in range(8)]
                yield
                S.group("pe", fns, reads=[b_wT, b_Sbf], writes=[bps[0], bps[1]])
                yield
                S.op("dve", lambda: nc.vector.tensor_tensor(
                    out=vn[c0:c1, :, :], in0=uS[c0:c1, :, :],
                    in1=pss.big[c0:c1, :].rearrange("p (h t) -> p h t", h=8), op=ALU.subtract),
                    reads=[bps[0], bps[1], b_uS], writes=[b_vn])
                pso, bpo = two_banks()
                fns = []
                for hh in range(8):
                    fns.append(lambda hh=hh: nc.tensor.matmul(pv(pso, hh)[c0:c1, :], lhsT=qdT[:, hh, c0:c1],
                                                              rhs=Sbf[:, hh, :], start=True, stop=False))
                    fns.append(lambda hh=hh: nc.tensor.matmul(pv(pso, hh)[c0:c1, :], lhsT=aT[c0:c1, hh, c0:c1],
                                                              rhs=vn[c0:c1, hh, :], start=False, stop=True))
                yield
                S.group("pe", fns, reads=[b_qdT, b_Sbf, b_aT, b_vn], writes=[bpo[0], bpo[1]])
                yield
                S.op("act", lambda: nc.scalar.copy(
                    out=osb[c0:c1, :, :], in_=pso.big[c0:c1, :].rearrange("p (h t) -> p h t", h=8)),
                    reads=[bpo[0], bpo[1]], writes=[b_osb])
                pss2, bps2 = two_banks()
                fns = [(lambda hh=hh: nc.tensor.matmul(pv(pss2, hh), lhsT=ktl[c0:c1, hh, :], rhs=vn[c0:c1, hh, :],
                                                       start=True, stop=True)) for hh in range(8)]
                yield
                S.group("pe", fns, reads=[b_ktl, b_vn], writes=[bps2[0], bps2[1]])
                yield
                S.op("pool", lambda: nc.gpsimd.tensor_tensor(out=Sst[:], in0=Sst[:], in1=bc_h(glb[:, c, :]),
                                                             op=ALU.mult), reads=[b_S, b_glb], writes=[b_S])
                yield
                S.op("dve", lambda: nc.vector.tensor_tensor(
                    out=Sst[:], in0=Sst[:], in1=pss2.big[:].rearrange("p (h t) -> p h t", h=8), op=ALU.add),
                    reads=[bps2[0], bps2[1], b_S], writes=[b_S])
                yield
                S.op("act", lambda: nc.scalar.copy(out=Sbf[:], in_=Sst[:]), reads=[b_S], writes=[b_Sbf])
            yield
            S.op("dve", lambda: nc.vector.tensor_tensor(out=sqt[:], in0=osb[:], in1=osb[:], op=ALU.mult),
                 reads=[b_osb], writes=[b_sqt])
            yield
            S.op("dve", lambda: nc.vector.tensor_reduce(out=st8[:, 0:8], in_=sqt[:], axis=AX.X, op=ALU.add),
                 reads=[b_sqt], writes=[b_st8])
            yield
            S.op("pool", lambda: nc.gpsimd.tensor_scalar(out=st8[:, 8:16], in0=st8[:, 0:8], scalar1=1.0 / 128,
                                                         scalar2=EPS, op0=ALU.mult, op1=ALU.add),
                 reads=[b_st8], writes=[b_st8])
            yield
            S.op("pool", lambda: nc.gpsimd.tensor_tensor(out=st8[:, 16:24], in0=st8[:, 8:16], in1=nhalf[:], op=ALU.pow),
                 reads=[b_st8, b_c], writes=[b_st8])
            yield
            S.op("pool", lambda: nc.gpsimd.tensor_tensor(out=zs[:], in0=zs[:], in1=bc_m(gn[:]), op=ALU.mult),
                 reads=[b_zs, b_c], writes=[b_zs])
            yield
            S.op("dve", lambda: nc.vector.tensor_tensor(out=osb[:], in0=osb[:], in1=bc_h(st8[:, 16:24]), op=ALU.mult),
                 reads=[b_osb, b_st8], writes=[b_osb])
            yield
            S.op("dve", lambda: nc.vector.tensor_tensor(out=yb[:], in0=osb[:], in1=zs[:], op=ALU.mult),
                 reads=[b_osb, b_zs], writes=[b_yb])
            if k.debug:
                yield
                S.dma_op("sp", k.YGDN[r0:r1, :].rearrange("p (h d) -> p h d", h=8), yb[:], reads=[b_yb])
            yield
            tr8(yb, b_yb, yT, b_yT)
            yield
            for cb in range(2):
                ps, bp = nextps()
                fns = [(lambda h=h: nc.tensor.matmul(ps[:], lhsT=yT[:, h, :], rhs=wg[:, h, cb * 512:(cb + 1) * 512],
                                                     start=(h == 0), stop=(h == 7))) for h in range(8)]
                yield
                S.group("pe", fns, reads=[b_yT, b_c], writes=[bp])
                yield
                S.op("dve", lambda: nc.vector.tensor_tensor(out=mg[:, cb * 512:(cb + 1) * 512], in0=ps[:],
                                                            in1=sga[:, cb * 512:(cb + 1) * 512], op=ALU.mult),
                     reads=[bp, b_sga], writes=[b_mg])
            yield
            S.op("pool", lambda: nc.gpsimd.tensor_tensor(out=mgb[:], in0=mg[:], in1=mdsa[:], op=ALU.add),
                 reads=[b_mg, b_mdsa], writes=[b_mgb])
            yield
            tr8(mgb[:].rearrange("p (h d) -> p h d", h=8), b_mgb, mT, b_mT)
            yield
            for cb in range(2):
                ps, bp = nextps()
                fns = [(lambda h=h: nc.tensor.matmul(ps[:], lhsT=mT[:, h, :], rhs=wo[:, h, cb * 512:(cb + 1) * 512],
                                                     start=(h == 0), stop=(h == 7))) for h in range(8)]
                yield
                S.group("pe", fns, reads=[b_mT, b_c], writes=[bp])
                yield
                S.op("act", lambda: nc.scalar.copy(out=mx[:, cb * 512:(cb + 1) * 512], in_=ps[:]),
                     reads=[bp], writes=[b_mx])
            yield
            S.op("act", lambda: nc.scalar.activation(out=jb[:], in_=mx[:], func=AF.Square, accum_out=st8[:, 0:1]),
                 reads=[b_mx], writes=[b_jb, b_st8])
            yield
            S.op("pool", lambda: nc.gpsimd.tensor_scalar(out=st8[:, 8:9], in0=st8[:, 0:1], scalar1=1.0 / D,
                                                         scalar2=EPS, op0=ALU.mult, op1=ALU.add),
                 reads=[b_st8], writes=[b_st8])
            yield
            S.op("pool", lambda: nc.gpsimd.tensor_tensor(out=st8[:, 16:17], in0=st8[:, 8:9], in1=nhalf[:, 0:1],
                                                         op=ALU.pow), reads=[b_st8, b_c], writes=[b_st8])
            yield
            S.op("dve", lambda: nc.vector.scalar_tensor_tensor(out=mx[:], in0=mx[:], scalar=st8[:, 16:17], in1=g2[:],
                                                               op0=ALU.mult, op1=ALU.mult),
                 reads=[b_mx, b_st8, b_c], writes=[b_mx])
            yield
            S.op("pool", lambda: nc.gpsimd.tensor_tensor(out=mx[:], in0=mx[:], in1=hx[:], op=ALU.add),
                 reads=[b_mx, b_hx], writes=[b_mx])
            yield
            S.dma_op("sp", k.H1[r0:r1, :], mx[:], reads=[b_mx])

        def drain(g, pool):
            cur_pool[0] = pool
            n = 0
            for _ in g:
                n += 1
            return n

        def lockstep(gS, nS, gP, nP):
            aS = aP = True
            pS = pP = 0.0
            cS = cP = 0
            while aS or aP:
                if aS and (not aP or pS <= pP):
                    cur_pool[0] = "seq"
                    try:
                        next(gS)
                        cS += 1
                        pS += 1.0 / nS
                    except StopIteration:
                        aS = False
                else:
                    cur_pool[0] = "par"
                    try:
                        next(gP)
                        cP += 1
                        pP += 1.0 / nP
                    except StopIteration:
                        aP = False
            return cS, cP

        nP = drain(tile_par(0), "par")
        nS = nP
        for i in range(NT):
            if i + 1 < NT:
                cS, cP = lockstep(tile_seq(i), nS, tile_par(i + 1), nP)
                nS, nP = max(cS, 1), max(cP, 1)
            else:
                drain(tile_seq(i), "seq")
        S.barrier()


def phase_E(k):
    nc, S = k.nc, k.S
    with ExitStack() as es:
        def sb(name, shape, dt):
            return es.enter_context(nc.sbuf_tensor(name, list(shape), dt))
        b_c = Buf("constE")
        wu = sb("wu_sb", [128, 8, 4096], BF16)
        wdn = sb("wdn_sb", [128, 32, D], BF16)
        for kk in range(8):
            S.dma_op("pool", wu[:, kk, :], k.w_up[kk * 128:(kk + 1) * 128, :], writes=[b_c])
        for kk in range(0, 32, 4):
            S.dma_op("pool", wdn[:, kk:kk + 4, :],
                     k.w_down[kk * 128:(kk + 4) * 128, :].rearrange("(f p) n -> p f n", p=128), writes=[b_c])
        g3 = sb("g3", [128, D], F32)
        g4 = sb("g4", [128, D], F32)
        S.dma_op("sp", g3[:], k.norms[2:3, :].partition_broadcast(128), writes=[b_c])
        S.dma_op("sp", g4[:], k.norms[3:4, :].partition_broadcast(128), writes=[b_c])
        nhalf = sb("nhalfE", [128, 1], F32)
        S.op("pool", lambda: nc.gpsimd.memset(nhalf[:], -0.5), writes=[b_c])
        S.barrier()
        h1 = [sb("h1_%d" % i, [128, 2, D], F32) for i in range(2)]; b_h1 = [Buf("h1_%d" % i) for i in range(2)]
        jb = sb("jbE", [128, D], BF16); b_jb = Buf("jbE")
        st = [sb("stE%d" % i, [128, 2, 8], F32) for i in range(2)]; b_st = [Buf("stE%d" % i) for i in range(2)]
        n2 = [sb("n2_%d" % i, [128, D], BF16) for i in range(2)]; b_n2 = [Buf("n2_%d" % i) for i in range(2)]
        n2T = [sb("n2T%d" % i, [128, 8, 256], BF16) for i in range(2)]; b_n2T = [Buf("n2T%d" % i) for i in range(2)]
        uT1 = sb("uT0", [128, 32, 256], BF16); b_uT1 = Buf("uT0")
        uT = [uT1, uT1]; b_uT = [b_uT1, b_uT1]
        rl = [sb("rl%d" % i, [128, 512], F32) for i in range(2)]; b_rl = [Buf("rl%d" % i) for i in range(2)]
        mo = [sb("mo%d" % i, [128, D], F32) for i in range(2)]; b_mo = [Buf("mo%d" % i) for i in range(2)]
        cnt = {"r": 0, "n": 0, "m": 0}
        NG = (NT - 1) // 2

        def head(gi):
            p = gi % 2
            for t in range(2):
                i = 1 + 2 * gi + t
                r0, r1 = i * 128, (i + 1) * 128
                S.dma_op("sp", h1[p][:, t, :], k.H1[r0:r1, :], writes=[b_h1[p]])
            for t in range(2):
                S.op("act", lambda: nc.scalar.activation(out=jb[:], in_=h1[p][:, t, :], func=AF.Square,
                                                         accum_out=st[p][:, t, 0:1]),
                     reads=[b_h1[p]], writes=[b_jb, b_st[p]])
                S.op("pool", lambda: nc.gpsimd.tensor_scalar(out=st[p][:, t, 1:2], in0=st[p][:, t, 0:1],
                                                             scalar1=1.0 / D, scalar2=EPS, op0=ALU.mult, op1=ALU.add),
                     reads=[b_st[p]], writes=[b_st[p]])
                S.op("pool", lambda: nc.gpsimd.tensor_tensor(out=st[p][:, t, 2:3], in0=st[p][:, t, 1:2], in1=nhalf[:],
                                                             op=ALU.pow), reads=[b_st[p], b_c], writes=[b_st[p]])
                np_ = cnt["n"] % 2
                cnt["n"] += 1
                S.op("dve", lambda: nc.vector.scalar_tensor_tensor(out=n2[np_][:], in0=h1[p][:, t, :],
                                                                   scalar=st[p][:, t, 2:3], in1=g3[:],
                                                                   op0=ALU.mult, op1=ALU.mult),
                     reads=[b_h1[p], b_st[p], b_c], writes=[b_n2[np_]])
                ps, bp = k.nextps()
                psb = ps[:].bitcast(BF16).rearrange("p (k t) -> p k t", k=8)
                fns = [(lambda kk=kk: nc.tensor.transpose(psb[:, kk, :], n2[np_][:, kk * 128:(kk + 1) * 128],
                                                          k.identb[:])) for kk in range(8)]
                S.group("pe", fns, reads=[b_n2[np_], k.b_ident], writes=[bp])
                S.op("act", lambda: nc.scalar.copy(out=n2T[p][:, :, t * 128:(t + 1) * 128], in_=psb),
                     reads=[bp], writes=[b_n2T[p]])

        def up(gi):
            p = gi % 2
            for fb in range(16):
                ps, bp = k.nextps()
                fns = []
                for f2 in range(2):
                    f = fb * 2 + f2
                    for kk in range(8):
                        fns.append(lambda f=f, f2=f2, kk=kk: nc.tensor.matmul(
                            ps[:, f2 * 256:(f2 + 1) * 256], lhsT=wu[:, kk, f * 128:(f + 1) * 128], rhs=n2T[p][:, kk, :],
                            start=(kk == 0), stop=(kk == 7)))
                S.group("pe", fns, reads=[b_n2T[p], b_c], writes=[bp])
                rp = cnt["r"] % 2
                cnt["r"] += 1
                S.op("act", lambda: nc.scalar.activation(out=rl[rp][:], in_=ps[:], func=AF.Relu),
                     reads=[bp], writes=[b_rl[rp]])
                S.op("dve", lambda: nc.vector.tensor_tensor(
                    out=uT[p][:, fb * 2:fb * 2 + 2, :].rearrange("p f t -> p (f t)"), in0=rl[rp][:], in1=rl[rp][:],
                    op=ALU.mult), reads=[b_rl[rp]], writes=[b_uT[p]])

        def down(gi):
            p = gi % 2
            for t in range(2):
                i = 1 + 2 * gi + t
                r0, r1 = i * 128, (i + 1) * 128
                mp = cnt["m"] % 2
                cnt["m"] += 1
                for cb in range(2):
                    ps, bp = k.nextps()
                    fns = [(lambda f=f: nc.tensor.matmul(ps[:], lhsT=uT[p][:, f, t * 128:(t + 1) * 128],
                                                         rhs=wdn[:, f, cb * 512:(cb + 1) * 512],
                                                         start=(f == 0), stop=(f == 31))) for f in range(32)]
                    S.group("pe", fns, reads=[b_uT[p], b_c], writes=[bp])
                    S.op("act", lambda: nc.scalar.copy(out=mo[mp][:, cb * 512:(cb + 1) * 512], in_=ps[:]),
                         reads=[bp], writes=[b_mo[mp]])
                S.op("act", lambda: nc.scalar.activation(out=jb[:], in_=mo[mp][:], func=AF.Square,
                                                         accum_out=st[p][:, t, 4:5]),
                     reads=[b_mo[mp]], writes=[b_jb, b_st[p]])
                S.op("pool", lambda: nc.gpsimd.tensor_scalar(out=st[p][:, t, 5:6], in0=st[p][:, t, 4:5],
                                                             scalar1=1.0 / D, scalar2=EPS, op0=ALU.mult, op1=ALU.add),
                     reads=[b_st[p]], writes=[b_st[p]])
                S.op("pool", lambda: nc.gpsimd.tensor_tensor(out=st[p][:, t, 6:7], in0=st[p][:, t, 5:6], in1=nhalf[:],
                                                             op=ALU.pow), reads=[b_st[p], b_c], writes=[b_st[p]])
                S.op("dve", lambda: nc.vector.scalar_tensor_tensor(out=mo[mp][:], in0=mo[mp][:],
                                                                   scalar=st[p][:, t, 6:7], in1=g4[:],
                                                                   op0=ALU.mult, op1=ALU.mult),
                     reads=[b_mo[mp], b_st[p], b_c], writes=[b_mo[mp]])
                S.op("pool", lambda: nc.gpsimd.tensor_tensor(out=mo[mp][:], in0=mo[mp][:], in1=h1[p][:, t, :],
                                                             op=ALU.add),
                     reads=[b_mo[mp], b_h1[p]], writes=[b_mo[mp]])
                S.dma_op("sp", k.out[r0 - 128:r1 - 128, :], mo[mp][:], reads=[b_mo[mp]])

        head(0)
        for gi in range(NG):
            up(gi)
            if gi + 1 < NG:
                head(gi + 1)
            down(gi)
        S.barrier()

def host_consts():
    pos = np.concatenate([np.zeros(PAD, np.float32), np.arange(T - PAD, dtype=np.float32)])

    def tabs(dim, reps):
        inv = (10000.0 ** (-np.arange(0, dim, 2, dtype=np.float32) / dim)).astype(np.float32)
        ang = pos[:, None] * inv[None, :]
        c = np.cos(ang).astype(np.float32)
        s = np.sin(ang).astype(np.float32)
        cT = np.concatenate([c, c], 1).T
        sT = np.concatenate([-s, s], 1).T
        return (np.ascontiguousarray(np.tile(cT, (reps, 1))), np.ascontiguousarray(np.tile(sT, (reps, 1))))
    cosA, sinA = tabs(128, 1)
    cosI, sinI = tabs(64, 2)
    r = np.arange(128)
    NEG = np.float32(-1e30)
    mdiag = np.where(r[None, :] <= r[:, None], 0.0, NEG).astype(np.float32)
    m0 = np.where((r[None, :] <= r[:, None]) & (r[None, :] >= PAD), 0.0, NEG).astype(np.float32)
    mpad = np.where(r[None, :] >= PAD, 0.0, NEG).astype(np.float32) * np.ones((128, 1), np.float32)
    masks = np.ascontiguousarray(np.stack([mdiag, m0, mpad], 0))
    pow2 = (2.0 ** -(np.arange(15, dtype=np.float32) + 1))[None, :].astype(np.float32)
    same = (r[:, None] // 64 == r[None, :] // 64)
    UT = (same & (r[:, None] <= r[None, :])).astype(np.float32)
    SAME = same.astype(np.float32)
    nSL = -(same & (r[:, None] > r[None, :])).astype(np.float32)
    nSU = -(same & (r[:, None] < r[None, :])).astype(np.float32)
    gmasks = np.ascontiguousarray(np.stack([UT, SAME, nSL, nSU], 0))
    cmask = np.stack([(r < 64), (r >= 64)], 1).astype(np.float32)
    return dict(cosA=cosA, sinA=sinA, cosI=cosI, sinI=sinI, ident=np.eye(128, dtype=np.float32),
                masks=masks, pow2=pow2, gmasks=gmasks, cmask=np.ascontiguousarray(cmask))


def swap_halves(w, hd):
    d, n = w.shape
    w = w.reshape(d, n // hd, 2, hd // 2)
    return np.ascontiguousarray(w[:, :, ::-1, :]).reshape(d, n)


def host_inputs(inputs):
    w_in = np.ascontiguousarray(inputs["w_in"][0])
    ik = w_in[:, C_IK:C_IK + 64]
    ikd = np.concatenate([ik, ik], 1)
    groups = []
    aq = w_in[:, C_AQ:C_AQ + 1024]
    aqs = swap_halves(aq, 128)
    for h in range(8):
        groups += [aq[:, h * 128:(h + 1) * 128], aqs[:, h * 128:(h + 1) * 128]]
    ak = w_in[:, C_AK:C_AK + 256]
    aks = swap_halves(ak, 128)
    for h in range(2):
        groups += [ak[:, h * 128:(h + 1) * 128], aks[:, h * 128:(h + 1) * 128]]
    iq = w_in[:, C_IQ:C_IQ + 512]
    iqs = swap_halves(iq, 64)
    for h in range(4):
        groups += [iq[:, h * 128:(h + 1) * 128], iqs[:, h * 128:(h + 1) * 128]]
    groups += [ikd, swap_halves(ikd, 64)]
    w_sw = np.concatenate(groups, 1)
    common = dict(
        meta=np.ascontiguousarray(inputs["meta_tokens"]),
        w_in=w_in, w_sw=np.ascontiguousarray(w_sw),
        conv_w=np.ascontiguousarray(inputs["conv_w"][0]),
        conv_wT=np.ascontiguousarray(inputs["conv_w"][0].T),
        a_log=np.ascontiguousarray(inputs["a_log"]), dt_bias=np.ascontiguousarray(inputs["dt_bias"]),
        gdn_norm=np.ascontiguousarray(inputs["gdn_norm"]),
        wg=np.ascontiguousarray(inputs["w_branch_gdn"][0]), wd=np.ascontiguousarray(inputs["w_branch_dsa"][0]),
        wo=np.ascontiguousarray(inputs["w_out"][0]),
        w_up=np.ascontiguousarray(inputs["w_up"][0]), w_down=np.ascontiguousarray(inputs["w_down"][0]),
        norms=np.ascontiguousarray(np.concatenate([inputs["pre_mix_norm"], inputs["post_mix_norm"],
                                                   inputs["pre_mlp_norm"], inputs["post_mlp_norm"]], 0)),
    )
    common.update(host_consts())
    return common


def kernel(**inputs):
    inputs = {k_: np.asarray(v) for k_, v in inputs.items()}
    nc = build()
    common = host_inputs(inputs)
    in_maps = []
    for b in range(8):
        m = dict(common)
        m["x"] = np.ascontiguousarray(inputs["x"][b])
        in_maps.append(m)
    res = run_bass_kernel_spmd(nc, in_maps, core_ids=list(range(8)))
    return np.stack([r["out"] for r in res.results], 0).astype(np.float32)
```
